# Optimizing a Trainium2 kernel written in Bass

```python
import math
import jax, jax.numpy as jnp
from jax import lax
import numpy as np

D_MODEL = 2048
BATCH = 4
SEQ = 2048
DEPTH = 2

GRID_W = 64
CTX_LEN = 256
EPS = 1e-6
F32 = jnp.float32
NEG_BIG = -1e30
MIN_FORGET = 1e-6

HG_HEADS = 6
HG_DK = 128
HG_DV = 128
HG_WIDTH = HG_HEADS * HG_DK
HG_CHUNK = 64

NA_HEADS = 4
NA_HD = 128
NA_WIDTH = NA_HEADS * NA_HD
NA_WIN_ROWS = 8
NA_WIN_COLS = 16

SSD_HEADS = 12
SSD_HD = 64
SSD_WIDTH = SSD_HEADS * SSD_HD
SSD_GROUPS = 4
SSD_STATE = 128
SSD_CONV = 4
SSD_CHUNK = 128
SSD_CONV_CH = SSD_WIDTH + 2 * SSD_GROUPS * SSD_STATE

ROPE_BASE = 10000.0
N_BRANCH = 3
N_MIX_COLS = 5 * HG_WIDTH + 3 * NA_WIDTH + SSD_WIDTH + SSD_CONV_CH + 2 * SSD_HEADS
N_IN = N_MIX_COLS + N_BRANCH * D_MODEL

N_EXPERTS = 32
TOP_K = 4
D_FF = 2048
SWIGLU_ALPHA = 1.702
SWIGLU_LIMIT = 7.0
MOE_BLOCK = 128

kernel_name = "hybrid_hgrn2_natten_ssd_moe_dit"


def rms_norm(x, w):
    xf = x.astype(F32)
    y = xf * lax.rsqrt(jnp.mean(xf * xf, axis=-1, keepdims=True) + EPS)
    return (y * w.astype(F32)).astype(x.dtype)


def modulate(x, w, shift, scale):
    return rms_norm(x, w) * (1.0 + scale) + shift


def split_in(y):
    sizes = (HG_WIDTH,) * 5 + (NA_WIDTH,) * 3 + (SSD_WIDTH, SSD_CONV_CH, SSD_HEADS, SSD_HEADS)
    return jnp.split(y, np.cumsum(sizes).tolist(), axis=-1)


def axial_rope_2d(t):
    length, n = t.shape[1], t.shape[-1]
    half = n // 2
    pos = jnp.arange(length)
    inv_freq = 1.0 / (ROPE_BASE ** (jnp.arange(0, half, 2, dtype=F32) / half))

    def rotate(u, p):
        ang = p.astype(F32)[:, None] * inv_freq[None, :]
        cos, sin = jnp.cos(ang)[None, :, None, :], jnp.sin(ang)[None, :, None, :]
        u1, u2 = jnp.split(u.astype(F32), 2, axis=-1)
        return jnp.concatenate([u1 * cos - u2 * sin, u2 * cos + u1 * sin], axis=-1)

    out = jnp.concatenate([rotate(t[..., :half], pos // GRID_W), rotate(t[..., half:], pos % GRID_W)], axis=-1)
    return out.astype(t.dtype)


def depthwise_conv_centred(x, w, b):
    k = w.shape[0]
    y = lax.conv_general_dilated(x, w[:, None, :].astype(x.dtype), window_strides=(1,),
                                 padding=[((k - 1) // 2, k // 2)],
                                 dimension_numbers=("NWC", "WIO", "NWC"),
                                 feature_group_count=x.shape[-1])
    return y + b


def to_chunks(t, chunk):
    b, l = t.shape[:2]
    return jnp.moveaxis(t.reshape(b, l // chunk, chunk, *t.shape[2:]), 1, 0)


def from_chunks(t):
    t = jnp.moveaxis(t, 0, 1)
    return t.reshape(t.shape[0], t.shape[1] * t.shape[2], *t.shape[3:])


def masked_decay(cum, causal):
    diff = cum[:, :, None] - cum[:, None, :]
    return jnp.where(causal, jnp.exp(jnp.where(causal, diff, 0.0)), 0.0)


def gla_chunk_scan(q, k, v, logf, s0, return_y, chunk=HG_CHUNK):
    causal = jnp.tril(jnp.ones((chunk, chunk), bool))[None, :, :, None, None]

    def step(s, inp):
        qc, kc, vc, gc = inp
        cum = jnp.cumsum(gc.astype(F32), axis=1)
        last = cum[:, -1]
        kd = kc * jnp.exp(last[:, None] - cum)
        s_new = s * jnp.exp(last)[..., None] + jnp.einsum("bshk,bshv->bhkv", kd, vc)
        if not return_y:
            return s_new, None
        decay = masked_decay(cum, causal)
        att = jnp.einsum("bthk,bshk,btshk->bhts", qc, kc, decay)
        y = jnp.einsum("bhts,bshv->bthv", att, vc) + jnp.einsum("bthk,bhkv->bthv", qc * jnp.exp(cum), s)
        return s_new, y

    s_fin, ys = lax.scan(step, s0, tuple(to_chunks(t, chunk) for t in (q, k, v, logf)))
    return (from_chunks(ys) if return_y else None), s_fin


def ssd_chunk_scan(x, dt, da, bm, cm, s0, return_y, chunk=SSD_CHUNK):
    b, _, h, p = x.shape
    g, n = bm.shape[2], bm.shape[3]
    hg = h // g
    causal = jnp.tril(jnp.ones((chunk, chunk), bool))[None, :, :, None, None]

    def step(s, inp):
        xc, dtc, dac, bc, cc = inp
        xc = xc.reshape(b, chunk, g, hg, p)
        dtc = dtc.reshape(b, chunk, g, hg)
        cum = jnp.cumsum(dac.astype(F32).reshape(b, chunk, g, hg), axis=1)
        last = cum[:, -1]
        sg = s.reshape(b, g, hg, p, n)
        w_end = dtc * jnp.exp(last[:, None] - cum)
        s_new = sg * jnp.exp(last)[..., None, None] + jnp.einsum("bsgn,bsgj,bsgjp->bgjpn", bc, w_end, xc)
        s_new = s_new.reshape(b, h, p, n)
        if not return_y:
            return s_new, None
        seg = masked_decay(cum, causal)
        cb = jnp.einsum("btgn,bsgn->bgts", cc, bc)
        y = jnp.einsum("bgts,btsgj,bsgj,bsgjp->btgjp", cb, seg, dtc, xc)
        y = y + jnp.einsum("btgn,bgjpn->btgjp", cc, sg) * jnp.exp(cum)[..., None]
        return s_new, y.reshape(b, chunk, h, p)

    s_fin, ys = lax.scan(step, s0, tuple(to_chunks(t, chunk) for t in (x, dt, da, bm, cm)))
    return (from_chunks(ys) if return_y else None), s_fin


def bidirectional_scan(scan_fn, lat_fwd, lat_bwd, ctx_fwd, ctx_bwd, s_zero, need_ctx):
    flip = lambda args: tuple(jnp.flip(a, axis=1) for a in args)
    yc_f, sc_f = scan_fn(*ctx_fwd, s_zero, need_ctx)
    yc_b, sc_b = scan_fn(*flip(ctx_bwd), s_zero, need_ctx)
    yl_f, _ = scan_fn(*lat_fwd, sc_f, True)
    yl_b, _ = scan_fn(*flip(lat_bwd), sc_b, True)
    y_lat = yl_f + jnp.flip(yl_b, axis=1)
    y_ctx = yc_f + jnp.flip(yc_b, axis=1) if need_ctx else None
    return y_lat, y_ctx


def hgrn2_mixer(parts_lat, parts_ctx, lb, norm_w, need_ctx):
    def prep(q, f_fwd, f_bwd, i):
        heads = lambda t: t.reshape(t.shape[0], t.shape[1], HG_HEADS, -1)
        q = heads(jax.nn.silu(q)) * (HG_DK ** -0.5)
        v = heads(i)
        dirs = []
        for f_raw, lbd in ((f_fwd, lb[0]), (f_bwd, lb[1])):
            f = lbd + (1.0 - lbd) * jax.nn.sigmoid(f_raw.astype(F32))
            logf = jnp.log(jnp.maximum(f, MIN_FORGET))
            dirs.append((q, heads((1.0 - f).astype(i.dtype)), v, heads(logf)))
        return dirs

    def finish(o, g):
        gh = g.reshape(o.shape)
        o = rms_norm(o, norm_w) * jax.nn.silu(gh.astype(F32))
        return o.reshape(o.shape[0], o.shape[1], HG_WIDTH).astype(g.dtype)

    lf, lbw = prep(*parts_lat[:4])
    cf, cbw = prep(*parts_ctx[:4])
    s_zero = jnp.zeros((parts_lat[0].shape[0], HG_HEADS, HG_DK, HG_DV), F32)
    y_l, y_c = bidirectional_scan(gla_chunk_scan, lf, lbw, cf, cbw, s_zero, need_ctx)
    y_lat = finish(y_l, parts_lat[4])
    y_ctx = finish(y_c, parts_ctx[4]) if need_ctx else None
    return y_lat, y_ctx


def neighbourhood_attention(q_lat, k_lat, v_lat, q_ctx, k_ctx, v_ctx, rpb, need_ctx):
    b, s, h, d = q_lat.shape
    rows = s // GRID_W
    wr = min(NA_WIN_ROWS, rows)
    scale = d ** -0.5
    qg = q_lat.reshape(b, rows, GRID_W, h, d)
    kg = k_lat.reshape(b, rows, GRID_W, h, d)
    vg = v_lat.reshape(b, rows, GRID_W, h, d)
    r = jnp.arange(rows)
    row_idx = jnp.clip(r - wr // 2, 0, rows - wr)[:, None] + jnp.arange(wr)[None, :]
    k_blk = kg[:, row_idx]
    v_blk = vg[:, row_idx]
    j = jnp.arange(GRID_W)
    c0 = jnp.clip(j - NA_WIN_COLS // 2, 0, GRID_W - NA_WIN_COLS)
    col_mask = (j[None, :] >= c0[:, None]) & (j[None, :] < c0[:, None] + NA_WIN_COLS)
    dc = jnp.clip(j[None, :] - j[:, None], -(NA_WIN_COLS - 1), NA_WIN_COLS - 1)
    dr = row_idx - r[:, None]
    bias = rpb[:, (dr + NA_WIN_ROWS - 1)[:, None, :, None], (dc + NA_WIN_COLS - 1)[None, :, None, :]]
    s_lat = jnp.einsum("brjhd,brwchd->bhrjwc", qg, k_blk).astype(F32) * scale + bias.astype(F32)
    s_lat = jnp.where(col_mask[None, None, None, :, None, :], s_lat, NEG_BIG)
    s_lat = s_lat.reshape(b, h, rows, GRID_W, wr * GRID_W)
    s_ctx = jnp.einsum("brjhd,bnhd->bhrjn", qg, k_ctx).astype(F32) * scale
    p = jax.nn.softmax(jnp.concatenate([s_lat, s_ctx], axis=-1), axis=-1).astype(v_lat.dtype)
    p_lat = p[..., :wr * GRID_W].reshape(b, h, rows, GRID_W, wr, GRID_W)
    p_ctx = p[..., wr * GRID_W:]
    o = jnp.einsum("bhrjwc,brwchd->brjhd", p_lat, v_blk) + jnp.einsum("bhrjn,bnhd->brjhd", p_ctx, v_ctx)
    o_lat = o.reshape(b, s, h * d)
    if not need_ctx:
        return o_lat, None
    n_ctx = q_ctx.shape[1]
    p_cc = jax.nn.softmax(jnp.einsum("bmhd,bnhd->bhmn", q_ctx, k_ctx).astype(F32) * scale, axis=-1)
    o_ctx = jnp.einsum("bhmn,bnhd->bmhd", p_cc.astype(v_ctx.dtype), v_ctx).reshape(b, n_ctx, h * d)
    return o_lat, o_ctx


def ssd_mixer(parts_lat, parts_ctx, conv_w, conv_b, dt_bias, a_log, d_skip, norm_w, need_ctx):
    def prep(z, xbc, dt_fwd, dt_bwd, rotary):
        b, l, _ = xbc.shape
        xbc = jax.nn.silu(depthwise_conv_centred(xbc, conv_w, conv_b))
        xs, bm, cm = jnp.split(xbc, [SSD_WIDTH, SSD_WIDTH + SSD_GROUPS * SSD_STATE], axis=-1)
        xs = xs.reshape(b, l, SSD_HEADS, SSD_HD)
        bm = bm.reshape(b, l, SSD_GROUPS, SSD_STATE)
        cm = cm.reshape(b, l, SSD_GROUPS, SSD_STATE)
        if rotary:
            bm, cm = axial_rope_2d(bm), axial_rope_2d(cm)
        dirs = []
        for k, dt_raw in enumerate((dt_fwd, dt_bwd)):
            dt = jax.nn.softplus(dt_raw.astype(F32) + dt_bias[k])
            dirs.append((xs, dt, -dt * jnp.exp(a_log[k].astype(F32)), bm, cm))
        return xs, dirs

    def finish(y, xs, z):
        b, l = z.shape[:2]
        y = (y + d_skip[:, None] * xs).reshape(b, l, SSD_WIDTH) * jax.nn.silu(z.astype(F32))
        y = rms_norm(y.reshape(b, l, SSD_GROUPS, SSD_WIDTH // SSD_GROUPS), norm_w.reshape(SSD_GROUPS, -1))
        return y.reshape(b, l, SSD_WIDTH).astype(z.dtype)

    xs_l, (fl, bl) = prep(*parts_lat, True)
    xs_c, (fc, bc) = prep(*parts_ctx, False)
    s_zero = jnp.zeros((xs_l.shape[0], SSD_HEADS, SSD_HD, SSD_STATE), F32)
    y_l, y_c = bidirectional_scan(ssd_chunk_scan, fl, bl, fc, bc, s_zero, need_ctx)
    y_lat = finish(y_l, xs_l, parts_lat[0])
    y_ctx = finish(y_c, xs_c, parts_ctx[0]) if need_ctx else None
    return y_lat, y_ctx


def clamped_swiglu(gate, up):
    gate = jnp.minimum(gate, SWIGLU_LIMIT)
    up = jnp.clip(up, -SWIGLU_LIMIT, SWIGLU_LIMIT)
    return gate * jax.nn.sigmoid(SWIGLU_ALPHA * gate) * (up + 1.0)


def moe_ffn(x, w_router, b_router, w_gate, b_gate, w_up, b_up, w_down, b_down):
    t, d = x.shape
    logits = (x @ w_router).astype(F32) + b_router.astype(F32)
    top_val, top_idx = lax.top_k(logits, TOP_K)
    top_w = jax.nn.softmax(top_val, axis=-1)
    n_assign = t * TOP_K
    exp_id = top_idx.reshape(-1)
    tok_id = jnp.arange(n_assign) // TOP_K
    order = jnp.argsort(exp_id)
    e_sorted = exp_id[order]
    counts = jnp.bincount(exp_id, length=N_EXPERTS)
    padded = (counts + MOE_BLOCK - 1) // MOE_BLOCK * MOE_BLOCK
    pad_end = jnp.cumsum(padded)
    pad_start = pad_end - padded
    sort_start = jnp.cumsum(counts) - counts
    dest = pad_start[e_sorted] + jnp.arange(n_assign) - sort_start[e_sorted]
    n_blocks = (n_assign + N_EXPERTS * (MOE_BLOCK - 1) + MOE_BLOCK - 1) // MOE_BLOCK
    cap = n_blocks * MOE_BLOCK
    slot_tok = jnp.full((cap,), t, jnp.int32).at[dest].set(tok_id[order])
    slot_w = jnp.zeros((cap,), F32).at[dest].set(top_w.reshape(-1)[order])
    block_exp = jnp.minimum(jnp.searchsorted(pad_end, jnp.arange(n_blocks) * MOE_BLOCK, side="right"), N_EXPERTS - 1)
    x_pad = jnp.concatenate([x, jnp.zeros((1, d), x.dtype)], axis=0)
    xb = x_pad[slot_tok].reshape(n_blocks, MOE_BLOCK, d)

    def expert_block(args):
        xblk, e = args
        gate = xblk @ w_gate[e] + b_gate[e]
        up = xblk @ w_up[e] + b_up[e]
        return clamped_swiglu(gate, up) @ w_down[e] + b_down[e]

    yb = lax.map(expert_block, (xb, block_exp))
    out = jnp.zeros((t + 1, d), F32).at[slot_tok].add(yb.reshape(cap, d).astype(F32) * slot_w[:, None])
    return out[:t].astype(x.dtype)


def trunk_layer(x, xc, c, c_ctx, lb, w_mod, b_mod, norm1_w, norm2_w, w_in, hg_norm_w, na_q_norm_w,
                na_k_norm_w, na_rpb, ssd_conv_w, ssd_conv_b, ssd_dt_bias, ssd_a_log, ssd_d, ssd_norm_w,
                w_branch_hg, w_branch_na, w_branch_ssd, w_out, moe_w_router, moe_b_router, moe_w_gate,
                moe_b_gate, moe_w_up, moe_b_up, moe_w_down, moe_b_down, need_ctx):
    b, s, d = x.shape
    n_ctx = xc.shape[1]
    mod = jax.nn.silu(c) @ w_mod + b_mod
    mod_c = jax.nn.silu(c_ctx) @ w_mod + b_mod
    sh1, sc1, g1, sh2, sc2, g2 = jnp.split(mod[:, None, :], 6, axis=-1)
    csh1, csc1, cg1, csh2, csc2, cg2 = jnp.split(mod_c, 6, axis=-1)

    h = modulate(x, norm1_w, sh1, sc1)
    hc = modulate(xc, norm1_w, csh1, csc1)
    pl = split_in(h @ w_in)
    pc = split_in(hc @ (w_in if need_ctx else w_in[:, :N_MIX_COLS]))

    y_hg, yc_hg = hgrn2_mixer(pl[0:5], pc[0:5], lb, hg_norm_w, need_ctx)

    def na_qkv(parts):
        q, k, v = (t.reshape(t.shape[0], t.shape[1], NA_HEADS, NA_HD) for t in parts)
        return rms_norm(q, na_q_norm_w), rms_norm(k, na_k_norm_w), v

    ql, kl, vl = na_qkv(pl[5:8])
    qc, kc, vc = na_qkv(pc[5:8])
    y_na, yc_na = neighbourhood_attention(ql, kl, vl, qc, kc, vc, na_rpb, need_ctx)

    y_ssd, yc_ssd = ssd_mixer(pl[8:12], pc[8:12], ssd_conv_w, ssd_conv_b, ssd_dt_bias, ssd_a_log,
                              ssd_d, ssd_norm_w, need_ctx)

    def merge(gate_logits, ya, yb, yc_):
        ga, gb, gc = jnp.split(jax.nn.sigmoid(gate_logits), N_BRANCH, axis=-1)
        m = ga * (ya @ w_branch_hg) + gb * (yb @ w_branch_na) + gc * (yc_ @ w_branch_ssd)
        return m @ w_out

    moe = lambda tokens: moe_ffn(tokens, moe_w_router, moe_b_router, moe_w_gate, moe_b_gate,
                                 moe_w_up, moe_b_up, moe_w_down, moe_b_down)

    x = x + g1 * merge(pl[12], y_hg, y_na, y_ssd)
    h2 = modulate(x, norm2_w, sh2, sc2).reshape(b * s, d)
    if need_ctx:
        xc = xc + cg1 * merge(pc[12], yc_hg, yc_na, yc_ssd)
        h2c = modulate(xc, norm2_w, csh2, csc2).reshape(b * n_ctx, d)
        f = moe(jnp.concatenate([h2, h2c], axis=0))
        x = x + g2 * f[:b * s].reshape(b, s, d)
        xc = xc + cg2 * f[b * s:].reshape(b, n_ctx, d)
        return x, xc
    x = x + g2 * moe(h2).reshape(b, s, d)
    return x, None


def setup_inputs(seed: int = 0) -> dict:
    key = jax.random.key(seed)
    keys = iter(jax.random.split(key, 40))

    def normal(shape, scale):
        return jax.random.normal(next(keys), shape, F32) * scale

    def gain(shape):
        return 1.0 + normal(shape, 0.02)

    L, D = DEPTH, D_MODEL
    dt0 = jnp.exp(jax.random.uniform(next(keys), (L, 2, SSD_HEADS), F32, math.log(1e-3), math.log(1e-1)))
    a0 = jax.random.uniform(next(keys), (L, 2, SSD_HEADS), F32, 1.0, 16.0)
    return {
        "x": normal((BATCH, SEQ, D), 1.0),
        "c": normal((BATCH, D), 1.0),
        "ctx": normal((BATCH, CTX_LEN, D), 1.0),
        "c_ctx": normal((D,), 1.0),
        "w_mod": normal((L, D, 6 * D), 0.5 * D ** -0.5),
        "b_mod": normal((L, 6 * D), 0.01),
        "norm1_w": gain((L, D)),
        "norm2_w": gain((L, D)),
        "w_in": normal((L, D, N_IN), D ** -0.5),
        "hg_lb_logits": normal((L, 2, HG_WIDTH), 0.1),
        "hg_norm_w": gain((L, HG_DV)),
        "na_q_norm_w": gain((L, NA_HD)),
        "na_k_norm_w": gain((L, NA_HD)),
        "na_rpb": normal((L, NA_HEADS, 2 * NA_WIN_ROWS - 1, 2 * NA_WIN_COLS - 1), 0.02),
        "ssd_conv_w": normal((L, SSD_CONV, SSD_CONV_CH), SSD_CONV ** -0.5),
        "ssd_conv_b": normal((L, SSD_CONV_CH), 0.01),
        "ssd_dt_bias": dt0 + jnp.log(-jnp.expm1(-dt0)),
        "ssd_a_log": jnp.log(a0),
        "ssd_d": gain((L, SSD_HEADS)),
        "ssd_norm_w": gain((L, SSD_WIDTH)),
        "w_branch_hg": normal((L, HG_WIDTH, D), HG_WIDTH ** -0.5),
        "w_branch_na": normal((L, NA_WIDTH, D), NA_WIDTH ** -0.5),
        "w_branch_ssd": normal((L, SSD_WIDTH, D), SSD_WIDTH ** -0.5),
        "w_out": normal((L, D, D), D ** -0.5),
        "moe_w_router": normal((L, D, N_EXPERTS), D ** -0.5),
        "moe_b_router": normal((L, N_EXPERTS), 0.01),
        "moe_w_gate": normal((L, N_EXPERTS, D, D_FF), D ** -0.5),
        "moe_b_gate": normal((L, N_EXPERTS, D_FF), 0.01),
        "moe_w_up": normal((L, N_EXPERTS, D, D_FF), D ** -0.5),
        "moe_b_up": normal((L, N_EXPERTS, D_FF), 0.01),
        "moe_w_down": normal((L, N_EXPERTS, D_FF, D), D_FF ** -0.5),
        "moe_b_down": normal((L, N_EXPERTS, D), 0.01),
    }


def reference(x, c, ctx, c_ctx, w_mod, b_mod, norm1_w, norm2_w, w_in, hg_lb_logits, hg_norm_w,
              na_q_norm_w, na_k_norm_w, na_rpb, ssd_conv_w, ssd_conv_b, ssd_dt_bias, ssd_a_log, ssd_d,
              ssd_norm_w, w_branch_hg, w_branch_na, w_branch_ssd, w_out, moe_w_router, moe_b_router,
              moe_w_gate, moe_b_gate, moe_w_up, moe_b_up, moe_w_down, moe_b_down):
    lb_soft = jax.nn.softmax(hg_lb_logits.astype(F32), axis=0)
    lower_bounds = jnp.cumsum(lb_soft, axis=0) - lb_soft[0]
    stacked = (w_mod, b_mod, norm1_w, norm2_w, w_in, hg_norm_w, na_q_norm_w, na_k_norm_w, na_rpb,
               ssd_conv_w, ssd_conv_b, ssd_dt_bias, ssd_a_log, ssd_d, ssd_norm_w, w_branch_hg,
               w_branch_na, w_branch_ssd, w_out, moe_w_router, moe_b_router, moe_w_gate, moe_b_gate,
               moe_w_up, moe_b_up, moe_w_down, moe_b_down)
    h_lat, h_ctx = x, ctx
    for i in range(DEPTH):
        h_lat, h_ctx = trunk_layer(h_lat, h_ctx, c, c_ctx, lower_bounds[i], *[p[i] for p in stacked],
                                   need_ctx=(i < DEPTH - 1))
    return h_lat
```

```python
import numpy as np
from contextlib import ExitStack
import concourse.bass as bass
import concourse.mybir as mybir

F32 = mybir.dt.float32
BF16 = mybir.dt.bfloat16
AF = mybir.ActivationFunctionType
ALU = mybir.AluOpType
AX = mybir.AxisListType

COMPUTE = ("pe", "act", "dve", "pool")
ENGINES = ("pe", "act", "dve", "pool", "sp")
N_DMA_SEMS = 48


class Prog:
    def __init__(self, nc):
        self.nc = nc
        self.es = ExitStack()
        self.ops = {e: [] for e in ENGINES}
        self.count = {e: 0 for e in ENGINES}
        self.waited = {e: {} for e in ENGINES}
        self.state = {}
        self.desc = {}
        self.sems = {}
        for e in COMPUTE:
            self.sems[e] = self.es.enter_context(nc.semaphore("s_" + e))
        self.dma_sems = []
        self.dma_uses = []
        for i in range(N_DMA_SEMS):
            self.sems[("d", i)] = self.es.enter_context(nc.semaphore("s_d%d" % i))
            self.dma_uses.append(0)
        self.dma_rr = 0
        self.all_dma_tokens = []
        self.ntiles = 0

    def sb(self, shape, dtype, name=None):
        self.ntiles += 1
        name = "sb_" + (name or ("t%d" % self.ntiles))
        return self.es.enter_context(self.nc.sbuf_tensor(name, list(shape), dtype))

    def ps(self, shape, dtype, name=None):
        self.ntiles += 1
        name = "pp_" + (name or ("p%d" % self.ntiles))
        return self.es.enter_context(self.nc.psum_tensor(name, list(shape), dtype))

    def _conflicts(self, key):
        out = []
        for i in range(1, len(key) + 1):
            st = self.state.get(key[:i])
            if st is not None:
                out.append(st)
        for k in self.desc.get(key, ()):
            st = self.state.get(k)
            if st is not None:
                out.append(st)
        return out

    def _touch(self, key):
        if key not in self.state:
            self.state[key] = [None, {}]
            for i in range(1, len(key)):
                self.desc.setdefault(key[:i], set()).add(key)
        return self.state[key]

    def op(self, engine, fn, reads=(), writes=(), dma=False):
        reads = [k if isinstance(k, tuple) else (k,) for k in reads]
        writes = [k if isinstance(k, tuple) else (k,) for k in writes]
        for k in list(reads):
            if isinstance(k[0], str) and (k[0].startswith("ps") or k[0].startswith("pt")) and k not in writes:
                writes.append(k)
        reads = [k for k in reads if k not in writes]
        deps = {}

        def add(tok, is_writer):
            semkey, val, eng = tok
            if not dma and eng == engine and not isinstance(semkey, tuple):
                if engine == "pe":
                    return
                if not is_writer:
                    return
            if deps.get(semkey, 0) < val:
                deps[semkey] = val

        for k in reads:
            for st in self._conflicts(k):
                if st[0] is not None:
                    add(st[0], True)
        for k in writes:
            for st in self._conflicts(k):
                if st[0] is not None:
                    add(st[0], True)
                for sk, (v, e) in st[1].items():
                    add((sk, v, e), False)
        if dma:
            i = self.dma_rr
            self.dma_rr = (self.dma_rr + 1) % N_DMA_SEMS
            prev = self.dma_uses[i]
            if prev > 0:
                if deps.get(("d", i), 0) < 16 * prev:
                    deps[("d", i)] = 16 * prev
            self.dma_uses[i] = prev + 1
            tok = (("d", i), 16 * (prev + 1), engine)
            inc = 16
            self.all_dma_tokens.append(tok)
        else:
            self.count[engine] += 1
            tok = (engine, self.count[engine], engine)
            inc = 1
        waits = []
        w = self.waited[engine]
        for sk, v in deps.items():
            if w.get(sk, 0) < v:
                w[sk] = v
                waits.append((sk, v))
        self.ops[engine].append((fn, waits, tok[0], inc))
        for k in reads:
            st = self._touch(k)
            st[1][tok[0]] = (tok[1], engine)
        for k in writes:
            st = self._touch(k)
            for kk in list(self.desc.get(k, ())):
                self.state.pop(kk, None)
            st[0] = tok
            st[1] = {}
        return tok

    def dma(self, out, in_, reads=(), writes=(), q="sp", **kw):
        return self.op(q, lambda eng: eng.dma_start(out=out, in_=in_, **kw), reads, writes, dma=True)

    def finish(self):
        w = self.waited["sp"]
        waits = []
        for i in range(N_DMA_SEMS):
            v = 16 * self.dma_uses[i]
            if v > 0 and w.get(("d", i), 0) < v:
                waits.append((("d", i), v))
                w[("d", i)] = v
        self.ops["sp"].append((None, waits, None, 0))

    def emit(self):
        nc = self.nc
        self.finish()
        engmap = {"pe": "tensor", "act": "scalar", "dve": "vector", "pool": "gpsimd", "sp": "sync"}
        with nc.Block() as block:
            for e in ENGINES:
                ops = self.ops[e]
                if not ops:
                    continue

                def body(eng, ops=ops):
                    for fn, waits, semkey, inc in ops:
                        for sk, v in waits:
                            eng.wait_ge(self.sems[sk], v)
                        if fn is not None:
                            ins = fn(eng)
                            ins.then_inc(self.sems[semkey], inc)

                getattr(block, engmap[e])(body)
        self.es.close()
D = 2048
KC = 16
EPS = 1e-6
NE = 32


def v16(buf):
    return buf[:, :].rearrange("p (k n) -> p k n", k=KC)


def wblock(dram2d, c0, w=512):
    return dram2d.rearrange("(k p) n -> p k n", p=128)[:, :, c0:c0 + w]


def run_stream(p, ring, blocks):
    R = len(ring)

    def load(j):
        ap, vf, _ = blocks[j]
        p.dma(vf(ring[j % R]), ap, writes=[("ring", j % R)], q="pool")

    for j in range(min(R, len(blocks))):
        load(j)
    for j, (ap, vf, use) in enumerate(blocks):
        use(ring[j % R], ("ring", j % R))
        if j + R < len(blocks):
            load(j + R)


def emit_modcols(p, blocks, wmod_d, QS, scT, bmodT, modc, PS):
    for qi, q in enumerate(QS):
        for cb in range(4):
            def use(buf, key, qi=qi, q=q, cb=cb):
                bv = v16(buf)
                ps = PS[(qi * 4 + cb) % 2]
                pk = "ps%d" % ((qi * 4 + cb) % 2)
                for j in range(4):
                    for kc in range(KC):
                        p.op("pe", lambda e, kc=kc, j=j: e.matmul(ps[:, j * 2:j * 2 + 2], lhsT=bv[:, kc, j * 128:(j + 1) * 128],
                                                               rhs=scT[:, kc, :], start=(kc == 0), stop=(kc == KC - 1)),
                             reads=[key, "scT"], writes=[pk])
                for j in range(4):
                    dc = cb * 4 + j
                    p.op("dve", lambda e, j=j, dc=dc: e.tensor_scalar(modc[:, qi, dc, :], ps[:, j * 2:j * 2 + 2],
                                                                     bmodT[:, q * 16 + dc: q * 16 + dc + 1], None, ALU.add),
                         reads=[pk, "bmodT"], writes=[("modc", qi)])
            blocks.append((wblock(wmod_d, q * D + cb * 512), v16, use))


def finish_modc(p, modc, pairs):
    for qi, nT, nk in pairs:
        for v in range(2):
            p.op("dve", lambda e, qi=qi, v=v: e.tensor_scalar(modc[:, qi, :, v], modc[:, qi, :, v], 1.0, float(np.sqrt(D)), ALU.add, ALU.mult),
                 reads=[("modc", qi)], writes=[("modc", qi)])
            p.op("dve", lambda e, qi=qi, v=v, nT=nT: e.tensor_tensor(modc[:, qi, :, v], modc[:, qi, :, v], nT[:], ALU.mult),
                 reads=[("modc", qi), nk], writes=[("modc", qi)])


def emit_gblocks(p, blocks, wmod_d, q, Gt, gname, scT, onesb, bmodb, bi, PS):
    for cb in range(4):
        def use(buf, key, cb=cb):
            bv = v16(buf)
            for v in range(2):
                ps = PS[2 + v]; pk = "ps%d" % (2 + v)
                for kc in range(KC):
                    p.op("pe", lambda e, kc=kc, v=v, ps=ps: e.matmul(ps[:, :], lhsT=scT[:, kc, v:v + 1].to_broadcast([128, 128]), rhs=bv[:, kc, :],
                                                                  start=(kc == 0), stop=False), reads=[key, "scT"], writes=[pk])
                p.op("pe", lambda e, v=v, ps=ps: e.matmul(ps[:, :], lhsT=onesb[0:1, :], rhs=bmodb[0:1, bi, cb * 512:(cb + 1) * 512],
                                                       start=False, stop=True), reads=["onesb", "bmodb"], writes=[pk])
                p.op("act", lambda e, v=v, ps=ps: e.copy(Gt[v][:, cb * 512:(cb + 1) * 512], ps[:, :]), reads=[pk], writes=[(gname, v)])
        blocks.append((wblock(wmod_d, q * D + cb * 512), v16, use))


def norm_to_T(p, xtile, xkey, modc, qi_sh, qi_A, v, dstT, dkey, col0, small, xn, identb, PT, epsc):
    ss = small[:, 0:1]; rstd = small[:, 1:2]
    p.op("act", lambda e: e.activation(xn[1][:], xtile, AF.Square, accum_out=ss), reads=[xkey], writes=["xn1", ("small", 0)])
    p.op("act", lambda e: e.activation(small[:, 2:3], ss, AF.Ln, bias=epsc[:, 0:1], scale=1.0), reads=[("small", 0), "epsc"], writes=[("small", 2)])
    p.op("act", lambda e: e.activation(rstd, small[:, 2:3], AF.Exp, scale=-0.5), reads=[("small", 2)], writes=[("small", 1)])
    xb = xn[0]
    p.op("dve", lambda e: e.tensor_scalar(xb[:], xtile, rstd, None, ALU.mult), reads=[xkey, ("small", 1)], writes=["xn0"])
    for hh in range(2):
        for j in range(8):
            kc = hh * 8 + j
            p.op("pe", lambda e, kc=kc, j=j, hh=hh: e.transpose(PT[hh][:, j * 128:(j + 1) * 128], xb[:, kc * 128:(kc + 1) * 128], identb[:]),
                 reads=["xn0", "identb"], writes=["pt%d" % hh])
        for j in range(8):
            kc = hh * 8 + j
            p.op("act", lambda e, kc=kc, j=j, hh=hh: e.activation(dstT[:, kc, col0:col0 + 128], PT[hh][:, j * 128:(j + 1) * 128], AF.Identity,
                                                                  bias=modc[:, qi_sh, kc, v:v + 1], scale=modc[:, qi_A, kc, v:v + 1]),
                 reads=["pt%d" % hh, ("modc", qi_sh), ("modc", qi_A)], writes=[dkey])


def build_B1(NT, n_a, TBS):
    T = NT * 128
    nc = bass.Bass("TRN2", target_bir_lowering=False)

    def din(name, shape, dt=F32):
        return nc.dram_tensor(name, list(shape), dt, kind="ExternalInput").ap()

    x_d = din("x", [T, D]); yT_d = din("yT", [D, T], BF16)
    modc_d = din("modc4", [128, 4, KC, 2]); g1_d = din("G1", [128, 2, D])
    n1T_d = din("n1T", [128, KC]); n2T_d = din("n2T", [128, KC])
    wgi_d = din("wgi", [D, 3 * D]); wb_d = din("wb", [D, D]); wo_d = din("wo", [D, D])
    wr_d = din("wr", [D, NE]); br_d = din("br", [1, NE])
    ident_d = din("ident", [128, 128])
    xmid_d = nc.dram_tensor("xmid", [T, D], F32, kind="ExternalOutput").ap()
    h2T_d = nc.dram_tensor("h2T", [D, T], BF16, kind="ExternalOutput").ap()
    rw_d = nc.dram_tensor("rw", [T, NE], F32, kind="ExternalOutput").ap()

    p = Prog(nc)
    RING = 3
    ring = [p.sb([128, 8192], BF16, "ring%d" % i) for i in range(RING)]
    identb = p.sb([128, 128], BF16, "identb")
    onesb = p.sb([1, 128], BF16, "onesb")
    n1T = p.sb([128, KC], F32, "n1T"); n2T = p.sb([128, KC], F32, "n2T")
    modc = p.sb([128, 4, KC, 2], F32, "modc")
    wrb = p.sb([128, KC, NE], BF16, "wrb"); brb = p.sb([1, NE], BF16, "brb")
    small = p.sb([128, 64], F32, "small")
    epsc = p.sb([128, 1], F32, "epsc")
    p.op("dve", lambda e: e.memset(epsc[:], float(D * EPS)), writes=["epsc"])
    xn = [p.sb([128, D], BF16, "xn%d" % i) for i in range(2)]
    G1 = [p.sb([128, D], F32, "G1_%d" % i) for i in range(2)]
    hT_blk = p.sb([128, KC, 384], BF16, "hT_blk")
    yT_blk = p.sb([128, KC, 384], BF16, "yT_blk")
    mT_blk = p.sb([128, KC, 384], BF16, "mT_blk")
    h2T_blk = p.sb([128, KC, 384], BF16, "h2T_blk")
    gsig = p.sb([128, 3, 4, 384], F32, "gsig")
    xres = p.sb([128, 3, D], F32, "xres"); xmid = p.sb([128, 3, D], F32, "xmid")
    macc = p.sb([128, 384], F32, "macc"); mtmp = p.sb([128, 384], F32, "mtmp")
    lg = p.sb([128, NE], F32, "lg"); mx8 = p.sb([128, 8], F32, "mx8"); rwt = p.sb([128, 3, NE], F32, "rwt"); msk = p.sb([128, NE], F32, "msk")
    PS = [p.ps([128, 512], F32, "ps%d" % i) for i in range(6)]
    PT = [p.ps([128, 1024], BF16, "pt%d" % i) for i in range(2)]

    p.dma(identb[:], ident_d, writes=["identb"], q="pool")
    p.op("dve", lambda e: e.memset(onesb[:], 1.0), writes=["onesb"])
    p.dma(modc[:], modc_d, writes=["modc"])
    for v in range(2):
        p.dma(G1[v][:], g1_d[:, v, :], writes=[("G1", v)])
    p.dma(n1T[:], n1T_d, writes=["n1T"]); p.dma(n2T[:], n2T_d, writes=["n2T"])
    p.dma(wrb[:], wr_d.rearrange("(k p) n -> p k n", p=128), writes=["wrb"], q="pool")
    p.dma(brb[:], br_d, writes=["brb"], q="pool")

    blocks = []

    BR = [(0, 6), (6, 4), (10, 6)]
    t0 = 0
    for bi, nb in enumerate(TBS):
        TB = nb * 128
        tiles = list(range(t0, t0 + nb))
        c0 = t0 * 128

        def pre(bi=bi, nb=nb, TB=TB, tiles=tiles, c0=c0):
            if bi == 0:
                finish_modc(p, modc, [(1, n1T, "n1T"), (3, n2T, "n2T")])
            p.dma(yT_blk[:, :, 0:TB], yT_d.rearrange("(k p) t -> p k t", p=128)[:, :, c0:c0 + TB], writes=["yT_blk"])
            for li, ti in enumerate(tiles):
                v = 0 if ti < n_a else 1
                p.dma(xres[:, li, :], x_d[ti * 128:(ti + 1) * 128, :], writes=[("xres", li)])
                norm_to_T(p, xres[:, li, :], ("xres", li), modc, 0, 1, v, hT_blk, "hT_blk", li * 128, small, xn, identb, PT, epsc)

        for cb in range(4):
            for br in range(3):
                def use(buf, key, bi=bi, cb=cb, br=br, TB=TB, pre=pre):
                    if cb == 0 and br == 0:
                        pre()
                    bv = v16(buf)
                    for j in range(4):
                        ps = PS[j % 2]; pk = "ps%d" % (j % 2)
                        for kc in range(KC):
                            p.op("pe", lambda e, kc=kc, j=j, ps=ps: e.matmul(ps[:, 0:TB], lhsT=bv[:, kc, j * 128:(j + 1) * 128], rhs=hT_blk[:, kc, 0:TB],
                                                                          start=(kc == 0), stop=(kc == KC - 1)), reads=[key, "hT_blk"], writes=[pk])
                        p.op("act", lambda e, j=j, ps=ps, br=br: e.activation(gsig[:, br, j, 0:TB], ps[:, 0:TB], AF.Sigmoid), reads=[pk], writes=[("gsig", br, j)])
                blocks.append((wblock(wgi_d, br * D + cb * 512), v16, use))

            def use(buf, key, bi=bi, cb=cb, TB=TB):
                bv = v16(buf)
                for j in range(4):
                    dc = cb * 4 + j
                    for br in range(3):
                        k0, nk = BR[br]
                        ps = PS[2 + (br % 2)]; pk = "ps%d" % (2 + (br % 2))
                        for kk in range(nk):
                            p.op("pe", lambda e, kk=kk, k0=k0, nk=nk, j=j, ps=ps: e.matmul(ps[:, 0:TB], lhsT=bv[:, k0 + kk, j * 128:(j + 1) * 128], rhs=yT_blk[:, k0 + kk, 0:TB],
                                                                                      start=(kk == 0), stop=(kk == nk - 1)), reads=[key, "yT_blk"], writes=[pk])
                        if br == 0:
                            p.op("dve", lambda e, j=j, ps=ps: e.tensor_tensor(macc[:, 0:TB], ps[:, 0:TB], gsig[:, 0, j, 0:TB], ALU.mult), reads=[pk, ("gsig", 0, j)], writes=["macc"])
                        else:
                            p.op("dve", lambda e, j=j, ps=ps, br=br: e.tensor_tensor(mtmp[:, 0:TB], ps[:, 0:TB], gsig[:, br, j, 0:TB], ALU.mult), reads=[pk, ("gsig", br, j)], writes=["mtmp"])
                            if br == 1:
                                p.op("dve", lambda e: e.tensor_tensor(macc[:, 0:TB], macc[:, 0:TB], mtmp[:, 0:TB], ALU.add), reads=["macc", "mtmp"], writes=["macc"])
                            else:
                                p.op("dve", lambda e, dc=dc: e.tensor_tensor(mT_blk[:, dc, 0:TB], macc[:, 0:TB], mtmp[:, 0:TB], ALU.add), reads=["macc", "mtmp"], writes=["mT_blk"])
            blocks.append((wblock(wb_d, cb * 512), v16, use))

        for cb in range(4):
            def use(buf, key, bi=bi, cb=cb, nb=nb, tiles=tiles, TB=TB, c0=c0):
                bv = v16(buf)
                for li, ti in enumerate(tiles):
                    v = 0 if ti < n_a else 1
                    ps = PS[4 + (li % 2)]; pk = "ps%d" % (4 + (li % 2))
                    for kc in range(KC):
                        p.op("pe", lambda e, kc=kc, li=li, ps=ps: e.matmul(ps[:, :], lhsT=mT_blk[:, kc, li * 128:(li + 1) * 128], rhs=bv[:, kc, :],
                                                                         start=(kc == 0), stop=(kc == KC - 1)), reads=[key, "mT_blk"], writes=[pk])
                    xm = xmid[:, li, cb * 512:(cb + 1) * 512]
                    p.op("dve", lambda e, ps=ps, v=v, xm=xm: e.tensor_tensor(xm, ps[:, :], G1[v][:, cb * 512:(cb + 1) * 512], ALU.mult), reads=[pk, ("G1", v)], writes=[("xmid", li, cb)])
                    p.op("pool", lambda e, xm=xm, li=li: e.tensor_tensor(xm, xm, xres[:, li, cb * 512:(cb + 1) * 512], ALU.add), reads=[("xmid", li, cb), ("xres", li)], writes=[("xmid", li, cb)])
                if cb == 3:
                    for li, ti in enumerate(tiles):
                        v = 0 if ti < n_a else 1
                        p.dma(xmid_d[ti * 128:(ti + 1) * 128, :], xmid[:, li, :], reads=[("xmid", li)])
                        norm_to_T(p, xmid[:, li, :], ("xmid", li), modc, 2, 3, v, h2T_blk, "h2T_blk", li * 128, small, xn, identb, PT, epsc)
                    p.dma(h2T_d.rearrange("(k p) t -> p k t", p=128)[:, :, c0:c0 + TB], h2T_blk[:, :, 0:TB], reads=["h2T_blk"])
                    for li, ti in enumerate(tiles):
                        ps = PS[li % 2]; pk = "ps%d" % (li % 2)
                        for kc in range(KC):
                            p.op("pe", lambda e, kc=kc, li=li, ps=ps: e.matmul(ps[:, 0:NE], lhsT=h2T_blk[:, kc, li * 128:(li + 1) * 128], rhs=wrb[:, kc, :],
                                                                             start=(kc == 0), stop=False), reads=["h2T_blk", "wrb"], writes=[pk])
                        p.op("pe", lambda e, ps=ps: e.matmul(ps[:, 0:NE], lhsT=onesb[0:1, :], rhs=brb[0:1, :], start=False, stop=True), reads=["onesb", "brb"], writes=[pk])
                        p.op("dve", lambda e, ps=ps: e.tensor_copy(lg[:], ps[:, 0:NE]), reads=[pk], writes=["lg"])
                        p.op("dve", lambda e: e.max(out=mx8[:], in_=lg[:]), reads=["lg"], writes=["mx8"])
                        p.op("dve", lambda e: e.tensor_scalar(msk[:], lg[:], mx8[:, 3:4], None, ALU.is_ge), reads=["lg", "mx8"], writes=["msk"])
                        p.op("dve", lambda e: e.tensor_scalar(small[:, 4:5], mx8[:, 0:1], -1.0, None, ALU.mult), reads=["mx8"], writes=[("small", 4)])
                        p.op("act", lambda e: e.activation(lg[:], lg[:], AF.Exp, bias=small[:, 4:5], scale=1.0), reads=["lg", ("small", 4)], writes=["lg"])
                        p.op("dve", lambda e: e.tensor_tensor(lg[:], lg[:], msk[:], ALU.mult), reads=["lg", "msk"], writes=["lg"])
                        p.op("dve", lambda e: e.reduce_sum(small[:, 5:6], lg[:], axis=AX.X), reads=["lg"], writes=[("small", 5)])
                        p.op("dve", lambda e: e.reciprocal(small[:, 6:7], small[:, 5:6]), reads=[("small", 5)], writes=[("small", 6)])
                        p.op("dve", lambda e, li=li: e.tensor_scalar(rwt[:, li, :], lg[:], small[:, 6:7], None, ALU.mult), reads=["lg", ("small", 6)], writes=[("rwt", li)])
                        p.dma(rw_d[ti * 128:(ti + 1) * 128, :], rwt[:, li, :], reads=[("rwt", li)])
            blocks.append((wblock(wo_d, cb * 512), v16, use))
        t0 += nb
    run_stream(p, ring, blocks)
    p.emit()
    return nc
MCOLS = 6 * D // 8


def build_M():
    nc = bass.Bass("TRN2", target_bir_lowering=False)

    def din(name, shape, dt=F32):
        return nc.dram_tensor(name, list(shape), dt, kind="ExternalInput").ap()
    cT_d = din("cT5", [128, KC, 5]); w_d = din("w_mod", [2, D, MCOLS]); b_d = din("b_mod", [2, 1, MCOLS])
    out_d = nc.dram_tensor("mod", [2, 5, MCOLS], F32, kind="ExternalOutput").ap()
    p = Prog(nc)
    ring = [p.sb([128, 8192], BF16, "ring%d" % i) for i in range(3)]
    cT = p.sb([128, KC, 5], F32, "cT"); scT = p.sb([128, KC, 5], BF16, "scT")
    onesb = p.sb([1, 8], BF16, "onesb"); bb = p.sb([1, 2, MCOLS], BF16, "bb")
    res = p.sb([5, 2, MCOLS], F32, "res")
    PS = [p.ps([128, 512], F32, "ps%d" % i) for i in range(2)]
    p.op("dve", lambda e: e.memset(onesb[:], 1.0), writes=["onesb"])
    p.dma(cT[:], cT_d, writes=["cT"])
    p.op("act", lambda e: e.activation(scT[:], cT[:], AF.Silu), reads=["cT"], writes=["scT"])
    for l in range(2):
        p.dma(bb[:, l, :], b_d[l], writes=["bb"], q="pool")
    blocks = []
    for l in range(2):
        for cb in range(3):
            def use(buf, key, l=l, cb=cb):
                bv = v16(buf)
                ps = PS[(l * 3 + cb) % 2]; pk = "ps%d" % ((l * 3 + cb) % 2)
                for kc in range(KC):
                    p.op("pe", lambda e, kc=kc, ps=ps: e.matmul(ps[0:5, :], lhsT=scT[:, kc, :], rhs=bv[:, kc, :], start=(kc == 0), stop=False), reads=[key, "scT"], writes=[pk])
                p.op("pe", lambda e, ps=ps: e.matmul(ps[0:5, :], lhsT=onesb[0:1, 0:5], rhs=bb[0:1, l, cb * 512:(cb + 1) * 512], start=False, stop=True), reads=["onesb", "bb"], writes=[pk])
                p.op("act", lambda e, ps=ps: e.copy(res[:, l, cb * 512:(cb + 1) * 512], ps[0:5, :]), reads=[pk], writes=["res"])
            blocks.append((wblock(w_d[l], cb * 512), v16, use))
    run_stream(p, ring, blocks)
    for l in range(2):
        p.dma(out_d[l], res[:, l, :], reads=["res"])
    p.emit()
    return nc


FB = 256
NFB = D // FB
SW_ALPHA = 1.702
SW_LIM = 7.0
NEL = 4
NCHUNK = 8
CT_ = 1152


def build_B2x():
    NT = 9
    TALL = NCHUNK * CT_
    nc = bass.Bass("TRN2", target_bir_lowering=False)

    def din(name, shape, dt=F32):
        return nc.dram_tensor(name, list(shape), dt, kind="ExternalInput").ap()
    h2T_d = din("h2T", [D, TALL], BF16); rw_d = din("rw", [TALL, NEL]); rwT_d = din("rwT", [NEL, TALL])
    wg_d = din("wg", [NEL, D, D]); wu_d = din("wu", [NEL, D, D]); wd_d = din("wd", [NEL, D, D])
    bgT_d = din("bgT", [128, NEL, KC]); buT_d = din("buT", [128, NEL, KC]); bd_d = din("bd", [NEL, D])
    out_d = nc.dram_tensor("part", [TALL, D], F32, kind="ExternalOutput").ap()

    p = Prog(nc)
    acc = p.sb([128, NT, D], F32, "acc")
    h2T = p.sb([128, KC, CT_], BF16, "h2T")
    ring = [p.sb([128, 8192], BF16, "ring%d" % i) for i in range(3)]
    actT = [p.sb([128, 2, CT_], BF16, "actT%d" % i) for i in range(2)]
    tg = [p.sb([128, 384], F32, "tg%d" % i) for i in range(2)]
    tsg = [p.sb([128, 384], F32, "tsg%d" % i) for i in range(2)]
    tu = [p.sb([128, 384], F32, "tu%d" % i) for i in range(2)]
    bgT = p.sb([128, NEL, KC], F32, "bgT"); buT = p.sb([128, NEL, KC], F32, "buT")
    rw = p.sb([128, NCHUNK * NT, NEL], F32, "rw"); rwT = p.sb([NEL, CT_], F32, "rwT")
    bdf = p.sb([NEL, D], F32, "bdf")
    PS = [p.ps([128, 512], F32, "ps%d" % i) for i in range(8)]
    p.dma(bgT[:], bgT_d, writes=["bgT"]); p.dma(buT[:], buT_d, writes=["buT"])
    p.dma(rw[:], rw_d.rearrange("(n p) e -> p n e", p=128), writes=["rw"])
    p.dma(bdf[:], bd_d, writes=["bdf"])
    tbs = [(0, 384), (384, 384), (768, 384)]

    def gu_view(buf):
        return buf[:, :].rearrange("p (g k n) -> p g k n", g=2, k=KC)

    def d_view(buf):
        return buf[:, 0:2 * D].rearrange("p (k n) -> p k n", k=2)

    blocks = []
    unit = [0]
    for ck in range(NCHUNK):
        tok0 = ck * CT_

        def chunk_pre(ck=ck, tok0=tok0):
            for kc in range(KC):
                p.dma(h2T[:, kc, :], h2T_d[kc * 128:(kc + 1) * 128, tok0:tok0 + CT_], writes=[("h2T", kc)])
            p.dma(rwT[:], rwT_d[:, tok0:tok0 + CT_], writes=["rwT"])
            n = 0
            for ti in range(NT):
                for cb in range(4):
                    ps = PS[4 + (n % 4)]; pk = "ps%d" % (4 + (n % 4)); n += 1
                    p.op("pe", lambda e, ti=ti, ps=ps, cb=cb: e.matmul(ps[:, :], lhsT=rwT[:, ti * 128:(ti + 1) * 128], rhs=bdf[:, cb * 512:(cb + 1) * 512], start=True, stop=True),
                         reads=["rwT", "bdf"], writes=[pk])
                    p.op("act", lambda e, ti=ti, ps=ps, cb=cb: e.copy(acc[:, ti, cb * 512:(cb + 1) * 512], ps[:, :]), reads=[pk], writes=[("acc", ti, cb)])

        for ex in range(NEL):
            for fb in range(NFB):
                first = (ex == 0 and fb == 0)
                last = (ex == NEL - 1 and fb == NFB - 1)

                def use_gu(buf, key, ex=ex, fb=fb, first=first, chunk_pre=chunk_pre):
                    if first:
                        chunk_pre()
                    bv = gu_view(buf)
                    at = actT[(ex * NFB + fb) % 2]; ak = "actT%d" % ((ex * NFB + fb) % 2)
                    for j in range(2):
                        fc = fb * 2 + j
                        for (c0, TB) in tbs:
                            u = unit[0]; unit[0] += 1
                            pg = PS[u % 2]; pgk = "ps%d" % (u % 2)
                            pu = PS[2 + u % 2]; puk = "ps%d" % (2 + u % 2)
                            for kc in range(KC):
                                p.op("pe", lambda e, kc=kc, j=j, pg=pg, c0=c0, TB=TB: e.matmul(pg[:, 0:TB], lhsT=bv[:, 0, kc, j * 128:(j + 1) * 128], rhs=h2T[:, kc, c0:c0 + TB],
                                                                                             start=(kc == 0), stop=(kc == KC - 1)), reads=[key, ("h2T", kc)], writes=[pgk])
                            for kc in range(KC):
                                p.op("pe", lambda e, kc=kc, j=j, pu=pu, c0=c0, TB=TB: e.matmul(pu[:, 0:TB], lhsT=bv[:, 1, kc, j * 128:(j + 1) * 128], rhs=h2T[:, kc, c0:c0 + TB],
                                                                                             start=(kc == 0), stop=(kc == KC - 1)), reads=[key, ("h2T", kc)], writes=[puk])
                            s = u % 2
                            g_, sg_, u_ = tg[s], tsg[s], tu[s]
                            p.op("dve", lambda e, pg=pg, g_=g_, TB=TB, fc=fc: e.tensor_scalar(g_[:, 0:TB], pg[:, 0:TB], bgT[:, ex, fc:fc + 1], SW_LIM, ALU.add, ALU.min),
                                 reads=[pgk, "bgT"], writes=["tg%d" % s])
                            p.op("act", lambda e, g_=g_, sg_=sg_, TB=TB: e.activation(sg_[:, 0:TB], g_[:, 0:TB], AF.Sigmoid, scale=SW_ALPHA), reads=["tg%d" % s], writes=["tsg%d" % s])
                            p.op("dve", lambda e, pu=pu, u_=u_, TB=TB, fc=fc: e.tensor_scalar(u_[:, 0:TB], pu[:, 0:TB], buT[:, ex, fc:fc + 1], SW_LIM, ALU.add, ALU.min),
                                 reads=[puk, "buT"], writes=["tu%d" % s])
                            p.op("dve", lambda e, u_=u_, TB=TB: e.tensor_scalar(u_[:, 0:TB], u_[:, 0:TB], -SW_LIM, 1.0, ALU.max, ALU.add), reads=["tu%d" % s], writes=["tu%d" % s])
                            p.op("pool", lambda e, g_=g_, sg_=sg_, TB=TB: e.tensor_tensor(sg_[:, 0:TB], g_[:, 0:TB], sg_[:, 0:TB], ALU.mult), reads=["tg%d" % s, "tsg%d" % s], writes=["tsg%d" % s])
                            p.op("pool", lambda e, u_=u_, sg_=sg_, TB=TB, at=at, j=j, c0=c0: e.tensor_tensor(at[:, j, c0:c0 + TB], sg_[:, 0:TB], u_[:, 0:TB], ALU.mult),
                                 reads=["tsg%d" % s, "tu%d" % s], writes=[(ak, j, c0)])
                gcols = slice(fb * FB, (fb + 1) * FB)
                blocks.append(([(wg_d[ex].rearrange("(k p) n -> p k n", p=128)[:, :, gcols], lambda buf: gu_view(buf)[:, 0]),
                                (wu_d[ex].rearrange("(k p) n -> p k n", p=128)[:, :, gcols], lambda buf: gu_view(buf)[:, 1])], use_gu))

                def use_d(buf, key, ex=ex, fb=fb, last=last, ck=ck, tok0=tok0):
                    bv = d_view(buf)
                    at = actT[(ex * NFB + fb) % 2]; ak = "actT%d" % ((ex * NFB + fb) % 2)
                    n = 0
                    for ti in range(NT):
                        for cb in range(4):
                            ps = PS[4 + (n % 4)]; pk = "ps%d" % (4 + (n % 4)); n += 1
                            for j in range(2):
                                p.op("pe", lambda e, j=j, ti=ti, cb=cb, ps=ps: e.matmul(ps[:, :], lhsT=at[:, j, ti * 128:(ti + 1) * 128], rhs=bv[:, j, cb * 512:(cb + 1) * 512],
                                                                                      start=(j == 0), stop=(j == 1)), reads=[key, ak], writes=[pk])
                            a = acc[:, ti, cb * 512:(cb + 1) * 512]
                            p.op("dve", lambda e, a=a, ps=ps, ti=ti: e.scalar_tensor_tensor(out=a, in0=ps[:, :], scalar=rw[:, ck * NT + ti, ex:ex + 1], in1=a, op0=ALU.mult, op1=ALU.add),
                                 reads=[pk, "rw", ("acc", ti, cb)], writes=[("acc", ti, cb)])
                        if last:
                            p.dma(out_d[tok0 + ti * 128: tok0 + (ti + 1) * 128, :], acc[:, ti, :], reads=[("acc", ti)])
                blocks.append(([(wd_d[ex].rearrange("(k p) n -> p k n", p=128)[:, fb * 2:(fb + 1) * 2, :], d_view)], use_d))
    run_stream2(p, ring, blocks)
    p.emit()
    return nc


def run_stream2(p, ring, blocks):
    R = len(ring)

    def load(j):
        for i, (ap, vf) in enumerate(blocks[j][0]):
            p.dma(vf(ring[j % R]), ap, writes=[("ring", j % R, i)], q="pool")

    for j in range(min(R, len(blocks))):
        load(j)
    for j, (_, use) in enumerate(blocks):
        use(ring[j % R], ("ring", j % R))
        if j + R < len(blocks):
            load(j + R)


def build_B3():
    NT = 9
    T = NT * 128
    nc = bass.Bass("TRN2", target_bir_lowering=False)

    def din(name, shape, dt=F32):
        return nc.dram_tensor(name, list(shape), dt, kind="ExternalInput").ap()
    parts_d = din("parts", [8, T, D]); xmid_d = din("xmid", [T, D]); g2_d = din("G2", [128, 2, D])
    out_d = nc.dram_tensor("out", [T, D], F32, kind="ExternalOutput").ap()
    p = Prog(nc)
    G2 = p.sb([128, 2, D], F32, "G2")
    pt = [p.sb([128, 8, D], F32, "pt%da" % i) for i in range(2)]
    xm = [p.sb([128, D], F32, "xm%d" % i) for i in range(2)]
    p.dma(G2[:], g2_d, writes=["G2"])
    for ti in range(NT):
        v = 0 if ti < 2 else 1
        s = ti % 2
        for k in range(8):
            p.dma(pt[s][:, k, :], parts_d[k, ti * 128:(ti + 1) * 128, :], writes=[("pp%d" % s, k)], q=("sp" if k % 2 == 0 else "act"))
        p.dma(xm[s][:], xmid_d[ti * 128:(ti + 1) * 128, :], writes=["xm%d" % s])
        for k in range(1, 8):
            eng = "dve" if k % 2 == 1 else "pool"
            p.op("dve", lambda e, s=s, k=k: e.tensor_tensor(pt[s][:, 0, :], pt[s][:, 0, :], pt[s][:, k, :], ALU.add), reads=[("pp%d" % s, 0), ("pp%d" % s, k)], writes=[("pp%d" % s, 0)])
        p.op("dve", lambda e, s=s, v=v: e.tensor_tensor(pt[s][:, 0, :], pt[s][:, 0, :], G2[:, v, :], ALU.mult), reads=[("pp%d" % s, 0), "G2"], writes=[("pp%d" % s, 0)])
        p.op("pool", lambda e, s=s: e.tensor_tensor(pt[s][:, 0, :], pt[s][:, 0, :], xm[s][:], ALU.add), reads=[("pp%d" % s, 0), "xm%d" % s], writes=[("pp%d" % s, 0)])
        p.dma(out_d[ti * 128:(ti + 1) * 128, :], pt[s][:, 0, :], reads=[("pp%d" % s, 0)])
    p.emit()
    return nc
TOK = 2304
NTA = 18
HD = 128


def load_hT(p, x_d, modc, hT, small, xn, identb, PT, epsc, xt):
    for ti in range(NTA):
        v = 0 if ti < 2 else 1
        xtl = xt[ti % len(xt)]; xk = "xt%d" % (ti % len(xt))
        p.dma(xtl, x_d[ti * 128:(ti + 1) * 128, :], writes=[xk])
        norm_to_T(p, xtl, xk, modc, 0, 1, v, hT, ("hT", ti), ti * 128, small, xn, identb, PT, epsc)


def a_common(nc, p, xt=None, xn=None):
    def din(name, shape, dt=F32):
        return nc.dram_tensor(name, list(shape), dt, kind="ExternalInput").ap()
    x_d = din("x", [TOK, D]); modc_d = din("modc", [128, 2, KC, 2]); n1T_d = din("n1T", [128, KC]); ident_d = din("ident", [128, 128])
    hT = p.sb([128, KC, TOK], BF16, "hT")
    identb = p.sb([128, 128], BF16, "identb"); identf = p.sb([128, 128], F32, "identf")
    modc = p.sb([128, 2, KC, 2], F32, "modc"); n1T = p.sb([128, KC], F32, "n1T")
    small = p.sb([128, 64], F32, "small"); epsc = p.sb([128, 1], F32, "epsc")
    if xn is None:
        xn = [p.sb([128, D], BF16, "xn%d" % i) for i in range(2)]
    if xt is None:
        xt = [p.sb([128, D], F32, "xt%d" % i)[:, :] for i in range(2)]
    PT = [p.ps([128, 1024], BF16, "pt%d" % i) for i in range(2)]
    p.op("dve", lambda e: e.memset(epsc[:], float(D * EPS)), writes=["epsc"])
    p.dma(identb[:], ident_d, writes=["identb"], q="pool")
    p.dma(identf[:], ident_d, writes=["identf"])
    p.dma(modc[:], modc_d, writes=["modc"]); p.dma(n1T[:], n1T_d, writes=["n1T"])
    finish_modc(p, modc, [(1, n1T, "n1T")])
    load_hT(p, x_d, modc, hT, small, xn, identb, PT, epsc, xt)
    return din, hT, identb, identf, small, PT


def build_A_na(need_ctx=True, stage=9):
    nc = bass.Bass("TRN2", target_bir_lowering=False)
    p = Prog(nc)
    din, hT, identb, identf, small, PT = a_common(nc, p)
    w_d = din("w_na", [D, 768])
    qkw_d = din("qkw", [128, 2])
    bias_d = din("bias_g", [128, 8, 2, 6 * 64]); mask_d = din("maskneg", [128, 6 * 64])
    yT_d = nc.dram_tensor("yT", [256, TOK], BF16, kind="ExternalOutput").ap()

    wq = p.sb([128, KC, 768], BF16, "wq")
    qT = p.sb([128, 2, TOK], BF16, "qT"); kT = p.sb([128, 2, TOK], BF16, "kT")
    vA = p.sb([128, NTA, 256], BF16, "vA"); vS = p.sb([128, 15, 256], BF16, "vS")
    yT = p.sb([128, 2, TOK], BF16, "yT")
    qkw = p.sb([128, 2], F32, "qkw"); epsq = p.sb([128, 1], F32, "epsq")
    bm = p.sb([128, 8, 2, 384], F32, "bm"); maskneg = p.sb([128, 384], F32, "maskneg")
    onesb = p.sb([128, 128], BF16, "onesb")
    qf = [p.sb([128, 384], F32, "qf%d" % i) for i in range(2)]
    sq = [p.sb([128, 384], BF16, "sq%d" % i) for i in range(2)]
    rs = [p.sb([128, 384], F32, "rs%d" % i) for i in range(2)]
    st = [p.sb([128, 512], F32, "st%d" % i) for i in range(2)]
    pT = [p.sb([128, 512], BF16, "pT%d" % i) for i in range(2)]
    rden = p.sb([128, 512], F32, "rden")
    PS = [p.ps([128, 512], F32, "ps%d" % i) for i in range(6)]

    p.op("dve", lambda e: e.memset(onesb[:], 1.0), writes=["onesb"])
    p.op("dve", lambda e: e.memset(epsq[:], float(HD * EPS)), writes=["epsq"])
    p.dma(qkw[:], qkw_d, writes=["qkw"])
    p.op("dve", lambda e: e.tensor_scalar(qkw[:, 1:2], qkw[:, 1:2], float(np.sqrt(HD)), None, ALU.mult), reads=["qkw"], writes=["qkw"])
    p.dma(bm[:], bias_d, writes=["bm"]); p.dma(maskneg[:], mask_d, writes=["maskneg"])
    for pat in range(8):
        for h in range(2):
            p.op("pool", lambda e, pat=pat, h=h: e.tensor_tensor(bm[:, pat, h, :], bm[:, pat, h, :], maskneg[:], ALU.add), reads=["bm", "maskneg"], writes=["bm"])
    p.dma(wq[:], w_d.rearrange("(k p) n -> p k n", p=128), writes=["wq"], q="pool")

    n = 0
    for which, dst in ((0, qT), (1, kT)):
        for h in range(2):
            col = which * 256 + h * 128
            for blk in range(6):
                c0 = blk * 384
                s = n % 2; n += 1
                ps = PS[s]; pk = "ps%d" % s
                for kc in range(KC):
                    p.op("pe", lambda e, kc=kc, ps=ps, col=col, c0=c0: e.matmul(ps[:, 0:384], lhsT=wq[:, kc, col:col + 128], rhs=hT[:, kc, c0:c0 + 384],
                                                                              start=(kc == 0), stop=(kc == KC - 1)), reads=["wq", "hT"], writes=[pk])
                p.op("act", lambda e, ps=ps, s=s: e.activation(sq[s][:], ps[:, 0:384], AF.Square), reads=[pk], writes=["sq%d" % s])
                p.op("dve", lambda e, ps=ps, s=s: e.tensor_copy(qf[s][:], ps[:, 0:384]), reads=[pk], writes=["qf%d" % s])
                ps2 = PS[2 + s]; pk2 = "ps%d" % (2 + s)
                p.op("pe", lambda e, ps2=ps2, s=s: e.matmul(ps2[:, 0:384], lhsT=onesb[:], rhs=sq[s][:], start=True, stop=True), reads=["onesb", "sq%d" % s], writes=[pk2])
                p.op("act", lambda e, ps2=ps2, s=s: e.activation(rs[s][:], ps2[:, 0:384], AF.Ln, bias=epsq[:, 0:1], scale=1.0), reads=[pk2, "epsq"], writes=["rs%d" % s])
                p.op("act", lambda e, s=s: e.activation(rs[s][:], rs[s][:], AF.Exp, scale=-0.5), reads=["rs%d" % s], writes=["rs%d" % s])
                p.op("dve", lambda e, s=s, dst=dst, h=h, c0=c0, which=which: e.scalar_tensor_tensor(out=dst[:, h, c0:c0 + 384], in0=qf[s][:], scalar=qkw[:, which:which + 1], in1=rs[s][:],
                                                                                                 op0=ALU.mult, op1=ALU.mult), reads=["qf%d" % s, "rs%d" % s, "qkw"], writes=[("qk", which, h)])
    for (vt, ntile, off) in (((vA, NTA, 0), (vS, 15, 256 + 64)) if stage >= 2 else ()):
        for ti in range(ntile):
            c0 = off + ti * 128
            s = n % 2; n += 1
            ps = PS[s]; pk = "ps%d" % s
            for kc in range(KC):
                p.op("pe", lambda e, kc=kc, ps=ps, c0=c0: e.matmul(ps[:, 0:256], lhsT=hT[:, kc, c0:c0 + 128], rhs=wq[:, kc, 512:768],
                                                                 start=(kc == 0), stop=(kc == KC - 1)), reads=["wq", "hT"], writes=[pk])
            p.op("act", lambda e, ps=ps, vt=vt, ti=ti: e.copy(vt[:, ti, :], ps[:, 0:256]), reads=[pk], writes=["v"])

    def vtile(tok0, h):
        if tok0 % 128 == 0:
            return vA[:, tok0 // 128, h * 128:(h + 1) * 128]
        return vS[:, (tok0 - 320) // 128, h * 128:(h + 1) * 128]

    it = 0
    for h in (range(2) if stage >= 3 else ()):
        for r8 in range(4):
            po = PS[2 + (r8 % 2)]; pok = "ps%d" % (2 + (r8 % 2))
            pd = PS[4 + (r8 % 2)]; pdk = "ps%d" % (4 + (r8 % 2))
            for rr in range(8):
                r = r8 * 8 + rr
                srow = min(max(r - 4, 0), 24)
                pat = r if r < 4 else (4 if r <= 28 else r - 24)
                q0 = 256 + r * 64
                ktok = [256 + 64 * srow + 128 * kt for kt in range(4)] + [0, 128]
                s = it % 2; it += 1
                ps = PS[s]; pk = "ps%d" % s
                for kt in range(6):
                    p.op("pe", lambda e, kt=kt, ps=ps, h=h, q0=q0, t0=ktok[kt]: e.matmul(ps[:, kt * 64:(kt + 1) * 64], lhsT=kT[:, h, t0:t0 + 128], rhs=qT[:, h, q0:q0 + 64],
                                                                                     start=True, stop=True), reads=[("qk", 0, h), ("qk", 1, h)], writes=[pk])
                p.op("dve", lambda e, ps=ps, s=s, pat=pat, h=h: e.tensor_tensor(st[s][:, 0:384], ps[:, 0:384], bm[:, pat, h, :], ALU.add), reads=[pk, "bm"], writes=["st%d" % s])
                p.op("act", lambda e, s=s: e.activation(pT[s][:, 0:384], st[s][:, 0:384], AF.Exp), reads=["st%d" % s], writes=["pT%d" % s])
                for kt in range(6):
                    p.op("pe", lambda e, kt=kt, s=s, h=h, rr=rr, po=po, t0=ktok[kt]: e.matmul(po[:, rr * 64:(rr + 1) * 64], lhsT=vtile(t0, h), rhs=pT[s][:, kt * 64:(kt + 1) * 64],
                                                                                          start=(kt == 0), stop=(kt == 5)), reads=["v", "pT%d" % s], writes=[pok])
                for kt in range(6):
                    p.op("pe", lambda e, kt=kt, s=s, rr=rr, pd=pd: e.matmul(pd[:, rr * 64:(rr + 1) * 64], lhsT=onesb[:], rhs=pT[s][:, kt * 64:(kt + 1) * 64],
                                                                          start=(kt == 0), stop=(kt == 5)), reads=["onesb", "pT%d" % s], writes=[pdk])
            p.op("dve", lambda e, pd=pd: e.reciprocal(rden[:], pd[:, :]), reads=[pdk], writes=["rden"])
            p.op("dve", lambda e, po=po, h=h, r8=r8: e.tensor_tensor(yT[:, h, 256 + r8 * 512: 256 + (r8 + 1) * 512], po[:, :], rden[:], ALU.mult), reads=[pok, "rden"], writes=[("yT", h)])
    if need_ctx and stage >= 4:
        for h in range(2):
            ps = PS[0]; pk = "ps0"
            for kt in range(2):
                p.op("pe", lambda e, kt=kt, h=h: e.matmul(PS[0][:, kt * 256:(kt + 1) * 256], lhsT=kT[:, h, kt * 128:(kt + 1) * 128], rhs=qT[:, h, 0:256],
                                                       start=True, stop=True), reads=[("qk", 0, h), ("qk", 1, h)], writes=["ps0"])
            p.op("act", lambda e: e.activation(pT[0][:, :], PS[0][:, :], AF.Exp), reads=["ps0"], writes=["pT0"])
            for kt in range(2):
                p.op("pe", lambda e, kt=kt, h=h: e.matmul(PS[2][:, 0:256], lhsT=vA[:, kt, h * 128:(h + 1) * 128], rhs=pT[0][:, kt * 256:(kt + 1) * 256],
                                                       start=(kt == 0), stop=(kt == 1)), reads=["v", "pT0"], writes=["ps2"])
            for kt in range(2):
                p.op("pe", lambda e, kt=kt: e.matmul(PS[4][:, 0:256], lhsT=onesb[:], rhs=pT[0][:, kt * 256:(kt + 1) * 256],
                                                  start=(kt == 0), stop=(kt == 1)), reads=["onesb", "pT0"], writes=["ps4"])
            p.op("dve", lambda e: e.reciprocal(rden[:, 0:256], PS[4][:, 0:256]), reads=["ps4"], writes=["rden"])
            p.op("dve", lambda e, h=h: e.tensor_tensor(yT[:, h, 0:256], PS[2][:, 0:256], rden[:, 0:256], ALU.mult), reads=["ps2", "rden"], writes=[("yT", h)])
    else:
        p.op("dve", lambda e: e.memset(yT[:, :, 0:256], 0.0), writes=["yT"])
    for h in range(2):
        p.dma(yT_d[h * 128:(h + 1) * 128, :], (yT if stage >= 3 else qT)[:, h, :], reads=[("yT", h), ("qk", 0, h)])
    p.emit()
    return nc
HC = 32
NCH = TOK // HC
FWD_ORDER = list(range(NCH))
NCTX = 256 // HC
BWD_ORDER = list(range(NCTX - 1, -1, -1)) + list(range(NCH - 1, NCTX - 1, -1))


def build_A_hg():
    nc = bass.Bass("TRN2", target_bir_lowering=False)
    p = Prog(nc)
    B = [p.sb([128, TOK], F32, "B%d" % i) for i in range(4)]
    xt = [B[0][:, 0:D]]
    b1v = B[1][:, :].bitcast(BF16)
    xn = [b1v[:, 0:D], b1v[:, D:2 * D]]
    din, hT, identb, identf, small, PT = a_common(nc, p, xt=xt, xn=xn)
    w_d = din("w_hg", [3, D, 640])
    lbl_d = din("lbl", [128, 2, 2, 3]); sel_d = din("lbsel", [128, 2])
    nw_d = din("hg_nw", [128, 1]); mask_d = din("hgmask", [HC, 2, HC])
    yT_d = nc.dram_tensor("yT", [384, TOK], BF16, kind="ExternalOutput").ap()

    W = p.sb([128, KC, 640], BF16, "W")
    QP = [p.sb([128, TOK], BF16, "QP%d" % i) for i in range(2)]
    KP = [p.sb([128, TOK], BF16, "KP%d" % i) for i in range(2)]
    SG = p.sb([128, TOK], BF16, "SG"); YO = p.sb([128, TOK], BF16, "YO")
    YACC = p.sb([128, TOK], F32, "YACC")
    V64 = p.sb([HC, NCH, 128], BF16, "V64")
    ones128 = p.sb([128, 128], BF16, "ones128")
    lbl = p.sb([128, 2, 2, 3], F32, "lbl"); sel = p.sb([128, 2], F32, "sel"); lb = p.sb([128, 2, 3], F32, "lb"); oml = p.sb([128, 2, 3], F32, "oml")
    nw = p.sb([128, 1], F32, "nw"); epsq = p.sb([128, 1], F32, "epsq")
    maskf = p.sb([HC, 2, HC], F32, "maskf")
    colA = p.sb([128, 2, 8, NCH], F32, "colA")
    attm = [p.sb([HC, HC], BF16, "attm%d" % i) for i in range(2)]
    ktt = [p.sb([HC, 128], BF16, "ktt%d" % i) for i in range(2)]
    Z = [p.sb([128, 128], F32, "Z%d" % i) for i in range(2)]
    Mb = [p.sb([128, 128], BF16, "Mb%d" % i) for i in range(2)]
    sq = [p.sb([128, 384], BF16, "sq%d" % i) for i in range(2)]
    rs = [p.sb([128, 384], F32, "rs%d" % i) for i in range(2)]
    PS = [p.ps([128, 512], F32, "ps%d" % i) for i in range(5)]
    PK = p.ps([HC, 128], BF16, "pk")

    p.op("dve", lambda e: e.memset(ones128[:], 1.0), writes=["ones128"])
    p.op("dve", lambda e: e.memset(epsq[:], float(128 * EPS)), writes=["epsq"])
    p.dma(lbl[:], lbl_d, writes=["lbl"]); p.dma(sel[:], sel_d, writes=["sel"]); p.dma(nw[:], nw_d, writes=["nw"]); p.dma(maskf[:], mask_d, writes=["maskf"])
    p.op("act", lambda e: e.activation(lbl[:], lbl[:], AF.Exp), reads=["lbl"], writes=["lbl"])
    p.op("dve", lambda e: e.tensor_tensor(oml[:], lbl[:, 0], lbl[:, 1], ALU.add), reads=["lbl"], writes=["oml"])
    p.op("dve", lambda e: e.reciprocal(oml[:], oml[:]), reads=["oml"], writes=["oml"])
    p.op("dve", lambda e: e.tensor_scalar(lb[:], lbl[:, 0], sel[:, 0:1], None, ALU.mult), reads=["lbl", "sel"], writes=["lb"])
    p.op("dve", lambda e: e.scalar_tensor_tensor(out=lb[:], in0=lbl[:, 1], scalar=sel[:, 1:2], in1=lb[:], op0=ALU.mult, op1=ALU.add), reads=["lbl", "sel", "lb"], writes=["lb"])
    p.op("dve", lambda e: e.tensor_tensor(lb[:], lb[:], oml[:], ALU.mult), reads=["lb", "oml"], writes=["lb"])
    p.op("dve", lambda e: e.tensor_scalar(oml[:], lb[:], -1.0, 1.0, ALU.mult, ALU.add), reads=["lb"], writes=["oml"])
    p.op("dve", lambda e: e.tensor_scalar(nw[:], nw[:], float(np.sqrt(128.0)), None, ALU.mult), reads=["nw"], writes=["nw"])

    def proj(col, evac):
        for blk in range(6):
            c0 = blk * 384
            ps = PS[blk % 2]; pk = "ps%d" % (blk % 2)
            for kc in range(KC):
                p.op("pe", lambda e, kc=kc, ps=ps, c0=c0: e.matmul(ps[:, 0:384], lhsT=W[:, kc, col:col + 128], rhs=hT[:, kc, c0:c0 + 384],
                                                                 start=(kc == 0), stop=(kc == KC - 1)), reads=["W", "hT"], writes=[pk])
            evac(ps, pk, c0)

    for h in range(3):
        p.dma(W[:], w_d[h].rearrange("(k p) n -> p k n", p=128), writes=["W"], q="pool")
        QS, F, LF, CUM = B[0], B[1], B[2], B[3]
        E = LF
        p.op("dve", lambda e: e.memset(YO[:], 1.0), writes=["YO"])
        proj(0, lambda ps, pk, c0: p.op("act", lambda e: e.activation(QS[:, c0:c0 + 384], ps[:, 0:384], AF.Silu), reads=[pk], writes=["QS"]))
        proj(384, lambda ps, pk, c0: p.op("act", lambda e: e.activation(SG[:, c0:c0 + 384], ps[:, 0:384], AF.Silu), reads=[pk], writes=["SG"]))
        for c in range(NCH):
            ps = PS[c % 2]; pk = "ps%d" % (c % 2)
            for kc in range(KC):
                p.op("pe", lambda e, kc=kc, ps=ps, c=c: e.matmul(ps[0:HC, 0:128], lhsT=hT[:, kc, c * HC:(c + 1) * HC], rhs=W[:, kc, 512:640],
                                                               start=(kc == 0), stop=(kc == KC - 1)), reads=["W", "hT"], writes=[pk])
            p.op("act", lambda e, ps=ps, c=c: e.copy(V64[:, c, :], ps[0:HC, 0:128]), reads=[pk], writes=["V64"])
        for d in range(2):
            proj(128 + d * 128, lambda ps, pk, c0: p.op("act", lambda e: e.activation(F[:, c0:c0 + 384], ps[:, 0:384], AF.Sigmoid), reads=[pk], writes=["F"]))
            p.op("dve", lambda e, d=d, h=h: e.tensor_scalar(F[:], F[:], oml[:, d, h:h + 1], lb[:, d, h:h + 1], ALU.mult, ALU.add), reads=["F", "oml", "lb"], writes=["F"])
            p.op("dve", lambda e: e.tensor_scalar(LF[:], F[:], 1e-6, None, ALU.max), reads=["F"], writes=["LF"])
            p.op("act", lambda e: e.activation(LF[:], LF[:], AF.Ln), reads=["LF"], writes=["LF"])
            p.op("dve", lambda e: e.tensor_tensor_scan(CUM[:], YO[:], LF[:], 0.0, ALU.mult, ALU.add), reads=["YO", "LF"], writes=["CUM"])
            C3 = CUM[:, :].rearrange("p (c j) -> p c j", j=HC)
            L3 = LF[:, :].rearrange("p (c j) -> p c j", j=HC)
            E3 = E[:, :].rearrange("p (c j) -> p c j", j=HC)
            cb, ct, cm, clm, cg, cG = (colA[:, d, i, :] for i in range(6))
            ck = ("colA", d)
            p.op("dve", lambda e, cb=cb: e.memset(cb[:, 0:1], 0.0), writes=[ck])
            p.op("dve", lambda e, cb=cb, C3=C3: e.tensor_copy(cb[:, 1:NCH], C3[:, 0:NCH - 1, HC - 1]), reads=["CUM"], writes=[ck])
            p.op("dve", lambda e, cb=cb, C3=C3: e.tensor_tensor(C3, C3, cb.unsqueeze(2).to_broadcast([128, NCH, HC]), ALU.subtract), reads=["CUM", ck], writes=["CUM"])
            p.op("dve", lambda e, ct=ct, C3=C3: e.tensor_copy(ct, C3[:, :, HC - 1]), reads=["CUM"], writes=[ck])
            if d == 1:
                p.op("dve", lambda e, ct=ct, C3=C3: e.tensor_tensor(C3, ct.unsqueeze(2).to_broadcast([128, NCH, HC]), C3, ALU.subtract), reads=["CUM", ck], writes=["CUM"])
                p.op("dve", lambda e: e.tensor_tensor(CUM[:], CUM[:], LF[:], ALU.add), reads=["CUM", "LF"], writes=["CUM"])
                p.op("dve", lambda e, cm=cm, C3=C3: e.tensor_copy(cm, C3[:, :, HC // 2]), reads=["CUM"], writes=[ck])
            else:
                p.op("dve", lambda e, cm=cm, C3=C3: e.tensor_copy(cm, C3[:, :, HC // 2 - 1]), reads=["CUM"], writes=[ck])
            p.op("dve", lambda e, ct=ct, cm=cm, clm=clm: e.tensor_tensor(clm, ct, cm, ALU.subtract), reads=[ck], writes=[ck])
            if d == 0:
                p.op("dve", lambda e, cg=cg, clm=clm, cm=cm: e.tensor_tensor(cg[:, 0:NCH - 1], clm[:, 0:NCH - 1], cm[:, 1:NCH], ALU.add), reads=[ck], writes=[ck])
                p.op("dve", lambda e, cg=cg, clm=clm: e.tensor_copy(cg[:, NCH - 1:NCH], clm[:, NCH - 1:NCH]), reads=[ck], writes=[ck])
            else:
                p.op("dve", lambda e, cg=cg, clm=clm, cm=cm: e.tensor_tensor(cg[:, 1:NCH], clm[:, 1:NCH], cm[:, 0:NCH - 1], ALU.add), reads=[ck], writes=[ck])
                p.op("dve", lambda e, cg=cg, clm=clm, cm=cm: e.tensor_tensor(cg[:, 0:1], clm[:, 0:1], cm[:, NCH - 1:NCH], ALU.add), reads=[ck], writes=[ck])
            p.op("act", lambda e, cg=cg, cG=cG: e.activation(cG, cg, AF.Exp), reads=[ck], writes=[ck])
            p.op("dve", lambda e, cm=cm, C3=C3, E3=E3: e.tensor_tensor(E3, C3, cm.unsqueeze(2).to_broadcast([128, NCH, HC]), ALU.subtract), reads=["CUM", ck, "LF"], writes=["LF"])
            p.op("act", lambda e: e.activation(CUM[:], E[:], AF.Exp), reads=["LF"], writes=["CUM"])
            p.op("dve", lambda e, d=d: e.scalar_tensor_tensor(out=QP[d][:], in0=CUM[:], scalar=float(128.0 ** -0.5), in1=QS[:], op0=ALU.mult, op1=ALU.mult), reads=["CUM", "QS"], writes=["QP%d" % d])
            p.op("act", lambda e: e.activation(E[:], E[:], AF.Exp, scale=-1.0), reads=["LF"], writes=["LF"])
            p.op("dve", lambda e: e.tensor_scalar(F[:], F[:], -1.0, 1.0, ALU.mult, ALU.add), reads=["F"], writes=["F"])
            p.op("dve", lambda e, d=d: e.tensor_tensor(KP[d][:], F[:], E[:], ALU.mult), reads=["F", "LF"], writes=["KP%d" % d])
        p.op("dve", lambda e: e.memset(YACC[:], 0.0), writes=["YACC"])
        orders = [FWD_ORDER, BWD_ORDER]
        for step in range(NCH):
            for d in range(2):
                c = orders[d][step]
                cs = slice(c * HC, (c + 1) * HC)
                pa = PS[2]; pak = "ps2"
                p.op("pe", lambda e, d=d, cs=cs: e.matmul(PS[2][0:HC, 0:HC], lhsT=KP[d][:, cs], rhs=QP[d][:, cs], start=True, stop=True), reads=["KP%d" % d, "QP%d" % d], writes=["ps2"])
                p.op("dve", lambda e, d=d: e.tensor_tensor(attm[d][:], PS[2][0:HC, 0:HC], maskf[:, d, :], ALU.mult), reads=["ps2", "maskf"], writes=["attm%d" % d])
                py = PS[3]; pyk = "ps3"
                p.op("pe", lambda e, d=d, c=c, step=step: e.matmul(PS[3][:, 0:HC], lhsT=V64[:, c, :], rhs=attm[d][:], start=True, stop=(step == 0)), reads=["V64", "attm%d" % d], writes=["ps3"])
                if step > 0:
                    p.op("pe", lambda e, d=d, cs=cs: e.matmul(PS[3][:, 0:HC], lhsT=Mb[d][:], rhs=QP[d][:, cs], start=False, stop=True), reads=["Mb%d" % d, "QP%d" % d], writes=["ps3"])
                p.op("dve", lambda e, cs=cs: e.tensor_tensor(YACC[:, cs], YACC[:, cs], PS[3][:, 0:HC], ALU.add), reads=["ps3", ("YACC", c)], writes=[("YACC", c)])
                if step < NCH - 1:
                    p.op("pe", lambda e, d=d, cs=cs: e.transpose(PK[:, :], KP[d][:, cs], identb[:]), reads=["KP%d" % d, "identb"], writes=["pk"])
                    p.op("act", lambda e, d=d: e.copy(ktt[d][:], PK[:, :]), reads=["pk"], writes=["ktt%d" % d])
                    p.op("pe", lambda e, d=d, c=c: e.matmul(PS[4][:, 0:128], lhsT=ktt[d][:], rhs=V64[:, c, :], start=True, stop=True), reads=["ktt%d" % d, "V64"], writes=["ps4"])
                    if step == 0:
                        p.op("dve", lambda e, d=d: e.tensor_copy(Z[d][:], PS[4][:, 0:128]), reads=["ps4"], writes=["Z%d" % d])
                    else:
                        cprev = orders[d][step - 1]
                        p.op("dve", lambda e, d=d, cprev=cprev: e.scalar_tensor_tensor(out=Z[d][:], in0=Z[d][:], scalar=colA[:, d, 5, cprev:cprev + 1], in1=PS[4][:, 0:128],
                                                                                     op0=ALU.mult, op1=ALU.add), reads=["Z%d" % d, ("colA", d), "ps4"], writes=["Z%d" % d])
                    p.op("act", lambda e, d=d, c=c: e.activation(Mb[d][:], Z[d][:], AF.Identity, scale=colA[:, d, 5, c:c + 1]), reads=["Z%d" % d, ("colA", d)], writes=["Mb%d" % d])
        for blk in range(6):
            c0 = blk * 384
            s = blk % 2
            p.op("act", lambda e, s=s, c0=c0: e.activation(sq[s][:], YACC[:, c0:c0 + 384], AF.Square), reads=["YACC"], writes=["sq%d" % s])
            p.op("pe", lambda e, s=s: e.matmul(PS[s][:, 0:384], lhsT=ones128[:], rhs=sq[s][:], start=True, stop=True), reads=["ones128", "sq%d" % s], writes=["ps%d" % s])
            p.op("act", lambda e, s=s: e.activation(rs[s][:], PS[s][:, 0:384], AF.Ln, bias=epsq[:, 0:1], scale=1.0), reads=["ps%d" % s, "epsq"], writes=["rs%d" % s])
            p.op("act", lambda e, s=s: e.activation(rs[s][:], rs[s][:], AF.Exp, scale=-0.5), reads=["rs%d" % s], writes=["rs%d" % s])
            p.op("dve", lambda e, s=s, c0=c0: e.scalar_tensor_tensor(out=rs[s][:], in0=YACC[:, c0:c0 + 384], scalar=nw[:, 0:1], in1=rs[s][:], op0=ALU.mult, op1=ALU.mult),
                 reads=["YACC", "nw", "rs%d" % s], writes=["rs%d" % s])
            p.op("dve", lambda e, s=s, c0=c0: e.tensor_tensor(YO[:, c0:c0 + 384], rs[s][:], SG[:, c0:c0 + 384], ALU.mult), reads=["rs%d" % s, "SG"], writes=["YO"])
        p.dma(yT_d[h * 128:(h + 1) * 128, :], YO[:], reads=["YO"])
    p.emit()
    return nc
SSD_FWD = list(range(NTA))
SSD_BWD = [1, 0] + list(range(NTA - 1, 1, -1))


def build_A_ssd():
    nc = bass.Bass("TRN2", target_bir_lowering=False)
    p = Prog(nc)
    XR = p.sb([128, TOK], F32, "XR"); ACC = p.sb([128, TOK], F32, "ACC")
    accv = ACC[:, :].bitcast(BF16)
    din, hT, identb, identf, small, PT = a_common(nc, p, xt=[XR[:, 0:D]], xn=[accv[:, 0:D], accv[:, D:2 * D]])
    wz_d = din("w_z", [D, 384]); wx_d = din("w_xbc", [D, 896]); wdt_d = din("w_dt", [D, 12])
    cw_d = din("conv_w", [128, 7, 4]); cb_d = din("conv_b", [128, 7])
    dtb_d = din("dt_bias", [128, 12]); alog_d = din("a_log", [128, 12])
    dsk_d = din("dskip", [128, 384]); nw_d = din("ssd_nw", [128, 384])
    cos_d = din("rope_cos", [128, 2048]); sin_d = din("rope_sin", [128, 2048]); pm_d = din("rope_pm", [128, 128])
    tri_d = din("tri", [128, 2, 128]); mneg_d = din("mneg", [128, 2, 128])
    yT_d = nc.dram_tensor("yT", [384, TOK], BF16, kind="ExternalOutput").ap()

    Wc = [p.sb([128, KC, 128], BF16, "Wc0")] * 2
    wdt = p.sb([128, KC, 12], BF16, "wdt")
    BT = p.sb([128, 2, TOK], BF16, "BT"); CT = p.sb([128, 2, TOK], BF16, "CT")
    xs_tok = p.sb([128, NTA, 384], BF16, "xs_tok"); B_tok = p.sb([128, NTA, 256], BF16, "B_tok")
    sz_tok = p.sb([128, NTA, 384], BF16, "sz_tok")
    yacc = p.sb([128, NTA, 384], F32, "yacc")
    cosb = p.sb([128, 512], BF16, "cosb"); sinb = p.sb([128, 512], BF16, "sinb"); pmb = p.sb([128, 128], BF16, "pmb")
    XB = p.sb([128, TOK], BF16, "XB")
    cw = p.sb([128, 7, 4], F32, "cw"); cbias = p.sb([128, 7], F32, "cbias")
    dtb = p.sb([128, 12], F32, "dtb"); aexp = p.sb([128, 12], F32, "aexp")
    dsk = p.sb([128, 384], F32, "dsk"); nwt = p.sb([128, 384], F32, "nwt")
    tri = p.sb([128, 2, 128], BF16, "tri"); mneg = p.sb([128, 2, 128], BF16, "mneg")
    dt = p.sb([128, NTA, 12], F32, "dt"); da = p.sb([128, NTA, 12], F32, "da")
    dah = p.sb([128, NTA, 12], BF16, "dah"); dal = p.sb([128, NTA, 12], BF16, "dal"); dtmp = p.sb([128, NTA, 12], F32, "dtmp")
    cum = p.sb([128, NTA, 12], F32, "cum"); bcol = p.sb([128, NTA, 12], F32, "bcol"); ecum = p.sb([128, NTA, 12], F32, "ecum")
    SD = [p.sb([128, 128], F32, "SD%d" % i) for i in range(2)]
    MT = [p.sb([128, 128], BF16, "MT%d" % i) for i in range(2)]
    CBs = [p.sb([128, 128], F32, "CBs%d" % i) for i in range(2)]
    xw = p.sb([128, 384], BF16, "xw"); el = p.sb([128, 6], F32, "el")
    tmp = p.sb([128, 384], F32, "tmp")
    hS = [p.sb([128, 384], F32, "hS%d" % i) for i in range(2)]
    hSb = [p.sb([128, 384], BF16, "hSb%d" % i) for i in range(2)]
    yfin = p.sb([128, 384], F32, "yfin"); tmp2 = yfin; yfb = p.sb([128, 384], BF16, "yfb"); ytile = p.sb([128, 3, 128], BF16, "ytile")
    gss = p.sb([128, 4], F32, "gss"); epsg = p.sb([128, 1], F32, "epsg")
    PS = [p.ps([128, 512], F32, "ps%d" % i) for i in range(6)]

    for t_, d_, k_ in ((cw, cw_d, "cw"), (cbias, cb_d, "cbias"), (dtb, dtb_d, "dtb"), (aexp, alog_d, "aexp"), (dsk, dsk_d, "dsk"), (nwt, nw_d, "nwt")):
        p.dma(t_[:], d_, writes=[k_])
    for t_, d_, k_ in ((pmb, pm_d, "pmb"), (tri, tri_d, "tri"), (mneg, mneg_d, "mneg"), (wdt, wdt_d.rearrange("(k p) n -> p k n", p=128), "wdt")):
        p.dma(t_[:], d_, writes=[k_], q="pool")
    p.op("act", lambda e: e.activation(aexp[:], aexp[:], AF.Exp), reads=["aexp"], writes=["aexp"])
    p.op("dve", lambda e: e.memset(epsg[:], float(192 * EPS)), writes=["epsg"])
    p.op("dve", lambda e: e.tensor_scalar(nwt[:], nwt[:], float(np.sqrt(192.0)), None, ALU.mult), reads=["nwt"], writes=["nwt"])

    for ti in range(NTA):
        ps = PS[ti % 2]; pk = "ps%d" % (ti % 2)
        for kc in range(KC):
            p.op("pe", lambda e, kc=kc, ps=ps, ti=ti: e.matmul(ps[:, 0:12], lhsT=hT[:, kc, ti * 128:(ti + 1) * 128], rhs=wdt[:, kc, :], start=(kc == 0), stop=(kc == KC - 1)),
                 reads=["hT", "wdt"], writes=[pk])
        p.op("dve", lambda e, ps=ps, ti=ti: e.tensor_tensor(dt[:, ti, :], ps[:, 0:12], dtb[:], ALU.add), reads=[pk, "dtb"], writes=["dt"])
    p.op("act", lambda e: e.activation(dt[:], dt[:], AF.Exp), reads=["dt"], writes=["dt"])
    p.op("act", lambda e: e.activation(dt[:], dt[:], AF.Ln, bias=1.0, scale=1.0), reads=["dt"], writes=["dt"])
    p.op("dve", lambda e: e.tensor_tensor(da[:], dt[:], aexp[:].unsqueeze(1).to_broadcast([128, NTA, 12]), ALU.mult), reads=["dt", "aexp"], writes=["da"])
    p.op("dve", lambda e: e.tensor_scalar(da[:], da[:], -1.0, None, ALU.mult), reads=["da"], writes=["da"])
    p.op("dve", lambda e: e.tensor_copy(dah[:], da[:]), reads=["da"], writes=["dah"])
    p.op("dve", lambda e: e.tensor_tensor(dtmp[:], da[:], dah[:], ALU.subtract), reads=["da", "dah"], writes=["dtmp"])
    p.op("dve", lambda e: e.tensor_copy(dal[:], dtmp[:]), reads=["dtmp"], writes=["dal"])
    for ti in range(NTA):
        ps = PS[ti % 2]; pk = "ps%d" % (ti % 2)
        for d in range(2):
            p.op("pe", lambda e, ps=ps, ti=ti, d=d: e.matmul(ps[:, d * 6:(d + 1) * 6], lhsT=tri[:, d, :], rhs=dah[:, ti, d * 6:(d + 1) * 6], start=True, stop=False), reads=["tri", "dah"], writes=[pk])
            p.op("pe", lambda e, ps=ps, ti=ti, d=d: e.matmul(ps[:, d * 6:(d + 1) * 6], lhsT=tri[:, d, :], rhs=dal[:, ti, d * 6:(d + 1) * 6], start=False, stop=True), reads=["tri", "dal"], writes=[pk])
        p.op("dve", lambda e, ps=ps, ti=ti: e.tensor_copy(cum[:, ti, :], ps[:, 0:12]), reads=[pk], writes=["cum"])
    p.op("act", lambda e: e.activation(ecum[:], cum[:], AF.Exp), reads=["cum"], writes=["ecum"])
    p.op("act", lambda e: e.activation(bcol[:], dt[:], AF.Ln), reads=["dt"], writes=["bcol"])
    p.op("dve", lambda e: e.tensor_tensor(bcol[:], bcol[:], cum[:], ALU.subtract), reads=["bcol", "cum"], writes=["bcol"])

    for j in range(3):
        wc = Wc[0]; wk = "Wc0"
        p.dma(wc[:], wz_d.rearrange("(k p) n -> p k n", p=128)[:, :, j * 128:(j + 1) * 128], writes=[wk], q="pool")
        for ti in range(NTA):
            ps = PS[ti % 2]; pk = "ps%d" % (ti % 2)
            for kc in range(KC):
                p.op("pe", lambda e, kc=kc, ps=ps, ti=ti, wc=wc: e.matmul(ps[:, 0:128], lhsT=hT[:, kc, ti * 128:(ti + 1) * 128], rhs=wc[:, kc, :], start=(kc == 0), stop=(kc == KC - 1)),
                     reads=["hT", wk], writes=[pk])
            p.op("act", lambda e, ps=ps, ti=ti, j=j: e.activation(sz_tok[:, ti, j * 128:(j + 1) * 128], ps[:, 0:128], AF.Silu), reads=[pk], writes=["sz_tok"])

    for ch in range(7):
        wc = Wc[0]; wk = "Wc0"
        p.dma(wc[:], wx_d.rearrange("(k p) n -> p k n", p=128)[:, :, ch * 128:(ch + 1) * 128], writes=[wk], q="pool")
        for blk in range(6):
            c0 = blk * 384
            ps = PS[blk % 2]; pk = "ps%d" % (blk % 2)
            for kc in range(KC):
                p.op("pe", lambda e, kc=kc, ps=ps, c0=c0, wc=wc: e.matmul(ps[:, 0:384], lhsT=wc[:, kc, :], rhs=hT[:, kc, c0:c0 + 384], start=(kc == 0), stop=(kc == KC - 1)),
                     reads=["hT", wk], writes=[pk])
            p.op("act", lambda e, ps=ps, c0=c0: e.copy(XR[:, c0:c0 + 384], ps[:, 0:384]), reads=[pk], writes=["XR"])
        p.op("act", lambda e, ch=ch: e.activation(ACC[:], XR[:], AF.Identity, bias=cbias[:, ch:ch + 1], scale=cw[:, ch, 1:2]), reads=["XR", "cbias", "cw"], writes=["ACC"])
        for (a, b) in ((0, 256), (256, TOK)):
            for (j, sh) in ((0, -1), (2, 1), (3, 2)):
                lo = max(a, a - sh); hi = min(b, b - sh)
                p.op("dve", lambda e, ch=ch, j=j, sh=sh, lo=lo, hi=hi: e.scalar_tensor_tensor(out=ACC[:, lo:hi], in0=XR[:, lo + sh:hi + sh], scalar=cw[:, ch, j:j + 1], in1=ACC[:, lo:hi],
                                                                                            op0=ALU.mult, op1=ALU.add), reads=["XR", "ACC", "cw"], writes=["ACC"])
        p.op("act", lambda e: e.activation(XB[:], ACC[:], AF.Silu), reads=["ACC"], writes=["XB"])
        if ch < 3:
            for ti in range(NTA):
                hh = ti % 2
                p.op("pe", lambda e, ti=ti, hh=hh: e.transpose(PT[hh][:, 0:128], XB[:, ti * 128:(ti + 1) * 128], identb[:]), reads=["XB", "identb"], writes=["pt%d" % hh])
                p.op("act", lambda e, ti=ti, hh=hh, ch=ch: e.copy(xs_tok[:, ti, ch * 128:(ch + 1) * 128], PT[hh][:, 0:128]), reads=["pt%d" % hh], writes=["xs_tok"])
        else:
            g = (ch - 3) % 2
            dst = BT if ch < 5 else CT; dk = "BT" if ch < 5 else "CT"
            p.op("act", lambda e, dst=dst, g=g: e.copy(dst[:, g, 0:256], XB[:, 0:256]), reads=["XB"], writes=[dk])
            for blk in range(4):
                c0 = 256 + blk * 512
                ps = PS[2 + blk % 2]; pk = "ps%d" % (2 + blk % 2)
                p.dma(cosb[:], cos_d[:, blk * 512:(blk + 1) * 512], writes=["cosb"], q="pool")
                p.dma(sinb[:], sin_d[:, blk * 512:(blk + 1) * 512], writes=["sinb"], q="pool")
                p.op("pe", lambda e, ps=ps, c0=c0: e.matmul(ps[:, :], lhsT=pmb[:], rhs=XB[:, c0:c0 + 512], start=True, stop=True), reads=["pmb", "XB"], writes=[pk])
                p.op("dve", lambda e, ps=ps, blk=blk: e.tensor_tensor(XR[:, 0:512], ps[:, :], sinb[:, :], ALU.mult), reads=[pk, "sinb"], writes=["XR"])
                p.op("pool", lambda e, c0=c0, blk=blk: e.tensor_tensor(XR[:, 512:1024], XB[:, c0:c0 + 512], cosb[:, :], ALU.mult), reads=["XB", "cosb"], writes=[("XR", 1)])
                p.op("dve", lambda e, dst=dst, g=g, c0=c0: e.tensor_tensor(dst[:, g, c0:c0 + 512], XR[:, 0:512], XR[:, 512:1024], ALU.add), reads=["XR"], writes=[dk])
            if ch < 5:
                for ti in range(NTA):
                    hh = ti % 2
                    p.op("pe", lambda e, ti=ti, hh=hh, g=g: e.transpose(PT[hh][:, 0:128], BT[:, g, ti * 128:(ti + 1) * 128], identb[:]), reads=["BT", "identb"], writes=["pt%d" % hh])
                    p.op("act", lambda e, ti=ti, hh=hh, g=g: e.copy(B_tok[:, ti, g * 128:(g + 1) * 128], PT[hh][:, 0:128]), reads=["pt%d" % hh], writes=["B_tok"])

    p.op("dve", lambda e: e.memset(yacc[:], 0.0), writes=["yacc"])
    orders = [SSD_FWD, SSD_BWD]
    it = 0
    for step in range(NTA):
        for d in range(2):
            c = orders[d][step]
            cs = slice(c * 128, (c + 1) * 128)
            lastcol = 127 if d == 0 else 0
            for g in range(2):
                p.op("pe", lambda e, g=g, cs=cs: e.matmul(PS[2][:, g * 128:(g + 1) * 128], lhsT=BT[:, g, cs], rhs=CT[:, g, cs], start=True, stop=True), reads=["BT", "CT"], writes=["ps2"])
                p.op("act", lambda e, g=g: e.copy(CBs[g][:], PS[2][:, g * 128:(g + 1) * 128]), reads=["ps2"], writes=["CBs%d" % g])
            for hd in range(6):
                g = hd // 3
                col = d * 6 + hd
                s = it % 2; it += 1
                pr = PS[s]; prk = "ps%d" % s
                p.op("pe", lambda e, pr=pr, c=c, col=col, d=d: e.matmul(pr[:, 0:128], lhsT=dah[:, c, col:col + 1].to_broadcast([128, 128]), rhs=tri[:, d, :], start=True, stop=False), reads=["dah", "tri"], writes=[prk])
                p.op("pe", lambda e, pr=pr, c=c, col=col, d=d: e.matmul(pr[:, 0:128], lhsT=dal[:, c, col:col + 1].to_broadcast([128, 128]), rhs=tri[:, d, :], start=False, stop=False), reads=["dal", "tri"], writes=[prk])
                p.op("pe", lambda e, pr=pr, d=d: e.matmul(pr[:, 0:128], lhsT=identb[:], rhs=mneg[:, d, :], start=False, stop=True), reads=["identb", "mneg"], writes=[prk])
                p.op("act", lambda e, pr=pr, s=s, c=c, col=col: e.activation(SD[s][:], pr[:, 0:128], AF.Exp, bias=bcol[:, c, col:col + 1], scale=1.0), reads=[prk, "bcol"], writes=["SD%d" % s])
                p.op("act", lambda e, pr=pr, hd=hd, lastcol=lastcol: e.activation(el[:, hd:hd + 1], pr[:, lastcol:lastcol + 1], AF.Exp), reads=[prk], writes=["el"])
                p.op("dve", lambda e, s=s, g=g: e.tensor_tensor(MT[s][:], SD[s][:], CBs[g][:], ALU.mult), reads=["SD%d" % s, "CBs%d" % g], writes=["MT%d" % s])
                p.op("pe", lambda e, s=s, c=c, hd=hd: e.matmul(PS[3][:, hd * 64:(hd + 1) * 64], lhsT=MT[s][:], rhs=xs_tok[:, c, hd * 64:(hd + 1) * 64], start=True, stop=True), reads=["MT%d" % s, "xs_tok"], writes=["ps3"])
                if step > 0:
                    p.op("pe", lambda e, g=g, cs=cs, hd=hd, d=d: e.matmul(PS[4][:, hd * 64:(hd + 1) * 64], lhsT=CT[:, g, cs], rhs=hSb[d][:, hd * 64:(hd + 1) * 64], start=True, stop=True), reads=["CT", "hSb%d" % d], writes=["ps4"])
                if step < NTA - 1:
                    p.op("dve", lambda e, s=s, c=c, hd=hd, lastcol=lastcol: e.tensor_scalar(xw[:, hd * 64:(hd + 1) * 64], xs_tok[:, c, hd * 64:(hd + 1) * 64], SD[s][:, lastcol:lastcol + 1], None, ALU.mult),
                         reads=["SD%d" % s, "xs_tok"], writes=["xw"])
            if step > 0:
                p.op("dve", lambda e, c=c, d=d: e.tensor_tensor(tmp[:, :].rearrange("p (h j) -> p h j", j=64), PS[4][:, 0:384].rearrange("p (h j) -> p h j", j=64),
                                                             ecum[:, c, d * 6:(d + 1) * 6].unsqueeze(2).to_broadcast([128, 6, 64]), ALU.mult), reads=["ps4", "ecum"], writes=["tmp"])
                p.op("pool", lambda e, c=c: e.tensor_tensor(yacc[:, c, :], yacc[:, c, :], tmp[:], ALU.add), reads=["tmp", ("yacc", c)], writes=[("yacc", c)])
            p.op("dve", lambda e, c=c: e.tensor_tensor(yacc[:, c, :], yacc[:, c, :], PS[3][:, 0:384], ALU.add), reads=["ps3", ("yacc", c)], writes=[("yacc", c)])
            if step < NTA - 1:
                for g in range(2):
                    p.op("pe", lambda e, g=g, c=c: e.matmul(PS[5][:, g * 192:(g + 1) * 192], lhsT=B_tok[:, c, g * 128:(g + 1) * 128], rhs=xw[:, g * 192:(g + 1) * 192], start=True, stop=True),
                         reads=["B_tok", "xw"], writes=["ps5"])
                if step == 0:
                    p.op("dve", lambda e, d=d: e.tensor_copy(hS[d][:], PS[5][:, 0:384]), reads=["ps5"], writes=["hS%d" % d])
                else:
                    p.op("pool", lambda e, d=d: e.tensor_tensor(tmp2[:, :].rearrange("p (h j) -> p h j", j=64), hS[d][:, :].rearrange("p (h j) -> p h j", j=64),
                                                              el[:, :].unsqueeze(2).to_broadcast([128, 6, 64]), ALU.mult), reads=["hS%d" % d, "el"], writes=["yfin"])
                    p.op("dve", lambda e, d=d: e.tensor_tensor(hS[d][:], tmp2[:], PS[5][:, 0:384], ALU.add), reads=["yfin", "ps5"], writes=["hS%d" % d])
                p.op("act", lambda e, d=d: e.copy(hSb[d][:], hS[d][:]), reads=["hS%d" % d], writes=["hSb%d" % d])

    for ti in range(NTA):
        p.op("dve", lambda e, ti=ti: e.tensor_tensor(yfin[:], xs_tok[:, ti, :], dsk[:], ALU.mult), reads=["xs_tok", "dsk"], writes=["yfin"])
        p.op("dve", lambda e, ti=ti: e.tensor_tensor(yfin[:], yfin[:], yacc[:, ti, :], ALU.add), reads=["yfin", ("yacc", ti)], writes=["yfin"])
        p.op("dve", lambda e, ti=ti: e.tensor_tensor(yfin[:], yfin[:], sz_tok[:, ti, :], ALU.mult), reads=["yfin", "sz_tok"], writes=["yfin"])
        p.op("pool", lambda e: e.tensor_tensor(tmp[:], yfin[:], yfin[:], ALU.mult), reads=["yfin"], writes=["tmp"])
        p.op("dve", lambda e: e.reduce_sum(gss[:, 0:2], tmp[:, :].rearrange("p (g j) -> p g j", j=192), axis=AX.X), reads=["tmp"], writes=["gss"])
        p.op("act", lambda e: e.activation(gss[:, 2:4], gss[:, 0:2], AF.Ln, bias=epsg[:, 0:1], scale=1.0), reads=["gss", "epsg"], writes=["gss"])
        p.op("act", lambda e: e.activation(gss[:, 2:4], gss[:, 2:4], AF.Exp, scale=-0.5), reads=["gss"], writes=["gss"])
        p.op("dve", lambda e: e.tensor_tensor(tmp[:, :].rearrange("p (g j) -> p g j", j=192), yfin[:, :].rearrange("p (g j) -> p g j", j=192),
                                              gss[:, 2:4].unsqueeze(2).to_broadcast([128, 2, 192]), ALU.mult), reads=["yfin", "gss"], writes=["tmp"])
        p.op("dve", lambda e: e.tensor_tensor(yfb[:], tmp[:], nwt[:], ALU.mult), reads=["tmp", "nwt"], writes=["yfb"])
        hh = ti % 2
        for j in range(3):
            p.op("pe", lambda e, j=j, hh=hh: e.transpose(PT[hh][:, j * 128:(j + 1) * 128], yfb[:, j * 128:(j + 1) * 128], identb[:]), reads=["yfb", "identb"], writes=["pt%d" % hh])
        p.op("act", lambda e, hh=hh: e.copy(ytile[:, :, :], PT[hh][:, 0:384].rearrange("p (j t) -> p j t", j=3)), reads=["pt%d" % hh], writes=["ytile"])
        p.dma(yT_d.rearrange("(j p) t -> p j t", p=128)[:, :, ti * 128:(ti + 1) * 128], ytile[:, :, :], reads=["ytile"])
    p.emit()
    return nc
import os
import time
import ml_dtypes
from concourse.bass_utils import run_bass_kernel_spmd

NMIX = 7960
_PROGS = {}
_DBG = {}


def _prog(name):
    if name not in _PROGS:
        _PROGS[name] = {"M": build_M, "na": build_A_na, "hg": build_A_hg, "ssd": build_A_ssd,
                        "B1": lambda: build_B1(9, 2, [3, 3, 3]), "B2x": build_B2x, "B3": build_B3}[name]()
    return _PROGS[name]


def _run(name, in_maps):
    t0 = time.time()
    res = run_bass_kernel_spmd(_prog(name), in_maps, core_ids=list(range(8)))
    if int(os.environ.get("KDEBUG", "0")):
        print("launch", name, "%.1fs" % (time.time() - t0), flush=True)
    return res.results


def lay16(v):
    return np.ascontiguousarray(v.reshape(16, 128).T)


def modc_from(mod_v0, mod_v1, qs):
    out = np.zeros((128, len(qs), 16, 2), np.float32)
    for qi, q in enumerate(qs):
        out[:, qi, :, 0] = lay16(mod_v0[q * 2048:(q + 1) * 2048])
        out[:, qi, :, 1] = lay16(mod_v1[q * 2048:(q + 1) * 2048])
    return out


def gtile(mod_v0, mod_v1, q):
    out = np.empty((128, 2, 2048), np.float32)
    out[:, 0, :] = mod_v0[q * 2048:(q + 1) * 2048][None, :]
    out[:, 1, :] = mod_v1[q * 2048:(q + 1) * 2048][None, :]
    return out


def na_bias_gather(rpb, heads):
    out = np.zeros((128, 8, 2, 6, 64), np.float32)
    pats = [0, 1, 2, 3, 10, 29, 30, 31]
    c = np.arange(64)[:, None]; j = np.arange(64)[None, :]
    dc = np.clip(c - j, -15, 15) + 15
    for pi, r in enumerate(pats):
        srow = min(max(r - 4, 0), 24)
        for hi, h in enumerate(heads):
            for kt in range(4):
                for wl in range(2):
                    dr = srow + 2 * kt + wl - r
                    out[wl * 64:(wl + 1) * 64, pi, hi, kt, :] = rpb[h, dr + 7][dc]
    return out.reshape(128, 8, 2, 384)


def na_maskneg():
    m = np.zeros((128, 6, 64), np.float32)
    c = np.arange(64)[:, None]; j = np.arange(64)[None, :]
    c0 = np.clip(j - 8, 0, 48)
    mm = np.where((c >= c0) & (c < c0 + 16), 0.0, -30000.0).astype(np.float32)
    for kt in range(4):
        m[0:64, kt] = mm; m[64:128, kt] = mm
    return m.reshape(128, 384)


def hg_inputs(w_in, lb_logits, hg_norm_w, L, half):
    heads = [3 * half + i for i in range(3)]
    order = [0, 1, 2, 4, 3]
    w = np.stack([np.concatenate([w_in[:, o * 768 + h * 128: o * 768 + (h + 1) * 128] for o in order], axis=1) for h in heads], axis=0)
    lbl = np.zeros((128, 2, 2, 3), np.float32)
    for l in range(2):
        for dd in range(2):
            for hi, h in enumerate(heads):
                lbl[:, l, dd, hi] = lb_logits[l, dd, h * 128:(h + 1) * 128]
    sel = np.zeros((128, 2), np.float32); sel[:, 1] = 1.0 if L == 1 else 0.0
    s = np.arange(32)[:, None]; t = np.arange(32)[None, :]
    mask = np.stack([(s <= t), (s >= t)], axis=1).astype(np.float32)
    return {"w_hg": np.ascontiguousarray(w), "lbl": lbl, "lbsel": sel, "hg_nw": np.ascontiguousarray(hg_norm_w[:, None]), "hgmask": np.ascontiguousarray(mask)}


def ssd_consts():
    inv = 1.0 / (10000.0 ** (np.arange(0, 64, 2, dtype=np.float32) / 64))
    t = np.arange(2048)
    row = (t // 64).astype(np.float32); col = (t % 64).astype(np.float32)
    cos = np.zeros((128, 2048), np.float32); sin = np.zeros((128, 2048), np.float32)
    for n in range(128):
        ang = (row if n < 64 else col) * inv[n % 32]
        cos[n] = np.cos(ang); sin[n] = np.sin(ang)
    pm = np.zeros((128, 128), np.float32)
    for n2 in range(128):
        if (n2 % 64) < 32:
            pm[n2 + 32, n2] = -1.0
        else:
            pm[n2 - 32, n2] = 1.0
    r = np.arange(128)[:, None]; tt = np.arange(128)[None, :]
    tri = np.stack([(r <= tt), (r >= tt)], axis=1).astype(np.float32)
    mneg = np.stack([np.where(r <= tt, 0.0, -30000.0), np.where(r >= tt, 0.0, -30000.0)], axis=1).astype(np.float32)
    return {"rope_cos": cos, "rope_sin": sin, "rope_pm": pm, "tri": np.ascontiguousarray(tri), "mneg": np.ascontiguousarray(mneg)}


def ssd_inputs(w_in, conv_w, conv_b, dt_bias, a_log, d_skip, norm_w, half, consts):
    heads = [6 * half + i for i in range(6)]
    zc = list(range(5376 + 384 * half, 5376 + 384 * (half + 1)))
    xsc = list(range(6144 + 384 * half, 6144 + 384 * (half + 1)))
    bc = list(range(6912 + 256 * half, 6912 + 256 * (half + 1)))
    cc = list(range(7424 + 256 * half, 7424 + 256 * (half + 1)))
    dtc = list(range(7936 + 6 * half, 7936 + 6 * half + 6)) + list(range(7948 + 6 * half, 7948 + 6 * half + 6))
    conv_ch = [c_ - 6144 for c_ in xsc + bc + cc]
    cwl = np.ascontiguousarray(conv_w[:, conv_ch].reshape(4, 7, 128).transpose(2, 1, 0))
    cbl = np.ascontiguousarray(conv_b[conv_ch].reshape(7, 128).T)
    dtb = np.concatenate([dt_bias[0][heads], dt_bias[1][heads]])
    alog = np.concatenate([a_log[0][heads], a_log[1][heads]])
    dsk = np.repeat(d_skip[heads], 64)
    nw = norm_w[384 * half:384 * (half + 1)]
    out = {"w_z": np.ascontiguousarray(w_in[:, zc]), "w_xbc": np.ascontiguousarray(w_in[:, xsc + bc + cc]), "w_dt": np.ascontiguousarray(w_in[:, dtc]),
           "conv_w": cwl, "conv_b": cbl, "dt_bias": np.ascontiguousarray(np.broadcast_to(dtb, (128, 12))), "a_log": np.ascontiguousarray(np.broadcast_to(alog, (128, 12))),
           "dskip": np.ascontiguousarray(np.broadcast_to(dsk, (128, 384))), "ssd_nw": np.ascontiguousarray(np.broadcast_to(nw, (128, 384)))}
    out.update(consts)
    return out


def kernel(x, c, ctx, c_ctx, w_mod, b_mod, norm1_w, norm2_w, w_in, hg_lb_logits, hg_norm_w,
           na_q_norm_w, na_k_norm_w, na_rpb, ssd_conv_w, ssd_conv_b, ssd_dt_bias, ssd_a_log, ssd_d,
           ssd_norm_w, w_branch_hg, w_branch_na, w_branch_ssd, w_out, moe_w_router, moe_b_router,
           moe_w_gate, moe_b_gate, moe_w_up, moe_b_up, moe_w_down, moe_b_down):
    f32 = lambda a: np.asarray(a, dtype=np.float32)
    x, c, ctx, c_ctx = f32(x), f32(c), f32(ctx), f32(c_ctx)
    dbg = bool(int(os.environ.get("KDEBUG", "0")))
    ident = np.eye(128, dtype=np.float32)
    cvecs = [c[b] for b in range(4)] + [c_ctx]
    cT5 = np.ascontiguousarray(np.stack([lay16(v) for v in cvecs], axis=-1))
    w_mod = f32(w_mod); b_mod = f32(b_mod)
    ims = [{"cT5": cT5, "w_mod": np.ascontiguousarray(w_mod[:, :, k * MCOLS:(k + 1) * MCOLS]),
            "b_mod": np.ascontiguousarray(b_mod[:, None, k * MCOLS:(k + 1) * MCOLS])} for k in range(8)]
    rM = _run("M", ims)
    mod = np.concatenate([np.asarray(rM[k]["mod"]) for k in range(8)], axis=2)
    if dbg:
        _DBG["mod"] = mod
    consts = ssd_consts()
    maskneg = na_maskneg()
    xl = [x[b] for b in range(4)]; xc = [ctx[b] for b in range(4)]
    for L in range(2):
        W_in = f32(w_in[L])
        xcat = [np.ascontiguousarray(np.concatenate([xc[b], xl[b]], axis=0)) for b in range(4)]
        n1T = lay16(f32(norm1_w[L])); n2T = lay16(f32(norm2_w[L]))
        base = [{"x": xcat[k // 2], "modc": modc_from(mod[L, 4], mod[L, k // 2], [0, 1]), "n1T": n1T, "ident": ident} for k in range(8)]
        ims = []
        for k in range(8):
            half = k % 2
            heads = [2 * half, 2 * half + 1]
            cols = [3840 + w_ * 512 + h * 128 + i for w_ in range(3) for h in heads for i in range(128)]
            m = dict(base[k]); m.update({"w_na": np.ascontiguousarray(W_in[:, cols]), "qkw": np.ascontiguousarray(np.stack([f32(na_q_norm_w[L]), f32(na_k_norm_w[L])], axis=1)),
                                         "bias_g": na_bias_gather(f32(na_rpb[L]), heads), "maskneg": maskneg})
            ims.append(m)
        r_na = _run("na", ims)
        ims = []
        for k in range(8):
            m = dict(base[k]); m.update(hg_inputs(W_in, f32(hg_lb_logits), f32(hg_norm_w[L]), L, k % 2)); ims.append(m)
        r_hg = _run("hg", ims)
        ims = []
        for k in range(8):
            m = dict(base[k]); m.update(ssd_inputs(W_in, f32(ssd_conv_w[L]), f32(ssd_conv_b[L]), f32(ssd_dt_bias[L]), f32(ssd_a_log[L]), f32(ssd_d[L]), f32(ssd_norm_w[L]), k % 2, consts)); ims.append(m)
        r_ssd = _run("ssd", ims)
        yT = [np.concatenate([np.asarray(r_hg[2 * b]["yT"]), np.asarray(r_hg[2 * b + 1]["yT"]), np.asarray(r_na[2 * b]["yT"]), np.asarray(r_na[2 * b + 1]["yT"]),
                              np.asarray(r_ssd[2 * b]["yT"]), np.asarray(r_ssd[2 * b + 1]["yT"])], axis=0) for b in range(4)]
        if dbg:
            _DBG["yT%d" % L] = [np.asarray(y).astype(np.float32) for y in yT]
        wgi = np.ascontiguousarray(W_in[:, NMIX:])
        wb = np.ascontiguousarray(np.concatenate([f32(w_branch_hg[L]), f32(w_branch_na[L]), f32(w_branch_ssd[L])], axis=0))
        ims = []
        for k in range(8):
            b, half = k // 2, k % 2
            sl = slice(1152 * half, 1152 * (half + 1))
            v0 = mod[L, 4] if half == 0 else mod[L, b]
            ims.append({"x": np.ascontiguousarray(xcat[b][sl]), "yT": np.ascontiguousarray(yT[b][:, sl]), "modc4": modc_from(v0, mod[L, b], [0, 1, 3, 4]),
                        "G1": gtile(v0, mod[L, b], 2), "n1T": n1T, "n2T": n2T, "wgi": wgi, "wb": wb, "wo": f32(w_out[L]),
                        "wr": f32(moe_w_router[L]), "br": f32(moe_b_router[L])[None, :], "ident": ident})
        r_b1 = _run("B1", ims)
        if dbg:
            _DBG["xmid%d" % L] = [np.asarray(r_b1[k]["xmid"]) for k in range(8)]
        h2T_all = np.ascontiguousarray(np.concatenate([np.asarray(r_b1[k]["h2T"]) for k in range(8)], axis=1))
        rw_all = np.concatenate([np.asarray(r_b1[k]["rw"]) for k in range(8)], axis=0)
        ims = []
        for k in range(8):
            es = slice(4 * k, 4 * k + 4)
            rwo = np.ascontiguousarray(rw_all[:, es])
            ims.append({"h2T": h2T_all, "rw": rwo, "rwT": np.ascontiguousarray(rwo.T), "wg": f32(moe_w_gate[L][es]), "wu": f32(moe_w_up[L][es]), "wd": f32(moe_w_down[L][es]),
                        "bgT": np.ascontiguousarray(f32(moe_b_gate[L][es]).reshape(4, 16, 128).transpose(2, 0, 1)),
                        "buT": np.ascontiguousarray(f32(moe_b_up[L][es]).reshape(4, 16, 128).transpose(2, 0, 1)), "bd": f32(moe_b_down[L][es])})
        r_b2 = _run("B2x", ims)
        ims = []
        for k in range(8):
            b, half = k // 2, k % 2
            v0 = mod[L, 4] if half == 0 else mod[L, b]
            parts = np.ascontiguousarray(np.stack([np.asarray(r_b2[j]["part"])[k * 1152:(k + 1) * 1152] for j in range(8)], axis=0))
            ims.append({"parts": parts, "xmid": np.asarray(r_b1[k]["xmid"]), "G2": gtile(v0, mod[L, b], 5)})
        r_b3 = _run("B3", ims)
        for b in range(4):
            full = np.concatenate([np.asarray(r_b3[2 * b]["out"]), np.asarray(r_b3[2 * b + 1]["out"])], axis=0)
            xc[b] = full[:256]; xl[b] = full[256:]
        if dbg:
            _DBG["xout%d" % L] = [a.copy() for a in xl]; _DBG["xcout%d" % L] = [a.copy() for a in xc]
    return np.stack(xl, axis=0).astype(np.float32)
```

```python
import numpy as np
from contextlib import ExitStack
import concourse.bass as bass
import concourse.mybir as mybir

F32 = mybir.dt.float32
BF16 = mybir.dt.bfloat16
AF = mybir.ActivationFunctionType
ALU = mybir.AluOpType
AX = mybir.AxisListType

COMPUTE = ("pe", "act", "dve", "pool")
ENGINES = ("pe", "act", "dve", "pool", "sp")
N_DMA_SEMS = 48


class Prog:
    def __init__(self, nc):
        self.nc = nc
        self.es = ExitStack()
        self.ops = {e: [] for e in ENGINES}
        self.count = {e: 0 for e in ENGINES}
        self.waited = {e: {} for e in ENGINES}
        self.state = {}
        self.desc = {}
        self.sems = {}
        for e in COMPUTE:
            self.sems[e] = self.es.enter_context(nc.semaphore("s_" + e))
        self.dma_sems = []
        self.dma_uses = []
        for i in range(N_DMA_SEMS):
            self.sems[("d", i)] = self.es.enter_context(nc.semaphore("s_d%d" % i))
            self.dma_uses.append(0)
        self.dma_rr = 0
        self.all_dma_tokens = []
        self.ntiles = 0

    def sb(self, shape, dtype, name=None):
        self.ntiles += 1
        name = "sb_" + (name or ("t%d" % self.ntiles))
        return self.es.enter_context(self.nc.sbuf_tensor(name, list(shape), dtype))

    def ps(self, shape, dtype, name=None):
        self.ntiles += 1
        name = "pp_" + (name or ("p%d" % self.ntiles))
        return self.es.enter_context(self.nc.psum_tensor(name, list(shape), dtype))

    def _conflicts(self, key):
        out = []
        for i in range(1, len(key) + 1):
            st = self.state.get(key[:i])
            if st is not None:
                out.append(st)
        for k in self.desc.get(key, ()):
            st = self.state.get(k)
            if st is not None:
                out.append(st)
        return out

    def _touch(self, key):
        if key not in self.state:
            self.state[key] = [None, {}]
            for i in range(1, len(key)):
                self.desc.setdefault(key[:i], set()).add(key)
        return self.state[key]

    def op(self, engine, fn, reads=(), writes=(), dma=False):
        reads = [k if isinstance(k, tuple) else (k,) for k in reads]
        writes = [k if isinstance(k, tuple) else (k,) for k in writes]
        for k in list(reads):
            if isinstance(k[0], str) and (k[0].startswith("ps") or k[0].startswith("pt")) and k not in writes:
                writes.append(k)
        reads = [k for k in reads if k not in writes]
        deps = {}

        def add(tok, is_writer):
            semkey, val, eng = tok
            if not dma and eng == engine and not isinstance(semkey, tuple):
                if engine == "pe":
                    return
                if not is_writer:
                    return
            if deps.get(semkey, 0) < val:
                deps[semkey] = val

        for k in reads:
            for st in self._conflicts(k):
                if st[0] is not None:
                    add(st[0], True)
        for k in writes:
            for st in self._conflicts(k):
                if st[0] is not None:
                    add(st[0], True)
                for sk, (v, e) in st[1].items():
                    add((sk, v, e), False)
        if dma:
            i = self.dma_rr
            self.dma_rr = (self.dma_rr + 1) % N_DMA_SEMS
            prev = self.dma_uses[i]
            if prev > 0:
                if deps.get(("d", i), 0) < 16 * prev:
                    deps[("d", i)] = 16 * prev
            self.dma_uses[i] = prev + 1
            tok = (("d", i), 16 * (prev + 1), engine)
            inc = 16
            self.all_dma_tokens.append(tok)
        else:
            self.count[engine] += 1
            tok = (engine, self.count[engine], engine)
            inc = 1
        waits = []
        w = self.waited[engine]
        for sk, v in deps.items():
            if w.get(sk, 0) < v:
                w[sk] = v
                waits.append((sk, v))
        self.ops[engine].append((fn, waits, tok[0], inc))
        for k in reads:
            st = self._touch(k)
            st[1][tok[0]] = (tok[1], engine)
        for k in writes:
            st = self._touch(k)
            for kk in list(self.desc.get(k, ())):
                self.state.pop(kk, None)
            st[0] = tok
            st[1] = {}
        return tok

    def dma(self, out, in_, reads=(), writes=(), q="sp", **kw):
        return self.op(q, lambda eng: eng.dma_start(out=out, in_=in_, **kw), reads, writes, dma=True)

    def finish(self):
        w = self.waited["sp"]
        waits = []
        for i in range(N_DMA_SEMS):
            v = 16 * self.dma_uses[i]
            if v > 0 and w.get(("d", i), 0) < v:
                waits.append((("d", i), v))
                w[("d", i)] = v
        self.ops["sp"].append((None, waits, None, 0))

    def emit(self):
        nc = self.nc
        self.finish()
        engmap = {"pe": "tensor", "act": "scalar", "dve": "vector", "pool": "gpsimd", "sp": "sync"}
        with nc.Block() as block:
            for e in ENGINES:
                ops = self.ops[e]
                if not ops:
                    continue

                def body(eng, ops=ops):
                    for fn, waits, semkey, inc in ops:
                        for sk, v in waits:
                            eng.wait_ge(self.sems[sk], v)
                        if fn is not None:
                            ins = fn(eng)
                            ins.then_inc(self.sems[semkey], inc)

                getattr(block, engmap[e])(body)
        self.es.close()
D = 2048
KC = 16
EPS = 1e-6
NE = 32


def v16(buf):
    return buf[:, :].rearrange("p (k n) -> p k n", k=KC)


def wblock(dram2d, c0, w=512):
    return dram2d.rearrange("(k p) n -> p k n", p=128)[:, :, c0:c0 + w]


def run_stream(p, ring, blocks):
    R = len(ring)

    def load(j):
        ap, vf, _ = blocks[j]
        p.dma(vf(ring[j % R]), ap, writes=[("ring", j % R)], q="pool")

    for j in range(min(R, len(blocks))):
        load(j)
    for j, (ap, vf, use) in enumerate(blocks):
        use(ring[j % R], ("ring", j % R))
        if j + R < len(blocks):
            load(j + R)


def emit_modcols(p, blocks, wmod_d, QS, scT, bmodT, modc, PS):
    for qi, q in enumerate(QS):
        for cb in range(4):
            def use(buf, key, qi=qi, q=q, cb=cb):
                bv = v16(buf)
                ps = PS[(qi * 4 + cb) % 2]
                pk = "ps%d" % ((qi * 4 + cb) % 2)
                for j in range(4):
                    for kc in range(KC):
                        p.op("pe", lambda e, kc=kc, j=j: e.matmul(ps[:, j * 2:j * 2 + 2], lhsT=bv[:, kc, j * 128:(j + 1) * 128],
                                                               rhs=scT[:, kc, :], start=(kc == 0), stop=(kc == KC - 1)),
                             reads=[key, "scT"], writes=[pk])
                for j in range(4):
                    dc = cb * 4 + j
                    p.op("dve", lambda e, j=j, dc=dc: e.tensor_scalar(modc[:, qi, dc, :], ps[:, j * 2:j * 2 + 2],
                                                                     bmodT[:, q * 16 + dc: q * 16 + dc + 1], None, ALU.add),
                         reads=[pk, "bmodT"], writes=[("modc", qi)])
            blocks.append((wblock(wmod_d, q * D + cb * 512), v16, use))


def finish_modc(p, modc, pairs):
    for qi, nT, nk in pairs:
        for v in range(2):
            p.op("dve", lambda e, qi=qi, v=v: e.tensor_scalar(modc[:, qi, :, v], modc[:, qi, :, v], 1.0, float(np.sqrt(D)), ALU.add, ALU.mult),
                 reads=[("modc", qi)], writes=[("modc", qi)])
            p.op("dve", lambda e, qi=qi, v=v, nT=nT: e.tensor_tensor(modc[:, qi, :, v], modc[:, qi, :, v], nT[:], ALU.mult),
                 reads=[("modc", qi), nk], writes=[("modc", qi)])


def emit_gblocks(p, blocks, wmod_d, q, Gt, gname, scT, onesb, bmodb, bi, PS):
    for cb in range(4):
        def use(buf, key, cb=cb):
            bv = v16(buf)
            for v in range(2):
                ps = PS[2 + v]; pk = "ps%d" % (2 + v)
                for kc in range(KC):
                    p.op("pe", lambda e, kc=kc, v=v, ps=ps: e.matmul(ps[:, :], lhsT=scT[:, kc, v:v + 1].to_broadcast([128, 128]), rhs=bv[:, kc, :],
                                                                  start=(kc == 0), stop=False), reads=[key, "scT"], writes=[pk])
                p.op("pe", lambda e, v=v, ps=ps: e.matmul(ps[:, :], lhsT=onesb[0:1, :], rhs=bmodb[0:1, bi, cb * 512:(cb + 1) * 512],
                                                       start=False, stop=True), reads=["onesb", "bmodb"], writes=[pk])
                p.op("act", lambda e, v=v, ps=ps: e.copy(Gt[v][:, cb * 512:(cb + 1) * 512], ps[:, :]), reads=[pk], writes=[(gname, v)])
        blocks.append((wblock(wmod_d, q * D + cb * 512), v16, use))


def norm_to_T(p, xtile, xkey, modc, qi_sh, qi_A, v, dstT, dkey, col0, small, xn, identb, PT, epsc):
    ss = small[:, 0:1]; rstd = small[:, 1:2]
    p.op("act", lambda e: e.activation(xn[1][:], xtile, AF.Square, accum_out=ss), reads=[xkey], writes=["xn1", ("small", 0)])
    p.op("act", lambda e: e.activation(small[:, 2:3], ss, AF.Ln, bias=epsc[:, 0:1], scale=1.0), reads=[("small", 0), "epsc"], writes=[("small", 2)])
    p.op("act", lambda e: e.activation(rstd, small[:, 2:3], AF.Exp, scale=-0.5), reads=[("small", 2)], writes=[("small", 1)])
    xb = xn[0]
    p.op("dve", lambda e: e.tensor_scalar(xb[:], xtile, rstd, None, ALU.mult), reads=[xkey, ("small", 1)], writes=["xn0"])
    for hh in range(2):
        for j in range(8):
            kc = hh * 8 + j
            p.op("pe", lambda e, kc=kc, j=j, hh=hh: e.transpose(PT[hh][:, j * 128:(j + 1) * 128], xb[:, kc * 128:(kc + 1) * 128], identb[:]),
                 reads=["xn0", "identb"], writes=["pt%d" % hh])
        for j in range(8):
            kc = hh * 8 + j
            p.op("act", lambda e, kc=kc, j=j, hh=hh: e.activation(dstT[:, kc, col0:col0 + 128], PT[hh][:, j * 128:(j + 1) * 128], AF.Identity,
                                                                  bias=modc[:, qi_sh, kc, v:v + 1], scale=modc[:, qi_A, kc, v:v + 1]),
                 reads=["pt%d" % hh, ("modc", qi_sh), ("modc", qi_A)], writes=[dkey])


def build_B1(NT, n_a, TBS):
    T = NT * 128
    nc = bass.Bass("TRN2", target_bir_lowering=False)

    def din(name, shape, dt=F32):
        return nc.dram_tensor(name, list(shape), dt, kind="ExternalInput").ap()

    x_d = din("x", [T, D]); yT_d = din("yT", [D, T], BF16)
    modc_d = din("modc4", [128, 4, KC, 2]); g1_d = din("G1", [128, 2, D])
    n1T_d = din("n1T", [128, KC]); n2T_d = din("n2T", [128, KC])
    wgi_d = din("wgi", [D, 3 * D]); wb_d = din("wb", [D, D]); wo_d = din("wo", [D, D])
    wr_d = din("wr", [D, NE]); br_d = din("br", [1, NE])
    ident_d = din("ident", [128, 128])
    xmid_d = nc.dram_tensor("xmid", [T, D], F32, kind="ExternalOutput").ap()
    h2T_d = nc.dram_tensor("h2T", [D, T], BF16, kind="ExternalOutput").ap()
    rw_d = nc.dram_tensor("rw", [T, NE], F32, kind="ExternalOutput").ap()

    p = Prog(nc)
    RING = 3
    ring = [p.sb([128, 8192], BF16, "ring%d" % i) for i in range(RING)]
    identb = p.sb([128, 128], BF16, "identb")
    onesb = p.sb([1, 128], BF16, "onesb")
    n1T = p.sb([128, KC], F32, "n1T"); n2T = p.sb([128, KC], F32, "n2T")
    modc = p.sb([128, 4, KC, 2], F32, "modc")
    wrb = p.sb([128, KC, NE], BF16, "wrb"); brb = p.sb([1, NE], BF16, "brb")
    small = p.sb([128, 64], F32, "small")
    epsc = p.sb([128, 1], F32, "epsc")
    p.op("dve", lambda e: e.memset(epsc[:], float(D * EPS)), writes=["epsc"])
    xn = [p.sb([128, D], BF16, "xn%d" % i) for i in range(2)]
    G1 = [p.sb([128, D], F32, "G1_%d" % i) for i in range(2)]
    hT_blk = p.sb([128, KC, 384], BF16, "hT_blk")
    yT_blk = p.sb([128, KC, 384], BF16, "yT_blk")
    mT_blk = p.sb([128, KC, 384], BF16, "mT_blk")
    h2T_blk = p.sb([128, KC, 384], BF16, "h2T_blk")
    gsig = p.sb([128, 3, 4, 384], F32, "gsig")
    xres = p.sb([128, 3, D], F32, "xres"); xmid = p.sb([128, 3, D], F32, "xmid")
    macc = p.sb([128, 384], F32, "macc"); mtmp = p.sb([128, 384], F32, "mtmp")
    lg = p.sb([128, NE], F32, "lg"); mx8 = p.sb([128, 8], F32, "mx8"); rwt = p.sb([128, 3, NE], F32, "rwt"); msk = p.sb([128, NE], F32, "msk")
    PS = [p.ps([128, 512], F32, "ps%d" % i) for i in range(6)]
    PT = [p.ps([128, 1024], BF16, "pt%d" % i) for i in range(2)]

    p.dma(identb[:], ident_d, writes=["identb"], q="pool")
    p.op("dve", lambda e: e.memset(onesb[:], 1.0), writes=["onesb"])
    p.dma(modc[:], modc_d, writes=["modc"])
    for v in range(2):
        p.dma(G1[v][:], g1_d[:, v, :], writes=[("G1", v)])
    p.dma(n1T[:], n1T_d, writes=["n1T"]); p.dma(n2T[:], n2T_d, writes=["n2T"])
    p.dma(wrb[:], wr_d.rearrange("(k p) n -> p k n", p=128), writes=["wrb"], q="pool")
    p.dma(brb[:], br_d, writes=["brb"], q="pool")

    blocks = []

    BR = [(0, 6), (6, 4), (10, 6)]
    t0 = 0
    for bi, nb in enumerate(TBS):
        TB = nb * 128
        tiles = list(range(t0, t0 + nb))
        c0 = t0 * 128

        def pre(bi=bi, nb=nb, TB=TB, tiles=tiles, c0=c0):
            if bi == 0:
                finish_modc(p, modc, [(1, n1T, "n1T"), (3, n2T, "n2T")])
            p.dma(yT_blk[:, :, 0:TB], yT_d.rearrange("(k p) t -> p k t", p=128)[:, :, c0:c0 + TB], writes=["yT_blk"])
            for li, ti in enumerate(tiles):
                v = 0 if ti < n_a else 1
                p.dma(xres[:, li, :], x_d[ti * 128:(ti + 1) * 128, :], writes=[("xres", li)])
                norm_to_T(p, xres[:, li, :], ("xres", li), modc, 0, 1, v, hT_blk, "hT_blk", li * 128, small, xn, identb, PT, epsc)

        for cb in range(4):
            for br in range(3):
                def use(buf, key, bi=bi, cb=cb, br=br, TB=TB, pre=pre):
                    if cb == 0 and br == 0:
                        pre()
                    bv = v16(buf)
                    for j in range(4):
                        ps = PS[j % 2]; pk = "ps%d" % (j % 2)
                        for kc in range(KC):
                            p.op("pe", lambda e, kc=kc, j=j, ps=ps: e.matmul(ps[:, 0:TB], lhsT=bv[:, kc, j * 128:(j + 1) * 128], rhs=hT_blk[:, kc, 0:TB],
                                                                          start=(kc == 0), stop=(kc == KC - 1)), reads=[key, "hT_blk"], writes=[pk])
                        p.op("act", lambda e, j=j, ps=ps, br=br: e.activation(gsig[:, br, j, 0:TB], ps[:, 0:TB], AF.Sigmoid), reads=[pk], writes=[("gsig", br, j)])
                blocks.append((wblock(wgi_d, br * D + cb * 512), v16, use))

            def use(buf, key, bi=bi, cb=cb, TB=TB):
                bv = v16(buf)
                for j in range(4):
                    dc = cb * 4 + j
                    for br in range(3):
                        k0, nk = BR[br]
                        ps = PS[2 + (br % 2)]; pk = "ps%d" % (2 + (br % 2))
                        for kk in range(nk):
                            p.op("pe", lambda e, kk=kk, k0=k0, nk=nk, j=j, ps=ps: e.matmul(ps[:, 0:TB], lhsT=bv[:, k0 + kk, j * 128:(j + 1) * 128], rhs=yT_blk[:, k0 + kk, 0:TB],
                                                                                      start=(kk == 0), stop=(kk == nk - 1)), reads=[key, "yT_blk"], writes=[pk])
                        if br == 0:
                            p.op("dve", lambda e, j=j, ps=ps: e.tensor_tensor(macc[:, 0:TB], ps[:, 0:TB], gsig[:, 0, j, 0:TB], ALU.mult), reads=[pk, ("gsig", 0, j)], writes=["macc"])
                        else:
                            p.op("dve", lambda e, j=j, ps=ps, br=br: e.tensor_tensor(mtmp[:, 0:TB], ps[:, 0:TB], gsig[:, br, j, 0:TB], ALU.mult), reads=[pk, ("gsig", br, j)], writes=["mtmp"])
                            if br == 1:
                                p.op("dve", lambda e: e.tensor_tensor(macc[:, 0:TB], macc[:, 0:TB], mtmp[:, 0:TB], ALU.add), reads=["macc", "mtmp"], writes=["macc"])
                            else:
                                p.op("dve", lambda e, dc=dc: e.tensor_tensor(mT_blk[:, dc, 0:TB], macc[:, 0:TB], mtmp[:, 0:TB], ALU.add), reads=["macc", "mtmp"], writes=["mT_blk"])
            blocks.append((wblock(wb_d, cb * 512), v16, use))

        for cb in range(4):
            def use(buf, key, bi=bi, cb=cb, nb=nb, tiles=tiles, TB=TB, c0=c0):
                bv = v16(buf)
                for li, ti in enumerate(tiles):
                    v = 0 if ti < n_a else 1
                    ps = PS[4 + (li % 2)]; pk = "ps%d" % (4 + (li % 2))
                    for kc in range(KC):
                        p.op("pe", lambda e, kc=kc, li=li, ps=ps: e.matmul(ps[:, :], lhsT=mT_blk[:, kc, li * 128:(li + 1) * 128], rhs=bv[:, kc, :],
                                                                         start=(kc == 0), stop=(kc == KC - 1)), reads=[key, "mT_blk"], writes=[pk])
                    xm = xmid[:, li, cb * 512:(cb + 1) * 512]
                    p.op("dve", lambda e, ps=ps, v=v, xm=xm: e.tensor_tensor(xm, ps[:, :], G1[v][:, cb * 512:(cb + 1) * 512], ALU.mult), reads=[pk, ("G1", v)], writes=[("xmid", li, cb)])
                    p.op("pool", lambda e, xm=xm, li=li: e.tensor_tensor(xm, xm, xres[:, li, cb * 512:(cb + 1) * 512], ALU.add), reads=[("xmid", li, cb), ("xres", li)], writes=[("xmid", li, cb)])
                if cb == 3:
                    for li, ti in enumerate(tiles):
                        v = 0 if ti < n_a else 1
                        p.dma(xmid_d[ti * 128:(ti + 1) * 128, :], xmid[:, li, :], reads=[("xmid", li)])
                        norm_to_T(p, xmid[:, li, :], ("xmid", li), modc, 2, 3, v, h2T_blk, "h2T_blk", li * 128, small, xn, identb, PT, epsc)
                    p.dma(h2T_d.rearrange("(k p) t -> p k t", p=128)[:, :, c0:c0 + TB], h2T_blk[:, :, 0:TB], reads=["h2T_blk"])
                    for li, ti in enumerate(tiles):
                        ps = PS[li % 2]; pk = "ps%d" % (li % 2)
                        for kc in range(KC):
                            p.op("pe", lambda e, kc=kc, li=li, ps=ps: e.matmul(ps[:, 0:NE], lhsT=h2T_blk[:, kc, li * 128:(li + 1) * 128], rhs=wrb[:, kc, :],
                                                                             start=(kc == 0), stop=False), reads=["h2T_blk", "wrb"], writes=[pk])
                        p.op("pe", lambda e, ps=ps: e.matmul(ps[:, 0:NE], lhsT=onesb[0:1, :], rhs=brb[0:1, :], start=False, stop=True), reads=["onesb", "brb"], writes=[pk])
                        p.op("dve", lambda e, ps=ps: e.tensor_copy(lg[:], ps[:, 0:NE]), reads=[pk], writes=["lg"])
                        p.op("dve", lambda e: e.max(out=mx8[:], in_=lg[:]), reads=["lg"], writes=["mx8"])
                        p.op("dve", lambda e: e.tensor_scalar(msk[:], lg[:], mx8[:, 3:4], None, ALU.is_ge), reads=["lg", "mx8"], writes=["msk"])
                        p.op("dve", lambda e: e.tensor_scalar(small[:, 4:5], mx8[:, 0:1], -1.0, None, ALU.mult), reads=["mx8"], writes=[("small", 4)])
                        p.op("act", lambda e: e.activation(lg[:], lg[:], AF.Exp, bias=small[:, 4:5], scale=1.0), reads=["lg", ("small", 4)], writes=["lg"])
                        p.op("dve", lambda e: e.tensor_tensor(lg[:], lg[:], msk[:], ALU.mult), reads=["lg", "msk"], writes=["lg"])
                        p.op("dve", lambda e: e.reduce_sum(small[:, 5:6], lg[:], axis=AX.X), reads=["lg"], writes=[("small", 5)])
                        p.op("dve", lambda e: e.reciprocal(small[:, 6:7], small[:, 5:6]), reads=[("small", 5)], writes=[("small", 6)])
                        p.op("dve", lambda e, li=li: e.tensor_scalar(rwt[:, li, :], lg[:], small[:, 6:7], None, ALU.mult), reads=["lg", ("small", 6)], writes=[("rwt", li)])
                        p.dma(rw_d[ti * 128:(ti + 1) * 128, :], rwt[:, li, :], reads=[("rwt", li)])
            blocks.append((wblock(wo_d, cb * 512), v16, use))
        t0 += nb
    run_stream(p, ring, blocks)
    p.emit()
    return nc
MCOLS = 6 * D // 8


def build_M():
    nc = bass.Bass("TRN2", target_bir_lowering=False)

    def din(name, shape, dt=F32):
        return nc.dram_tensor(name, list(shape), dt, kind="ExternalInput").ap()
    cT_d = din("cT5", [128, KC, 5]); w_d = din("w_mod", [2, D, MCOLS]); b_d = din("b_mod", [2, 1, MCOLS])
    out_d = nc.dram_tensor("mod", [2, 5, MCOLS], F32, kind="ExternalOutput").ap()
    p = Prog(nc)
    ring = [p.sb([128, 8192], BF16, "ring%d" % i) for i in range(3)]
    cT = p.sb([128, KC, 5], F32, "cT"); scT = p.sb([128, KC, 5], BF16, "scT")
    onesb = p.sb([1, 8], BF16, "onesb"); bb = p.sb([1, 2, MCOLS], BF16, "bb")
    res = p.sb([5, 2, MCOLS], F32, "res")
    PS = [p.ps([128, 512], F32, "ps%d" % i) for i in range(2)]
    p.op("dve", lambda e: e.memset(onesb[:], 1.0), writes=["onesb"])
    p.dma(cT[:], cT_d, writes=["cT"])
    p.op("act", lambda e: e.activation(scT[:], cT[:], AF.Silu), reads=["cT"], writes=["scT"])
    for l in range(2):
        p.dma(bb[:, l, :], b_d[l], writes=["bb"], q="pool")
    blocks = []
    for l in range(2):
        for cb in range(3):
            def use(buf, key, l=l, cb=cb):
                bv = v16(buf)
                ps = PS[(l * 3 + cb) % 2]; pk = "ps%d" % ((l * 3 + cb) % 2)
                for kc in range(KC):
                    p.op("pe", lambda e, kc=kc, ps=ps: e.matmul(ps[0:5, :], lhsT=scT[:, kc, :], rhs=bv[:, kc, :], start=(kc == 0), stop=False), reads=[key, "scT"], writes=[pk])
                p.op("pe", lambda e, ps=ps: e.matmul(ps[0:5, :], lhsT=onesb[0:1, 0:5], rhs=bb[0:1, l, cb * 512:(cb + 1) * 512], start=False, stop=True), reads=["onesb", "bb"], writes=[pk])
                p.op("act", lambda e, ps=ps: e.copy(res[:, l, cb * 512:(cb + 1) * 512], ps[0:5, :]), reads=[pk], writes=["res"])
            blocks.append((wblock(w_d[l], cb * 512), v16, use))
    run_stream(p, ring, blocks)
    for l in range(2):
        p.dma(out_d[l], res[:, l, :], reads=["res"])
    p.emit()
    return nc


FB = 256
NFB = D // FB
SW_ALPHA = 1.702
SW_LIM = 7.0
NEL = 4
NCHUNK = 8
CT_ = 1152


def build_B2x():
    NT = 9
    TALL = NCHUNK * CT_
    nc = bass.Bass("TRN2", target_bir_lowering=False)

    def din(name, shape, dt=F32):
        return nc.dram_tensor(name, list(shape), dt, kind="ExternalInput").ap()
    h2T_d = din("h2T", [D, TALL], BF16); rw_d = din("rw", [TALL, NEL]); rwT_d = din("rwT", [NEL, TALL])
    wg_d = din("wg", [NEL, D, D]); wu_d = din("wu", [NEL, D, D]); wd_d = din("wd", [NEL, D, D])
    bgT_d = din("bgT", [128, NEL, KC]); buT_d = din("buT", [128, NEL, KC]); bd_d = din("bd", [NEL, D])
    out_d = nc.dram_tensor("part", [TALL, D], F32, kind="ExternalOutput").ap()

    p = Prog(nc)
    acc = p.sb([128, NT, D], F32, "acc")
    h2T = p.sb([128, KC, CT_], BF16, "h2T")
    ring = [p.sb([128, 8192], BF16, "ring%d" % i) for i in range(4)]
    actT = [p.sb([128, 2, CT_], BF16, "actT%d" % i) for i in range(2)]
    tg = [p.sb([128, 384], F32, "tg%d" % i) for i in range(2)]
    tsg = [p.sb([128, 384], F32, "tsg%d" % i) for i in range(2)]
    tu = [p.sb([128, 384], F32, "tu%d" % i) for i in range(2)]
    bgT = p.sb([128, NEL, KC], F32, "bgT"); buT = p.sb([128, NEL, KC], F32, "buT")
    rw = p.sb([128, NCHUNK * NT, NEL], F32, "rw"); rwT = p.sb([NEL, CT_], F32, "rwT")
    bdf = [p.sb([NEL, 512], F32, "bdf%d" % i) for i in range(2)]
    pending = []
    PS = [p.ps([128, 512], F32, "ps%d" % i) for i in range(8)]
    p.dma(bgT[:], bgT_d, writes=["bgT"]); p.dma(buT[:], buT_d, writes=["buT"])
    p.dma(rw[:], rw_d.rearrange("(n p) e -> p n e", p=128), writes=["rw"])
    tbs = [(0, 384), (384, 384), (768, 384)]

    def gu_view(buf):
        return buf[:, :].rearrange("p (g k n) -> p g k n", g=2, k=KC)

    def d_view(buf):
        return buf[:, 0:2 * D].rearrange("p (k n) -> p k n", k=2)

    blocks = []
    unit = [0]
    for ck in range(NCHUNK):
        tok0 = ck * CT_

        def chunk_pre(ck=ck, tok0=tok0):
            while pending:
                pending.pop(0)()
            for kc in range(KC):
                p.dma(h2T[:, kc, :], h2T_d[kc * 128:(kc + 1) * 128, tok0:tok0 + CT_], writes=[("h2T", kc)])
            p.dma(rwT[:], rwT_d[:, tok0:tok0 + CT_], writes=["rwT"])
            n = 0
            for cb in range(4):
                bp = bdf[cb % 2]; bk = "bdf%d" % (cb % 2)
                p.dma(bp[:], bd_d[:, cb * 512:(cb + 1) * 512], writes=[bk])
                for ti in range(NT):
                    ps = PS[4 + (n % 4)]; pk = "ps%d" % (4 + (n % 4)); n += 1
                    p.op("pe", lambda e, ti=ti, ps=ps, bp=bp: e.matmul(ps[:, :], lhsT=rwT[:, ti * 128:(ti + 1) * 128], rhs=bp[:, :], start=True, stop=True),
                         reads=["rwT", bk], writes=[pk])
                    p.op("act", lambda e, ti=ti, ps=ps, cb=cb: e.copy(acc[:, ti, cb * 512:(cb + 1) * 512], ps[:, :]), reads=[pk], writes=[("acc", ti, cb)])

        for ex in range(NEL):
            for fb in range(NFB):
                first = (ex == 0 and fb == 0)
                last = (ex == NEL - 1 and fb == NFB - 1)

                def use_gu(buf, key, ex=ex, fb=fb, first=first, chunk_pre=chunk_pre):
                    if first:
                        chunk_pre()
                    bv = gu_view(buf)
                    at = actT[(ex * NFB + fb) % 2]; ak = "actT%d" % ((ex * NFB + fb) % 2)
                    for j in range(2):
                        fc = fb * 2 + j
                        for (c0, TB) in tbs:
                            u = unit[0]; unit[0] += 1
                            pg = PS[u % 2]; pgk = "ps%d" % (u % 2)
                            pu = PS[2 + u % 2]; puk = "ps%d" % (2 + u % 2)
                            for kc in range(KC):
                                p.op("pe", lambda e, kc=kc, j=j, pg=pg, c0=c0, TB=TB: e.matmul(pg[:, 0:TB], lhsT=bv[:, 0, kc, j * 128:(j + 1) * 128], rhs=h2T[:, kc, c0:c0 + TB],
                                                                                             start=(kc == 0), stop=(kc == KC - 1)), reads=[key, ("h2T", kc)], writes=[pgk])
                            for kc in range(KC):
                                p.op("pe", lambda e, kc=kc, j=j, pu=pu, c0=c0, TB=TB: e.matmul(pu[:, 0:TB], lhsT=bv[:, 1, kc, j * 128:(j + 1) * 128], rhs=h2T[:, kc, c0:c0 + TB],
                                                                                             start=(kc == 0), stop=(kc == KC - 1)), reads=[key, ("h2T", kc)], writes=[puk])
                            s = u % 2
                            g_, sg_, u_ = tg[s], tsg[s], tu[s]
                            p.op("dve", lambda e, pg=pg, g_=g_, TB=TB, fc=fc: e.tensor_scalar(g_[:, 0:TB], pg[:, 0:TB], bgT[:, ex, fc:fc + 1], SW_LIM, ALU.add, ALU.min),
                                 reads=[pgk, "bgT"], writes=["tg%d" % s])
                            p.op("act", lambda e, g_=g_, sg_=sg_, TB=TB: e.activation(sg_[:, 0:TB], g_[:, 0:TB], AF.Sigmoid, scale=SW_ALPHA), reads=["tg%d" % s], writes=["tsg%d" % s])
                            p.op("dve", lambda e, pu=pu, u_=u_, TB=TB, fc=fc: e.tensor_scalar(u_[:, 0:TB], pu[:, 0:TB], buT[:, ex, fc:fc + 1], SW_LIM, ALU.add, ALU.min),
                                 reads=[puk, "buT"], writes=["tu%d" % s])
                            p.op("dve", lambda e, u_=u_, TB=TB: e.tensor_scalar(u_[:, 0:TB], u_[:, 0:TB], -SW_LIM, 1.0, ALU.max, ALU.add), reads=["tu%d" % s], writes=["tu%d" % s])
                            p.op("pool", lambda e, g_=g_, sg_=sg_, TB=TB: e.tensor_tensor(sg_[:, 0:TB], g_[:, 0:TB], sg_[:, 0:TB], ALU.mult), reads=["tg%d" % s, "tsg%d" % s], writes=["tsg%d" % s])
                            p.op("pool", lambda e, u_=u_, sg_=sg_, TB=TB, at=at, j=j, c0=c0: e.tensor_tensor(at[:, j, c0:c0 + TB], sg_[:, 0:TB], u_[:, 0:TB], ALU.mult),
                                 reads=["tsg%d" % s, "tu%d" % s], writes=[(ak, j, c0)])
                            for _ in range(6):
                                if pending:
                                    pending.pop(0)()
                    while pending:
                        pending.pop(0)()
                gcols = slice(fb * FB, (fb + 1) * FB)
                blocks.append(([(wg_d[ex].rearrange("(k p) n -> p k n", p=128)[:, :, gcols], lambda buf: gu_view(buf)[:, 0]),
                                (wu_d[ex].rearrange("(k p) n -> p k n", p=128)[:, :, gcols], lambda buf: gu_view(buf)[:, 1])], use_gu))

                def use_d(buf, key, ex=ex, fb=fb, last=last, ck=ck, tok0=tok0):
                    bv = d_view(buf)
                    at = actT[(ex * NFB + fb) % 2]; ak = "actT%d" % ((ex * NFB + fb) % 2)
                    n = 0
                    for ti in range(NT):
                        for cb in range(4):
                            def grp(ti=ti, cb=cb, n=n):
                                ps = PS[4 + (n % 4)]; pk = "ps%d" % (4 + (n % 4))
                                for j in range(2):
                                    p.op("pe", lambda e, j=j: e.matmul(ps[:, :], lhsT=at[:, j, ti * 128:(ti + 1) * 128], rhs=bv[:, j, cb * 512:(cb + 1) * 512],
                                                                     start=(j == 0), stop=(j == 1)), reads=[key, ak], writes=[pk])
                                a = acc[:, ti, cb * 512:(cb + 1) * 512]
                                p.op("dve", lambda e: e.scalar_tensor_tensor(out=a, in0=ps[:, :], scalar=rw[:, ck * NT + ti, ex:ex + 1], in1=a, op0=ALU.mult, op1=ALU.add),
                                     reads=[pk, "rw", ("acc", ti, cb)], writes=[("acc", ti, cb)])
                                if last and cb == 3:
                                    p.dma(out_d[tok0 + ti * 128: tok0 + (ti + 1) * 128, :], acc[:, ti, :], reads=[("acc", ti)])
                            pending.append(grp)
                            n += 1
                    return True
                blocks.append(([(wd_d[ex].rearrange("(k p) n -> p k n", p=128)[:, fb * 2:(fb + 1) * 2, :], d_view)], use_d))
    run_stream2(p, ring, blocks)
    while pending:
        pending.pop(0)()
    p.emit()
    return nc


def run_stream2(p, ring, blocks):
    R = len(ring)

    def load(j):
        if j < len(blocks):
            for i, (ap, vf) in enumerate(blocks[j][0]):
                p.dma(vf(ring[j % R]), ap, writes=[("ring", j % R, i)], q="pool")

    for j in range(min(R, len(blocks))):
        load(j)
    held = []
    for j, (_, use) in enumerate(blocks):
        deferred = use(ring[j % R], ("ring", j % R))
        if deferred:
            held.append(j + R)
        else:
            for jj in held:
                load(jj)
            held = []
            load(j + R)


def build_B3():
    NT = 9
    T = NT * 128
    nc = bass.Bass("TRN2", target_bir_lowering=False)

    def din(name, shape, dt=F32):
        return nc.dram_tensor(name, list(shape), dt, kind="ExternalInput").ap()
    parts_d = din("parts", [8, T, D]); xmid_d = din("xmid", [T, D]); g2_d = din("G2", [128, 2, D])
    out_d = nc.dram_tensor("out", [T, D], F32, kind="ExternalOutput").ap()
    p = Prog(nc)
    G2 = p.sb([128, 2, D], F32, "G2")
    pt = [p.sb([128, 8, D], F32, "pt%da" % i) for i in range(2)]
    xm = [p.sb([128, D], F32, "xm%d" % i) for i in range(2)]
    p.dma(G2[:], g2_d, writes=["G2"])
    for ti in range(NT):
        v = 0 if ti < 2 else 1
        s = ti % 2
        for k in range(8):
            p.dma(pt[s][:, k, :], parts_d[k, ti * 128:(ti + 1) * 128, :], writes=[("pp%d" % s, k)], q=("sp" if k % 2 == 0 else "act"))
        p.dma(xm[s][:], xmid_d[ti * 128:(ti + 1) * 128, :], writes=["xm%d" % s])
        for k in range(1, 8):
            eng = "dve" if k % 2 == 1 else "pool"
            p.op("dve", lambda e, s=s, k=k: e.tensor_tensor(pt[s][:, 0, :], pt[s][:, 0, :], pt[s][:, k, :], ALU.add), reads=[("pp%d" % s, 0), ("pp%d" % s, k)], writes=[("pp%d" % s, 0)])
        p.op("dve", lambda e, s=s, v=v: e.tensor_tensor(pt[s][:, 0, :], pt[s][:, 0, :], G2[:, v, :], ALU.mult), reads=[("pp%d" % s, 0), "G2"], writes=[("pp%d" % s, 0)])
        p.op("pool", lambda e, s=s: e.tensor_tensor(pt[s][:, 0, :], pt[s][:, 0, :], xm[s][:], ALU.add), reads=[("pp%d" % s, 0), "xm%d" % s], writes=[("pp%d" % s, 0)])
        p.dma(out_d[ti * 128:(ti + 1) * 128, :], pt[s][:, 0, :], reads=[("pp%d" % s, 0)])
    p.emit()
    return nc
TOK = 2304
NTA = 18
HD = 128


def load_hT(p, x_d, modc, hT, small, xn, identb, PT, epsc, xt):
    for ti in range(NTA):
        v = 0 if ti < 2 else 1
        xtl = xt[ti % len(xt)]; xk = "xt%d" % (ti % len(xt))
        p.dma(xtl, x_d[ti * 128:(ti + 1) * 128, :], writes=[xk])
        norm_to_T(p, xtl, xk, modc, 0, 1, v, hT, ("hT", ti), ti * 128, small, xn, identb, PT, epsc)


def a_common(nc, p, xt=None, xn=None):
    def din(name, shape, dt=F32):
        return nc.dram_tensor(name, list(shape), dt, kind="ExternalInput").ap()
    x_d = din("x", [TOK, D]); modc_d = din("modc", [128, 2, KC, 2]); n1T_d = din("n1T", [128, KC]); ident_d = din("ident", [128, 128])
    hT = p.sb([128, KC, TOK], BF16, "hT")
    identb = p.sb([128, 128], BF16, "identb"); identf = p.sb([128, 128], F32, "identf")
    modc = p.sb([128, 2, KC, 2], F32, "modc"); n1T = p.sb([128, KC], F32, "n1T")
    small = p.sb([128, 64], F32, "small"); epsc = p.sb([128, 1], F32, "epsc")
    if xn is None:
        xn = [p.sb([128, D], BF16, "xn%d" % i) for i in range(2)]
    if xt is None:
        xt = [p.sb([128, D], F32, "xt%d" % i)[:, :] for i in range(2)]
    PT = [p.ps([128, 1024], BF16, "pt%d" % i) for i in range(2)]
    p.op("dve", lambda e: e.memset(epsc[:], float(D * EPS)), writes=["epsc"])
    p.dma(identb[:], ident_d, writes=["identb"], q="pool")
    p.dma(identf[:], ident_d, writes=["identf"])
    p.dma(modc[:], modc_d, writes=["modc"]); p.dma(n1T[:], n1T_d, writes=["n1T"])
    finish_modc(p, modc, [(1, n1T, "n1T")])
    load_hT(p, x_d, modc, hT, small, xn, identb, PT, epsc, xt)
    return din, hT, identb, identf, small, PT


def build_A_na(need_ctx=True, stage=9):
    nc = bass.Bass("TRN2", target_bir_lowering=False)
    p = Prog(nc)
    din, hT, identb, identf, small, PT = a_common(nc, p)
    w_d = din("w_na", [D, 768])
    qkw_d = din("qkw", [128, 2])
    bias_d = din("bias_g", [128, 8, 2, 6 * 64]); mask_d = din("maskneg", [128, 6 * 64])
    yT_d = nc.dram_tensor("yT", [256, TOK], BF16, kind="ExternalOutput").ap()

    wq = p.sb([128, KC, 768], BF16, "wq")
    qT = p.sb([128, 2, TOK], BF16, "qT"); kT = p.sb([128, 2, TOK], BF16, "kT")
    vA = p.sb([128, NTA, 256], BF16, "vA"); vS = p.sb([128, 15, 256], BF16, "vS")
    yT = p.sb([128, 2, TOK], BF16, "yT")
    qkw = p.sb([128, 2], F32, "qkw"); epsq = p.sb([128, 1], F32, "epsq")
    bm = p.sb([128, 8, 2, 384], F32, "bm"); maskneg = p.sb([128, 384], F32, "maskneg")
    onesb = p.sb([128, 128], BF16, "onesb")
    qf = [p.sb([128, 384], F32, "qf%d" % i) for i in range(2)]
    sq = [p.sb([128, 384], BF16, "sq%d" % i) for i in range(2)]
    rs = [p.sb([128, 384], F32, "rs%d" % i) for i in range(2)]
    st = [p.sb([128, 512], F32, "st%d" % i) for i in range(2)]
    pT = [p.sb([128, 512], BF16, "pT%d" % i) for i in range(2)]
    rden = p.sb([128, 512], F32, "rden")
    PS = [p.ps([128, 512], F32, "ps%d" % i) for i in range(6)]

    p.op("dve", lambda e: e.memset(onesb[:], 1.0), writes=["onesb"])
    p.op("dve", lambda e: e.memset(epsq[:], float(HD * EPS)), writes=["epsq"])
    p.dma(qkw[:], qkw_d, writes=["qkw"])
    p.op("dve", lambda e: e.tensor_scalar(qkw[:, 1:2], qkw[:, 1:2], float(np.sqrt(HD)), None, ALU.mult), reads=["qkw"], writes=["qkw"])
    p.dma(bm[:], bias_d, writes=["bm"]); p.dma(maskneg[:], mask_d, writes=["maskneg"])
    for pat in range(8):
        for h in range(2):
            p.op("pool", lambda e, pat=pat, h=h: e.tensor_tensor(bm[:, pat, h, :], bm[:, pat, h, :], maskneg[:], ALU.add), reads=["bm", "maskneg"], writes=["bm"])
    p.dma(wq[:], w_d.rearrange("(k p) n -> p k n", p=128), writes=["wq"], q="pool")

    n = 0
    for which, dst in ((0, qT), (1, kT)):
        for h in range(2):
            col = which * 256 + h * 128
            for blk in range(6):
                c0 = blk * 384
                s = n % 2; n += 1
                ps = PS[s]; pk = "ps%d" % s
                for kc in range(KC):
                    p.op("pe", lambda e, kc=kc, ps=ps, col=col, c0=c0: e.matmul(ps[:, 0:384], lhsT=wq[:, kc, col:col + 128], rhs=hT[:, kc, c0:c0 + 384],
                                                                              start=(kc == 0), stop=(kc == KC - 1)), reads=["wq", "hT"], writes=[pk])
                p.op("act", lambda e, ps=ps, s=s: e.activation(sq[s][:], ps[:, 0:384], AF.Square), reads=[pk], writes=["sq%d" % s])
                p.op("dve", lambda e, ps=ps, s=s: e.tensor_copy(qf[s][:], ps[:, 0:384]), reads=[pk], writes=["qf%d" % s])
                ps2 = PS[2 + s]; pk2 = "ps%d" % (2 + s)
                p.op("pe", lambda e, ps2=ps2, s=s: e.matmul(ps2[:, 0:384], lhsT=onesb[:], rhs=sq[s][:], start=True, stop=True), reads=["onesb", "sq%d" % s], writes=[pk2])
                p.op("act", lambda e, ps2=ps2, s=s: e.activation(rs[s][:], ps2[:, 0:384], AF.Ln, bias=epsq[:, 0:1], scale=1.0), reads=[pk2, "epsq"], writes=["rs%d" % s])
                p.op("act", lambda e, s=s: e.activation(rs[s][:], rs[s][:], AF.Exp, scale=-0.5), reads=["rs%d" % s], writes=["rs%d" % s])
                p.op("dve", lambda e, s=s, dst=dst, h=h, c0=c0, which=which: e.scalar_tensor_tensor(out=dst[:, h, c0:c0 + 384], in0=qf[s][:], scalar=qkw[:, which:which + 1], in1=rs[s][:],
                                                                                                 op0=ALU.mult, op1=ALU.mult), reads=["qf%d" % s, "rs%d" % s, "qkw"], writes=[("qk", which, h)])
    for (vt, ntile, off) in (((vA, NTA, 0), (vS, 15, 256 + 64)) if stage >= 2 else ()):
        for ti in range(ntile):
            c0 = off + ti * 128
            s = n % 2; n += 1
            ps = PS[s]; pk = "ps%d" % s
            for kc in range(KC):
                p.op("pe", lambda e, kc=kc, ps=ps, c0=c0: e.matmul(ps[:, 0:256], lhsT=hT[:, kc, c0:c0 + 128], rhs=wq[:, kc, 512:768],
                                                                 start=(kc == 0), stop=(kc == KC - 1)), reads=["wq", "hT"], writes=[pk])
            p.op("act", lambda e, ps=ps, vt=vt, ti=ti: e.copy(vt[:, ti, :], ps[:, 0:256]), reads=[pk], writes=["v"])

    def vtile(tok0, h):
        if tok0 % 128 == 0:
            return vA[:, tok0 // 128, h * 128:(h + 1) * 128]
        return vS[:, (tok0 - 320) // 128, h * 128:(h + 1) * 128]

    it = 0
    for h in (range(2) if stage >= 3 else ()):
        for r8 in range(4):
            po = PS[2 + (r8 % 2)]; pok = "ps%d" % (2 + (r8 % 2))
            pd = PS[4 + (r8 % 2)]; pdk = "ps%d" % (4 + (r8 % 2))
            for rr in range(8):
                r = r8 * 8 + rr
                srow = min(max(r - 4, 0), 24)
                pat = r if r < 4 else (4 if r <= 28 else r - 24)
                q0 = 256 + r * 64
                ktok = [256 + 64 * srow + 128 * kt for kt in range(4)] + [0, 128]
                s = it % 2; it += 1
                ps = PS[s]; pk = "ps%d" % s
                for kt in range(6):
                    p.op("pe", lambda e, kt=kt, ps=ps, h=h, q0=q0, t0=ktok[kt]: e.matmul(ps[:, kt * 64:(kt + 1) * 64], lhsT=kT[:, h, t0:t0 + 128], rhs=qT[:, h, q0:q0 + 64],
                                                                                     start=True, stop=True), reads=[("qk", 0, h), ("qk", 1, h)], writes=[pk])
                p.op("dve", lambda e, ps=ps, s=s, pat=pat, h=h: e.tensor_tensor(st[s][:, 0:384], ps[:, 0:384], bm[:, pat, h, :], ALU.add), reads=[pk, "bm"], writes=["st%d" % s])
                p.op("act", lambda e, s=s: e.activation(pT[s][:, 0:384], st[s][:, 0:384], AF.Exp), reads=["st%d" % s], writes=["pT%d" % s])
                for kt in range(6):
                    p.op("pe", lambda e, kt=kt, s=s, h=h, rr=rr, po=po, t0=ktok[kt]: e.matmul(po[:, rr * 64:(rr + 1) * 64], lhsT=vtile(t0, h), rhs=pT[s][:, kt * 64:(kt + 1) * 64],
                                                                                          start=(kt == 0), stop=(kt == 5)), reads=["v", "pT%d" % s], writes=[pok])
                for kt in range(6):
                    p.op("pe", lambda e, kt=kt, s=s, rr=rr, pd=pd: e.matmul(pd[:, rr * 64:(rr + 1) * 64], lhsT=onesb[:], rhs=pT[s][:, kt * 64:(kt + 1) * 64],
                                                                          start=(kt == 0), stop=(kt == 5)), reads=["onesb", "pT%d" % s], writes=[pdk])
            p.op("dve", lambda e, pd=pd: e.reciprocal(rden[:], pd[:, :]), reads=[pdk], writes=["rden"])
            p.op("dve", lambda e, po=po, h=h, r8=r8: e.tensor_tensor(yT[:, h, 256 + r8 * 512: 256 + (r8 + 1) * 512], po[:, :], rden[:], ALU.mult), reads=[pok, "rden"], writes=[("yT", h)])
    if need_ctx and stage >= 4:
        for h in range(2):
            ps = PS[0]; pk = "ps0"
            for kt in range(2):
                p.op("pe", lambda e, kt=kt, h=h: e.matmul(PS[0][:, kt * 256:(kt + 1) * 256], lhsT=kT[:, h, kt * 128:(kt + 1) * 128], rhs=qT[:, h, 0:256],
                                                       start=True, stop=True), reads=[("qk", 0, h), ("qk", 1, h)], writes=["ps0"])
            p.op("act", lambda e: e.activation(pT[0][:, :], PS[0][:, :], AF.Exp), reads=["ps0"], writes=["pT0"])
            for kt in range(2):
                p.op("pe", lambda e, kt=kt, h=h: e.matmul(PS[2][:, 0:256], lhsT=vA[:, kt, h * 128:(h + 1) * 128], rhs=pT[0][:, kt * 256:(kt + 1) * 256],
                                                       start=(kt == 0), stop=(kt == 1)), reads=["v", "pT0"], writes=["ps2"])
            for kt in range(2):
                p.op("pe", lambda e, kt=kt: e.matmul(PS[4][:, 0:256], lhsT=onesb[:], rhs=pT[0][:, kt * 256:(kt + 1) * 256],
                                                  start=(kt == 0), stop=(kt == 1)), reads=["onesb", "pT0"], writes=["ps4"])
            p.op("dve", lambda e: e.reciprocal(rden[:, 0:256], PS[4][:, 0:256]), reads=["ps4"], writes=["rden"])
            p.op("dve", lambda e, h=h: e.tensor_tensor(yT[:, h, 0:256], PS[2][:, 0:256], rden[:, 0:256], ALU.mult), reads=["ps2", "rden"], writes=[("yT", h)])
    else:
        p.op("dve", lambda e: e.memset(yT[:, :, 0:256], 0.0), writes=["yT"])
    for h in range(2):
        p.dma(yT_d[h * 128:(h + 1) * 128, :], (yT if stage >= 3 else qT)[:, h, :], reads=[("yT", h), ("qk", 0, h)])
    p.emit()
    return nc
HC = 32
NCH = TOK // HC
FWD_ORDER = list(range(NCH))
NCTX = 256 // HC
BWD_ORDER = list(range(NCTX - 1, -1, -1)) + list(range(NCH - 1, NCTX - 1, -1))


def build_A_hg():
    nc = bass.Bass("TRN2", target_bir_lowering=False)
    p = Prog(nc)
    B = [p.sb([128, TOK], F32, "B%d" % i) for i in range(4)]
    xt = [B[0][:, 0:D]]
    b1v = B[1][:, :].bitcast(BF16)
    xn = [b1v[:, 0:D], b1v[:, D:2 * D]]
    din, hT, identb, identf, small, PT = a_common(nc, p, xt=xt, xn=xn)
    w_d = din("w_hg", [3, D, 640])
    lbl_d = din("lbl", [128, 2, 2, 3]); sel_d = din("lbsel", [128, 2])
    nw_d = din("hg_nw", [128, 1]); mask_d = din("hgmask", [HC, 2, HC])
    yT_d = nc.dram_tensor("yT", [384, TOK], BF16, kind="ExternalOutput").ap()

    W = p.sb([128, KC, 640], BF16, "W")
    QP = [p.sb([128, TOK], BF16, "QP%d" % i) for i in range(2)]
    KP = [p.sb([128, TOK], BF16, "KP%d" % i) for i in range(2)]
    SG = p.sb([128, TOK], BF16, "SG"); YO = p.sb([128, TOK], BF16, "YO")
    YACC = p.sb([128, TOK], F32, "YACC")
    V64 = p.sb([HC, NCH, 128], BF16, "V64")
    ones128 = p.sb([128, 128], BF16, "ones128")
    lbl = p.sb([128, 2, 2, 3], F32, "lbl"); sel = p.sb([128, 2], F32, "sel"); lb = p.sb([128, 2, 3], F32, "lb"); oml = p.sb([128, 2, 3], F32, "oml")
    nw = p.sb([128, 1], F32, "nw"); epsq = p.sb([128, 1], F32, "epsq")
    maskf = p.sb([HC, 2, HC], F32, "maskf")
    colA = p.sb([128, 2, 8, NCH], F32, "colA")
    attm = [p.sb([HC, HC], BF16, "attm%d" % i) for i in range(2)]
    ktt = [p.sb([HC, 128], BF16, "ktt%d" % i) for i in range(2)]
    Z = [p.sb([128, 128], F32, "Z%d" % i) for i in range(2)]
    Mb = [p.sb([128, 128], BF16, "Mb%d" % i) for i in range(2)]
    sq = [p.sb([128, 384], BF16, "sq%d" % i) for i in range(2)]
    rs = [p.sb([128, 384], F32, "rs%d" % i) for i in range(2)]
    PS = [p.ps([128, 512], F32, "ps%d" % i) for i in range(5)]
    PK = p.ps([HC, 128], BF16, "pk")

    p.op("dve", lambda e: e.memset(ones128[:], 1.0), writes=["ones128"])
    p.op("dve", lambda e: e.memset(epsq[:], float(128 * EPS)), writes=["epsq"])
    p.dma(lbl[:], lbl_d, writes=["lbl"]); p.dma(sel[:], sel_d, writes=["sel"]); p.dma(nw[:], nw_d, writes=["nw"]); p.dma(maskf[:], mask_d, writes=["maskf"])
    p.op("act", lambda e: e.activation(lbl[:], lbl[:], AF.Exp), reads=["lbl"], writes=["lbl"])
    p.op("dve", lambda e: e.tensor_tensor(oml[:], lbl[:, 0], lbl[:, 1], ALU.add), reads=["lbl"], writes=["oml"])
    p.op("dve", lambda e: e.reciprocal(oml[:], oml[:]), reads=["oml"], writes=["oml"])
    p.op("dve", lambda e: e.tensor_scalar(lb[:], lbl[:, 0], sel[:, 0:1], None, ALU.mult), reads=["lbl", "sel"], writes=["lb"])
    p.op("dve", lambda e: e.scalar_tensor_tensor(out=lb[:], in0=lbl[:, 1], scalar=sel[:, 1:2], in1=lb[:], op0=ALU.mult, op1=ALU.add), reads=["lbl", "sel", "lb"], writes=["lb"])
    p.op("dve", lambda e: e.tensor_tensor(lb[:], lb[:], oml[:], ALU.mult), reads=["lb", "oml"], writes=["lb"])
    p.op("dve", lambda e: e.tensor_scalar(oml[:], lb[:], -1.0, 1.0, ALU.mult, ALU.add), reads=["lb"], writes=["oml"])
    p.op("dve", lambda e: e.tensor_scalar(nw[:], nw[:], float(np.sqrt(128.0)), None, ALU.mult), reads=["nw"], writes=["nw"])

    def proj(col, evac):
        for blk in range(6):
            c0 = blk * 384
            ps = PS[blk % 2]; pk = "ps%d" % (blk % 2)
            for kc in range(KC):
                p.op("pe", lambda e, kc=kc, ps=ps, c0=c0: e.matmul(ps[:, 0:384], lhsT=W[:, kc, col:col + 128], rhs=hT[:, kc, c0:c0 + 384],
                                                                 start=(kc == 0), stop=(kc == KC - 1)), reads=["W", "hT"], writes=[pk])
            evac(ps, pk, c0)

    for h in range(3):
        p.dma(W[:], w_d[h].rearrange("(k p) n -> p k n", p=128), writes=["W"], q="pool")
        QS, F, LF, CUM = B[0], B[1], B[2], B[3]
        E = LF
        p.op("dve", lambda e: e.memset(YO[:], 1.0), writes=["YO"])
        proj(0, lambda ps, pk, c0: p.op("act", lambda e: e.activation(QS[:, c0:c0 + 384], ps[:, 0:384], AF.Silu), reads=[pk], writes=["QS"]))
        proj(384, lambda ps, pk, c0: p.op("act", lambda e: e.activation(SG[:, c0:c0 + 384], ps[:, 0:384], AF.Silu), reads=[pk], writes=["SG"]))
        for c in range(NCH):
            ps = PS[c % 2]; pk = "ps%d" % (c % 2)
            for kc in range(KC):
                p.op("pe", lambda e, kc=kc, ps=ps, c=c: e.matmul(ps[0:HC, 0:128], lhsT=hT[:, kc, c * HC:(c + 1) * HC], rhs=W[:, kc, 512:640],
                                                               start=(kc == 0), stop=(kc == KC - 1)), reads=["W", "hT"], writes=[pk])
            p.op("act", lambda e, ps=ps, c=c: e.copy(V64[:, c, :], ps[0:HC, 0:128]), reads=[pk], writes=["V64"])
        for d in range(2):
            proj(128 + d * 128, lambda ps, pk, c0: p.op("act", lambda e: e.activation(F[:, c0:c0 + 384], ps[:, 0:384], AF.Sigmoid), reads=[pk], writes=["F"]))
            p.op("dve", lambda e, d=d, h=h: e.tensor_scalar(F[:], F[:], oml[:, d, h:h + 1], lb[:, d, h:h + 1], ALU.mult, ALU.add), reads=["F", "oml", "lb"], writes=["F"])
            p.op("dve", lambda e: e.tensor_scalar(LF[:], F[:], 1e-6, None, ALU.max), reads=["F"], writes=["LF"])
            p.op("act", lambda e: e.activation(LF[:], LF[:], AF.Ln), reads=["LF"], writes=["LF"])
            p.op("dve", lambda e: e.tensor_tensor_scan(CUM[:], YO[:], LF[:], 0.0, ALU.mult, ALU.add), reads=["YO", "LF"], writes=["CUM"])
            C3 = CUM[:, :].rearrange("p (c j) -> p c j", j=HC)
            L3 = LF[:, :].rearrange("p (c j) -> p c j", j=HC)
            E3 = E[:, :].rearrange("p (c j) -> p c j", j=HC)
            cb, ct, cm, clm, cg, cG = (colA[:, d, i, :] for i in range(6))
            ck = ("colA", d)
            p.op("dve", lambda e, cb=cb: e.memset(cb[:, 0:1], 0.0), writes=[ck])
            p.op("dve", lambda e, cb=cb, C3=C3: e.tensor_copy(cb[:, 1:NCH], C3[:, 0:NCH - 1, HC - 1]), reads=["CUM"], writes=[ck])
            p.op("dve", lambda e, cb=cb, C3=C3: e.tensor_tensor(C3, C3, cb.unsqueeze(2).to_broadcast([128, NCH, HC]), ALU.subtract), reads=["CUM", ck], writes=["CUM"])
            p.op("dve", lambda e, ct=ct, C3=C3: e.tensor_copy(ct, C3[:, :, HC - 1]), reads=["CUM"], writes=[ck])
            if d == 1:
                p.op("dve", lambda e, ct=ct, C3=C3: e.tensor_tensor(C3, ct.unsqueeze(2).to_broadcast([128, NCH, HC]), C3, ALU.subtract), reads=["CUM", ck], writes=["CUM"])
                p.op("dve", lambda e: e.tensor_tensor(CUM[:], CUM[:], LF[:], ALU.add), reads=["CUM", "LF"], writes=["CUM"])
                p.op("dve", lambda e, cm=cm, C3=C3: e.tensor_copy(cm, C3[:, :, HC // 2]), reads=["CUM"], writes=[ck])
            else:
                p.op("dve", lambda e, cm=cm, C3=C3: e.tensor_copy(cm, C3[:, :, HC // 2 - 1]), reads=["CUM"], writes=[ck])
            p.op("dve", lambda e, ct=ct, cm=cm, clm=clm: e.tensor_tensor(clm, ct, cm, ALU.subtract), reads=[ck], writes=[ck])
            if d == 0:
                p.op("dve", lambda e, cg=cg, clm=clm, cm=cm: e.tensor_tensor(cg[:, 0:NCH - 1], clm[:, 0:NCH - 1], cm[:, 1:NCH], ALU.add), reads=[ck], writes=[ck])
                p.op("dve", lambda e, cg=cg, clm=clm: e.tensor_copy(cg[:, NCH - 1:NCH], clm[:, NCH - 1:NCH]), reads=[ck], writes=[ck])
            else:
                p.op("dve", lambda e, cg=cg, clm=clm, cm=cm: e.tensor_tensor(cg[:, 1:NCH], clm[:, 1:NCH], cm[:, 0:NCH - 1], ALU.add), reads=[ck], writes=[ck])
                p.op("dve", lambda e, cg=cg, clm=clm, cm=cm: e.tensor_tensor(cg[:, 0:1], clm[:, 0:1], cm[:, NCH - 1:NCH], ALU.add), reads=[ck], writes=[ck])
            p.op("act", lambda e, cg=cg, cG=cG: e.activation(cG, cg, AF.Exp), reads=[ck], writes=[ck])
            p.op("dve", lambda e, cm=cm, C3=C3, E3=E3: e.tensor_tensor(E3, C3, cm.unsqueeze(2).to_broadcast([128, NCH, HC]), ALU.subtract), reads=["CUM", ck, "LF"], writes=["LF"])
            p.op("act", lambda e: e.activation(CUM[:], E[:], AF.Exp), reads=["LF"], writes=["CUM"])
            p.op("dve", lambda e, d=d: e.scalar_tensor_tensor(out=QP[d][:], in0=CUM[:], scalar=float(128.0 ** -0.5), in1=QS[:], op0=ALU.mult, op1=ALU.mult), reads=["CUM", "QS"], writes=["QP%d" % d])
            p.op("act", lambda e: e.activation(E[:], E[:], AF.Exp, scale=-1.0), reads=["LF"], writes=["LF"])
            p.op("dve", lambda e: e.tensor_scalar(F[:], F[:], -1.0, 1.0, ALU.mult, ALU.add), reads=["F"], writes=["F"])
            p.op("dve", lambda e, d=d: e.tensor_tensor(KP[d][:], F[:], E[:], ALU.mult), reads=["F", "LF"], writes=["KP%d" % d])
        p.op("dve", lambda e: e.memset(YACC[:], 0.0), writes=["YACC"])
        orders = [FWD_ORDER, BWD_ORDER]
        for step in range(NCH):
            for d in range(2):
                c = orders[d][step]
                cs = slice(c * HC, (c + 1) * HC)
                pa = PS[2]; pak = "ps2"
                p.op("pe", lambda e, d=d, cs=cs: e.matmul(PS[2][0:HC, 0:HC], lhsT=KP[d][:, cs], rhs=QP[d][:, cs], start=True, stop=True), reads=["KP%d" % d, "QP%d" % d], writes=["ps2"])
                p.op("dve", lambda e, d=d: e.tensor_tensor(attm[d][:], PS[2][0:HC, 0:HC], maskf[:, d, :], ALU.mult), reads=["ps2", "maskf"], writes=["attm%d" % d])
                py = PS[3]; pyk = "ps3"
                p.op("pe", lambda e, d=d, c=c, step=step: e.matmul(PS[3][:, 0:HC], lhsT=V64[:, c, :], rhs=attm[d][:], start=True, stop=(step == 0)), reads=["V64", "attm%d" % d], writes=["ps3"])
                if step > 0:
                    p.op("pe", lambda e, d=d, cs=cs: e.matmul(PS[3][:, 0:HC], lhsT=Mb[d][:], rhs=QP[d][:, cs], start=False, stop=True), reads=["Mb%d" % d, "QP%d" % d], writes=["ps3"])
                p.op("dve", lambda e, cs=cs: e.tensor_tensor(YACC[:, cs], YACC[:, cs], PS[3][:, 0:HC], ALU.add), reads=["ps3", ("YACC", c)], writes=[("YACC", c)])
                if step < NCH - 1:
                    p.op("pe", lambda e, d=d, cs=cs: e.transpose(PK[:, :], KP[d][:, cs], identb[:]), reads=["KP%d" % d, "identb"], writes=["pk"])
                    p.op("act", lambda e, d=d: e.copy(ktt[d][:], PK[:, :]), reads=["pk"], writes=["ktt%d" % d])
                    p.op("pe", lambda e, d=d, c=c: e.matmul(PS[4][:, 0:128], lhsT=ktt[d][:], rhs=V64[:, c, :], start=True, stop=True), reads=["ktt%d" % d, "V64"], writes=["ps4"])
                    if step == 0:
                        p.op("dve", lambda e, d=d: e.tensor_copy(Z[d][:], PS[4][:, 0:128]), reads=["ps4"], writes=["Z%d" % d])
                    else:
                        cprev = orders[d][step - 1]
                        p.op("dve", lambda e, d=d, cprev=cprev: e.scalar_tensor_tensor(out=Z[d][:], in0=Z[d][:], scalar=colA[:, d, 5, cprev:cprev + 1], in1=PS[4][:, 0:128],
                                                                                     op0=ALU.mult, op1=ALU.add), reads=["Z%d" % d, ("colA", d), "ps4"], writes=["Z%d" % d])
                    p.op("act", lambda e, d=d, c=c: e.activation(Mb[d][:], Z[d][:], AF.Identity, scale=colA[:, d, 5, c:c + 1]), reads=["Z%d" % d, ("colA", d)], writes=["Mb%d" % d])
        for blk in range(6):
            c0 = blk * 384
            s = blk % 2
            p.op("act", lambda e, s=s, c0=c0: e.activation(sq[s][:], YACC[:, c0:c0 + 384], AF.Square), reads=["YACC"], writes=["sq%d" % s])
            p.op("pe", lambda e, s=s: e.matmul(PS[s][:, 0:384], lhsT=ones128[:], rhs=sq[s][:], start=True, stop=True), reads=["ones128", "sq%d" % s], writes=["ps%d" % s])
            p.op("act", lambda e, s=s: e.activation(rs[s][:], PS[s][:, 0:384], AF.Ln, bias=epsq[:, 0:1], scale=1.0), reads=["ps%d" % s, "epsq"], writes=["rs%d" % s])
            p.op("act", lambda e, s=s: e.activation(rs[s][:], rs[s][:], AF.Exp, scale=-0.5), reads=["rs%d" % s], writes=["rs%d" % s])
            p.op("dve", lambda e, s=s, c0=c0: e.scalar_tensor_tensor(out=rs[s][:], in0=YACC[:, c0:c0 + 384], scalar=nw[:, 0:1], in1=rs[s][:], op0=ALU.mult, op1=ALU.mult),
                 reads=["YACC", "nw", "rs%d" % s], writes=["rs%d" % s])
            p.op("dve", lambda e, s=s, c0=c0: e.tensor_tensor(YO[:, c0:c0 + 384], rs[s][:], SG[:, c0:c0 + 384], ALU.mult), reads=["rs%d" % s, "SG"], writes=["YO"])
        p.dma(yT_d[h * 128:(h + 1) * 128, :], YO[:], reads=["YO"])
    p.emit()
    return nc
SSD_FWD = list(range(NTA))
SSD_BWD = [1, 0] + list(range(NTA - 1, 1, -1))


def build_A_ssd():
    nc = bass.Bass("TRN2", target_bir_lowering=False)
    p = Prog(nc)
    XR = p.sb([128, TOK], F32, "XR"); ACC = p.sb([128, TOK], F32, "ACC")
    accv = ACC[:, :].bitcast(BF16)
    din, hT, identb, identf, small, PT = a_common(nc, p, xt=[XR[:, 0:D]], xn=[accv[:, 0:D], accv[:, D:2 * D]])
    wz_d = din("w_z", [D, 384]); wx_d = din("w_xbc", [D, 896]); wdt_d = din("w_dt", [D, 12])
    cw_d = din("conv_w", [128, 7, 4]); cb_d = din("conv_b", [128, 7])
    dtb_d = din("dt_bias", [128, 12]); alog_d = din("a_log", [128, 12])
    dsk_d = din("dskip", [128, 384]); nw_d = din("ssd_nw", [128, 384])
    cos_d = din("rope_cos", [128, 2048]); sin_d = din("rope_sin", [128, 2048]); pm_d = din("rope_pm", [128, 128])
    tri_d = din("tri", [128, 2, 128]); mneg_d = din("mneg", [128, 2, 128])
    yT_d = nc.dram_tensor("yT", [384, TOK], BF16, kind="ExternalOutput").ap()

    Wc = [p.sb([128, KC, 128], BF16, "Wc0")] * 2
    wdt = p.sb([128, KC, 12], BF16, "wdt")
    BT = p.sb([128, 2, TOK], BF16, "BT"); CT = p.sb([128, 2, TOK], BF16, "CT")
    xs_tok = p.sb([128, NTA, 384], BF16, "xs_tok"); B_tok = p.sb([128, NTA, 256], BF16, "B_tok")
    sz_tok = p.sb([128, NTA, 384], BF16, "sz_tok")
    yacc = p.sb([128, NTA, 384], F32, "yacc")
    cosb = p.sb([128, 512], BF16, "cosb"); sinb = p.sb([128, 512], BF16, "sinb"); pmb = p.sb([128, 128], BF16, "pmb")
    XB = p.sb([128, TOK], BF16, "XB")
    cw = p.sb([128, 7, 4], F32, "cw"); cbias = p.sb([128, 7], F32, "cbias")
    dtb = p.sb([128, 12], F32, "dtb"); aexp = p.sb([128, 12], F32, "aexp")
    dsk = p.sb([128, 384], F32, "dsk"); nwt = p.sb([128, 384], F32, "nwt")
    tri = p.sb([128, 2, 128], BF16, "tri"); mneg = p.sb([128, 2, 128], BF16, "mneg")
    dt = p.sb([128, NTA, 12], F32, "dt"); da = p.sb([128, NTA, 12], F32, "da")
    dah = p.sb([128, NTA, 12], BF16, "dah"); dal = p.sb([128, NTA, 12], BF16, "dal"); dtmp = p.sb([128, NTA, 12], F32, "dtmp")
    cum = p.sb([128, NTA, 12], F32, "cum"); bcol = p.sb([128, NTA, 12], F32, "bcol"); ecum = p.sb([128, NTA, 12], F32, "ecum")
    SD = [p.sb([128, 128], F32, "SD%d" % i) for i in range(2)]
    MT = [p.sb([128, 128], BF16, "MT%d" % i) for i in range(2)]
    CBs = [p.sb([128, 128], F32, "CBs%d" % i) for i in range(2)]
    xw = p.sb([128, 384], BF16, "xw"); el = p.sb([128, 6], F32, "el")
    tmp = p.sb([128, 384], F32, "tmp")
    hS = [p.sb([128, 384], F32, "hS%d" % i) for i in range(2)]
    hSb = [p.sb([128, 384], BF16, "hSb%d" % i) for i in range(2)]
    yfin = p.sb([128, 384], F32, "yfin"); tmp2 = yfin; yfb = p.sb([128, 384], BF16, "yfb"); ytile = p.sb([128, 3, 128], BF16, "ytile")
    gss = p.sb([128, 4], F32, "gss"); epsg = p.sb([128, 1], F32, "epsg")
    PS = [p.ps([128, 512], F32, "ps%d" % i) for i in range(6)]

    for t_, d_, k_ in ((cw, cw_d, "cw"), (cbias, cb_d, "cbias"), (dtb, dtb_d, "dtb"), (aexp, alog_d, "aexp"), (dsk, dsk_d, "dsk"), (nwt, nw_d, "nwt")):
        p.dma(t_[:], d_, writes=[k_])
    for t_, d_, k_ in ((pmb, pm_d, "pmb"), (tri, tri_d, "tri"), (mneg, mneg_d, "mneg"), (wdt, wdt_d.rearrange("(k p) n -> p k n", p=128), "wdt")):
        p.dma(t_[:], d_, writes=[k_], q="pool")
    p.op("act", lambda e: e.activation(aexp[:], aexp[:], AF.Exp), reads=["aexp"], writes=["aexp"])
    p.op("dve", lambda e: e.memset(epsg[:], float(192 * EPS)), writes=["epsg"])
    p.op("dve", lambda e: e.tensor_scalar(nwt[:], nwt[:], float(np.sqrt(192.0)), None, ALU.mult), reads=["nwt"], writes=["nwt"])

    for ti in range(NTA):
        ps = PS[ti % 2]; pk = "ps%d" % (ti % 2)
        for kc in range(KC):
            p.op("pe", lambda e, kc=kc, ps=ps, ti=ti: e.matmul(ps[:, 0:12], lhsT=hT[:, kc, ti * 128:(ti + 1) * 128], rhs=wdt[:, kc, :], start=(kc == 0), stop=(kc == KC - 1)),
                 reads=["hT", "wdt"], writes=[pk])
        p.op("dve", lambda e, ps=ps, ti=ti: e.tensor_tensor(dt[:, ti, :], ps[:, 0:12], dtb[:], ALU.add), reads=[pk, "dtb"], writes=["dt"])
    p.op("act", lambda e: e.activation(dt[:], dt[:], AF.Exp), reads=["dt"], writes=["dt"])
    p.op("act", lambda e: e.activation(dt[:], dt[:], AF.Ln, bias=1.0, scale=1.0), reads=["dt"], writes=["dt"])
    p.op("dve", lambda e: e.tensor_tensor(da[:], dt[:], aexp[:].unsqueeze(1).to_broadcast([128, NTA, 12]), ALU.mult), reads=["dt", "aexp"], writes=["da"])
    p.op("dve", lambda e: e.tensor_scalar(da[:], da[:], -1.0, None, ALU.mult), reads=["da"], writes=["da"])
    p.op("dve", lambda e: e.tensor_copy(dah[:], da[:]), reads=["da"], writes=["dah"])
    p.op("dve", lambda e: e.tensor_tensor(dtmp[:], da[:], dah[:], ALU.subtract), reads=["da", "dah"], writes=["dtmp"])
    p.op("dve", lambda e: e.tensor_copy(dal[:], dtmp[:]), reads=["dtmp"], writes=["dal"])
    for ti in range(NTA):
        ps = PS[ti % 2]; pk = "ps%d" % (ti % 2)
        for d in range(2):
            p.op("pe", lambda e, ps=ps, ti=ti, d=d: e.matmul(ps[:, d * 6:(d + 1) * 6], lhsT=tri[:, d, :], rhs=dah[:, ti, d * 6:(d + 1) * 6], start=True, stop=False), reads=["tri", "dah"], writes=[pk])
            p.op("pe", lambda e, ps=ps, ti=ti, d=d: e.matmul(ps[:, d * 6:(d + 1) * 6], lhsT=tri[:, d, :], rhs=dal[:, ti, d * 6:(d + 1) * 6], start=False, stop=True), reads=["tri", "dal"], writes=[pk])
        p.op("dve", lambda e, ps=ps, ti=ti: e.tensor_copy(cum[:, ti, :], ps[:, 0:12]), reads=[pk], writes=["cum"])
    p.op("act", lambda e: e.activation(ecum[:], cum[:], AF.Exp), reads=["cum"], writes=["ecum"])
    p.op("act", lambda e: e.activation(bcol[:], dt[:], AF.Ln), reads=["dt"], writes=["bcol"])
    p.op("dve", lambda e: e.tensor_tensor(bcol[:], bcol[:], cum[:], ALU.subtract), reads=["bcol", "cum"], writes=["bcol"])

    for j in range(3):
        wc = Wc[0]; wk = "Wc0"
        p.dma(wc[:], wz_d.rearrange("(k p) n -> p k n", p=128)[:, :, j * 128:(j + 1) * 128], writes=[wk], q="pool")
        for ti in range(NTA):
            ps = PS[ti % 2]; pk = "ps%d" % (ti % 2)
            for kc in range(KC):
                p.op("pe", lambda e, kc=kc, ps=ps, ti=ti, wc=wc: e.matmul(ps[:, 0:128], lhsT=hT[:, kc, ti * 128:(ti + 1) * 128], rhs=wc[:, kc, :], start=(kc == 0), stop=(kc == KC - 1)),
                     reads=["hT", wk], writes=[pk])
            p.op("act", lambda e, ps=ps, ti=ti, j=j: e.activation(sz_tok[:, ti, j * 128:(j + 1) * 128], ps[:, 0:128], AF.Silu), reads=[pk], writes=["sz_tok"])

    for ch in range(7):
        wc = Wc[0]; wk = "Wc0"
        p.dma(wc[:], wx_d.rearrange("(k p) n -> p k n", p=128)[:, :, ch * 128:(ch + 1) * 128], writes=[wk], q="pool")
        for blk in range(6):
            c0 = blk * 384
            ps = PS[blk % 2]; pk = "ps%d" % (blk % 2)
            for kc in range(KC):
                p.op("pe", lambda e, kc=kc, ps=ps, c0=c0, wc=wc: e.matmul(ps[:, 0:384], lhsT=wc[:, kc, :], rhs=hT[:, kc, c0:c0 + 384], start=(kc == 0), stop=(kc == KC - 1)),
                     reads=["hT", wk], writes=[pk])
            p.op("act", lambda e, ps=ps, c0=c0: e.copy(XR[:, c0:c0 + 384], ps[:, 0:384]), reads=[pk], writes=["XR"])
        p.op("act", lambda e, ch=ch: e.activation(ACC[:], XR[:], AF.Identity, bias=cbias[:, ch:ch + 1], scale=cw[:, ch, 1:2]), reads=["XR", "cbias", "cw"], writes=["ACC"])
        for (a, b) in ((0, 256), (256, TOK)):
            for (j, sh) in ((0, -1), (2, 1), (3, 2)):
                lo = max(a, a - sh); hi = min(b, b - sh)
                p.op("dve", lambda e, ch=ch, j=j, sh=sh, lo=lo, hi=hi: e.scalar_tensor_tensor(out=ACC[:, lo:hi], in0=XR[:, lo + sh:hi + sh], scalar=cw[:, ch, j:j + 1], in1=ACC[:, lo:hi],
                                                                                            op0=ALU.mult, op1=ALU.add), reads=["XR", "ACC", "cw"], writes=["ACC"])
        p.op("act", lambda e: e.activation(XB[:], ACC[:], AF.Silu), reads=["ACC"], writes=["XB"])
        if ch < 3:
            for ti in range(NTA):
                hh = ti % 2
                p.op("pe", lambda e, ti=ti, hh=hh: e.transpose(PT[hh][:, 0:128], XB[:, ti * 128:(ti + 1) * 128], identb[:]), reads=["XB", "identb"], writes=["pt%d" % hh])
                p.op("act", lambda e, ti=ti, hh=hh, ch=ch: e.copy(xs_tok[:, ti, ch * 128:(ch + 1) * 128], PT[hh][:, 0:128]), reads=["pt%d" % hh], writes=["xs_tok"])
        else:
            g = (ch - 3) % 2
            dst = BT if ch < 5 else CT; dk = "BT" if ch < 5 else "CT"
            p.op("act", lambda e, dst=dst, g=g: e.copy(dst[:, g, 0:256], XB[:, 0:256]), reads=["XB"], writes=[dk])
            for blk in range(4):
                c0 = 256 + blk * 512
                ps = PS[2 + blk % 2]; pk = "ps%d" % (2 + blk % 2)
                p.dma(cosb[:], cos_d[:, blk * 512:(blk + 1) * 512], writes=["cosb"], q="pool")
                p.dma(sinb[:], sin_d[:, blk * 512:(blk + 1) * 512], writes=["sinb"], q="pool")
                p.op("pe", lambda e, ps=ps, c0=c0: e.matmul(ps[:, :], lhsT=pmb[:], rhs=XB[:, c0:c0 + 512], start=True, stop=True), reads=["pmb", "XB"], writes=[pk])
                p.op("dve", lambda e, ps=ps, blk=blk: e.tensor_tensor(XR[:, 0:512], ps[:, :], sinb[:, :], ALU.mult), reads=[pk, "sinb"], writes=["XR"])
                p.op("pool", lambda e, c0=c0, blk=blk: e.tensor_tensor(XR[:, 512:1024], XB[:, c0:c0 + 512], cosb[:, :], ALU.mult), reads=["XB", "cosb"], writes=[("XR", 1)])
                p.op("dve", lambda e, dst=dst, g=g, c0=c0: e.tensor_tensor(dst[:, g, c0:c0 + 512], XR[:, 0:512], XR[:, 512:1024], ALU.add), reads=["XR"], writes=[dk])
            if ch < 5:
                for ti in range(NTA):
                    hh = ti % 2
                    p.op("pe", lambda e, ti=ti, hh=hh, g=g: e.transpose(PT[hh][:, 0:128], BT[:, g, ti * 128:(ti + 1) * 128], identb[:]), reads=["BT", "identb"], writes=["pt%d" % hh])
                    p.op("act", lambda e, ti=ti, hh=hh, g=g: e.copy(B_tok[:, ti, g * 128:(g + 1) * 128], PT[hh][:, 0:128]), reads=["pt%d" % hh], writes=["B_tok"])

    p.op("dve", lambda e: e.memset(yacc[:], 0.0), writes=["yacc"])
    orders = [SSD_FWD, SSD_BWD]
    it = 0
    for step in range(NTA):
        for d in range(2):
            c = orders[d][step]
            cs = slice(c * 128, (c + 1) * 128)
            lastcol = 127 if d == 0 else 0
            for g in range(2):
                p.op("pe", lambda e, g=g, cs=cs: e.matmul(PS[2][:, g * 128:(g + 1) * 128], lhsT=BT[:, g, cs], rhs=CT[:, g, cs], start=True, stop=True), reads=["BT", "CT"], writes=["ps2"])
                p.op("act", lambda e, g=g: e.copy(CBs[g][:], PS[2][:, g * 128:(g + 1) * 128]), reads=["ps2"], writes=["CBs%d" % g])
            for hd in range(6):
                g = hd // 3
                col = d * 6 + hd
                s = it % 2; it += 1
                pr = PS[s]; prk = "ps%d" % s
                p.op("pe", lambda e, pr=pr, c=c, col=col, d=d: e.matmul(pr[:, 0:128], lhsT=dah[:, c, col:col + 1].to_broadcast([128, 128]), rhs=tri[:, d, :], start=True, stop=False), reads=["dah", "tri"], writes=[prk])
                p.op("pe", lambda e, pr=pr, c=c, col=col, d=d: e.matmul(pr[:, 0:128], lhsT=dal[:, c, col:col + 1].to_broadcast([128, 128]), rhs=tri[:, d, :], start=False, stop=False), reads=["dal", "tri"], writes=[prk])
                p.op("pe", lambda e, pr=pr, d=d: e.matmul(pr[:, 0:128], lhsT=identb[:], rhs=mneg[:, d, :], start=False, stop=True), reads=["identb", "mneg"], writes=[prk])
                p.op("act", lambda e, pr=pr, s=s, c=c, col=col: e.activation(SD[s][:], pr[:, 0:128], AF.Exp, bias=bcol[:, c, col:col + 1], scale=1.0), reads=[prk, "bcol"], writes=["SD%d" % s])
                p.op("act", lambda e, pr=pr, hd=hd, lastcol=lastcol: e.activation(el[:, hd:hd + 1], pr[:, lastcol:lastcol + 1], AF.Exp), reads=[prk], writes=["el"])
                p.op("dve", lambda e, s=s, g=g: e.tensor_tensor(MT[s][:], SD[s][:], CBs[g][:], ALU.mult), reads=["SD%d" % s, "CBs%d" % g], writes=["MT%d" % s])
                p.op("pe", lambda e, s=s, c=c, hd=hd: e.matmul(PS[3][:, hd * 64:(hd + 1) * 64], lhsT=MT[s][:], rhs=xs_tok[:, c, hd * 64:(hd + 1) * 64], start=True, stop=True), reads=["MT%d" % s, "xs_tok"], writes=["ps3"])
                if step > 0:
                    p.op("pe", lambda e, g=g, cs=cs, hd=hd, d=d: e.matmul(PS[4][:, hd * 64:(hd + 1) * 64], lhsT=CT[:, g, cs], rhs=hSb[d][:, hd * 64:(hd + 1) * 64], start=True, stop=True), reads=["CT", "hSb%d" % d], writes=["ps4"])
                if step < NTA - 1:
                    p.op("dve", lambda e, s=s, c=c, hd=hd, lastcol=lastcol: e.tensor_scalar(xw[:, hd * 64:(hd + 1) * 64], xs_tok[:, c, hd * 64:(hd + 1) * 64], SD[s][:, lastcol:lastcol + 1], None, ALU.mult),
                         reads=["SD%d" % s, "xs_tok"], writes=["xw"])
            if step > 0:
                p.op("dve", lambda e, c=c, d=d: e.tensor_tensor(tmp[:, :].rearrange("p (h j) -> p h j", j=64), PS[4][:, 0:384].rearrange("p (h j) -> p h j", j=64),
                                                             ecum[:, c, d * 6:(d + 1) * 6].unsqueeze(2).to_broadcast([128, 6, 64]), ALU.mult), reads=["ps4", "ecum"], writes=["tmp"])
                p.op("pool", lambda e, c=c: e.tensor_tensor(yacc[:, c, :], yacc[:, c, :], tmp[:], ALU.add), reads=["tmp", ("yacc", c)], writes=[("yacc", c)])
            p.op("dve", lambda e, c=c: e.tensor_tensor(yacc[:, c, :], yacc[:, c, :], PS[3][:, 0:384], ALU.add), reads=["ps3", ("yacc", c)], writes=[("yacc", c)])
            if step < NTA - 1:
                for g in range(2):
                    p.op("pe", lambda e, g=g, c=c: e.matmul(PS[5][:, g * 192:(g + 1) * 192], lhsT=B_tok[:, c, g * 128:(g + 1) * 128], rhs=xw[:, g * 192:(g + 1) * 192], start=True, stop=True),
                         reads=["B_tok", "xw"], writes=["ps5"])
                if step == 0:
                    p.op("dve", lambda e, d=d: e.tensor_copy(hS[d][:], PS[5][:, 0:384]), reads=["ps5"], writes=["hS%d" % d])
                else:
                    p.op("pool", lambda e, d=d: e.tensor_tensor(tmp2[:, :].rearrange("p (h j) -> p h j", j=64), hS[d][:, :].rearrange("p (h j) -> p h j", j=64),
                                                              el[:, :].unsqueeze(2).to_broadcast([128, 6, 64]), ALU.mult), reads=["hS%d" % d, "el"], writes=["yfin"])
                    p.op("dve", lambda e, d=d: e.tensor_tensor(hS[d][:], tmp2[:], PS[5][:, 0:384], ALU.add), reads=["yfin", "ps5"], writes=["hS%d" % d])
                p.op("act", lambda e, d=d: e.copy(hSb[d][:], hS[d][:]), reads=["hS%d" % d], writes=["hSb%d" % d])

    for ti in range(NTA):
        p.op("dve", lambda e, ti=ti: e.tensor_tensor(yfin[:], xs_tok[:, ti, :], dsk[:], ALU.mult), reads=["xs_tok", "dsk"], writes=["yfin"])
        p.op("dve", lambda e, ti=ti: e.tensor_tensor(yfin[:], yfin[:], yacc[:, ti, :], ALU.add), reads=["yfin", ("yacc", ti)], writes=["yfin"])
        p.op("dve", lambda e, ti=ti: e.tensor_tensor(yfin[:], yfin[:], sz_tok[:, ti, :], ALU.mult), reads=["yfin", "sz_tok"], writes=["yfin"])
        p.op("pool", lambda e: e.tensor_tensor(tmp[:], yfin[:], yfin[:], ALU.mult), reads=["yfin"], writes=["tmp"])
        p.op("dve", lambda e: e.reduce_sum(gss[:, 0:2], tmp[:, :].rearrange("p (g j) -> p g j", j=192), axis=AX.X), reads=["tmp"], writes=["gss"])
        p.op("act", lambda e: e.activation(gss[:, 2:4], gss[:, 0:2], AF.Ln, bias=epsg[:, 0:1], scale=1.0), reads=["gss", "epsg"], writes=["gss"])
        p.op("act", lambda e: e.activation(gss[:, 2:4], gss[:, 2:4], AF.Exp, scale=-0.5), reads=["gss"], writes=["gss"])
        p.op("dve", lambda e: e.tensor_tensor(tmp[:, :].rearrange("p (g j) -> p g j", j=192), yfin[:, :].rearrange("p (g j) -> p g j", j=192),
                                              gss[:, 2:4].unsqueeze(2).to_broadcast([128, 2, 192]), ALU.mult), reads=["yfin", "gss"], writes=["tmp"])
        p.op("dve", lambda e: e.tensor_tensor(yfb[:], tmp[:], nwt[:], ALU.mult), reads=["tmp", "nwt"], writes=["yfb"])
        hh = ti % 2
        for j in range(3):
            p.op("pe", lambda e, j=j, hh=hh: e.transpose(PT[hh][:, j * 128:(j + 1) * 128], yfb[:, j * 128:(j + 1) * 128], identb[:]), reads=["yfb", "identb"], writes=["pt%d" % hh])
        p.op("act", lambda e, hh=hh: e.copy(ytile[:, :, :], PT[hh][:, 0:384].rearrange("p (j t) -> p j t", j=3)), reads=["pt%d" % hh], writes=["ytile"])
        p.dma(yT_d.rearrange("(j p) t -> p j t", p=128)[:, :, ti * 128:(ti + 1) * 128], ytile[:, :, :], reads=["ytile"])
    p.emit()
    return nc
import os
import time
import ml_dtypes
from concourse.bass_utils import run_bass_kernel_spmd

NMIX = 7960
_PROGS = {}
_DBG = {}


def _prog(name):
    if name not in _PROGS:
        _PROGS[name] = {"M": build_M, "na": build_A_na, "hg": build_A_hg, "ssd": build_A_ssd,
                        "B1": lambda: build_B1(9, 2, [3, 3, 3]), "B2x": build_B2x, "B3": build_B3}[name]()
    return _PROGS[name]


def _run(name, in_maps):
    t0 = time.time()
    res = run_bass_kernel_spmd(_prog(name), in_maps, core_ids=list(range(8)))
    if int(os.environ.get("KDEBUG", "0")):
        print("launch", name, "%.1fs" % (time.time() - t0), flush=True)
    return res.results


def lay16(v):
    return np.ascontiguousarray(v.reshape(16, 128).T)


def modc_from(mod_v0, mod_v1, qs):
    out = np.zeros((128, len(qs), 16, 2), np.float32)
    for qi, q in enumerate(qs):
        out[:, qi, :, 0] = lay16(mod_v0[q * 2048:(q + 1) * 2048])
        out[:, qi, :, 1] = lay16(mod_v1[q * 2048:(q + 1) * 2048])
    return out


def gtile(mod_v0, mod_v1, q):
    out = np.empty((128, 2, 2048), np.float32)
    out[:, 0, :] = mod_v0[q * 2048:(q + 1) * 2048][None, :]
    out[:, 1, :] = mod_v1[q * 2048:(q + 1) * 2048][None, :]
    return out


def na_bias_gather(rpb, heads):
    out = np.zeros((128, 8, 2, 6, 64), np.float32)
    pats = [0, 1, 2, 3, 10, 29, 30, 31]
    c = np.arange(64)[:, None]; j = np.arange(64)[None, :]
    dc = np.clip(c - j, -15, 15) + 15
    for pi, r in enumerate(pats):
        srow = min(max(r - 4, 0), 24)
        for hi, h in enumerate(heads):
            for kt in range(4):
                for wl in range(2):
                    dr = srow + 2 * kt + wl - r
                    out[wl * 64:(wl + 1) * 64, pi, hi, kt, :] = rpb[h, dr + 7][dc]
    return out.reshape(128, 8, 2, 384)


def na_maskneg():
    m = np.zeros((128, 6, 64), np.float32)
    c = np.arange(64)[:, None]; j = np.arange(64)[None, :]
    c0 = np.clip(j - 8, 0, 48)
    mm = np.where((c >= c0) & (c < c0 + 16), 0.0, -30000.0).astype(np.float32)
    for kt in range(4):
        m[0:64, kt] = mm; m[64:128, kt] = mm
    return m.reshape(128, 384)


def hg_inputs(w_in, lb_logits, hg_norm_w, L, half):
    heads = [3 * half + i for i in range(3)]
    order = [0, 1, 2, 4, 3]
    w = np.stack([np.concatenate([w_in[:, o * 768 + h * 128: o * 768 + (h + 1) * 128] for o in order], axis=1) for h in heads], axis=0)
    lbl = np.zeros((128, 2, 2, 3), np.float32)
    for l in range(2):
        for dd in range(2):
            for hi, h in enumerate(heads):
                lbl[:, l, dd, hi] = lb_logits[l, dd, h * 128:(h + 1) * 128]
    sel = np.zeros((128, 2), np.float32); sel[:, 1] = 1.0 if L == 1 else 0.0
    s = np.arange(32)[:, None]; t = np.arange(32)[None, :]
    mask = np.stack([(s <= t), (s >= t)], axis=1).astype(np.float32)
    return {"w_hg": np.ascontiguousarray(w), "lbl": lbl, "lbsel": sel, "hg_nw": np.ascontiguousarray(hg_norm_w[:, None]), "hgmask": np.ascontiguousarray(mask)}


def ssd_consts():
    inv = 1.0 / (10000.0 ** (np.arange(0, 64, 2, dtype=np.float32) / 64))
    t = np.arange(2048)
    row = (t // 64).astype(np.float32); col = (t % 64).astype(np.float32)
    cos = np.zeros((128, 2048), np.float32); sin = np.zeros((128, 2048), np.float32)
    for n in range(128):
        ang = (row if n < 64 else col) * inv[n % 32]
        cos[n] = np.cos(ang); sin[n] = np.sin(ang)
    pm = np.zeros((128, 128), np.float32)
    for n2 in range(128):
        if (n2 % 64) < 32:
            pm[n2 + 32, n2] = -1.0
        else:
            pm[n2 - 32, n2] = 1.0
    r = np.arange(128)[:, None]; tt = np.arange(128)[None, :]
    tri = np.stack([(r <= tt), (r >= tt)], axis=1).astype(np.float32)
    mneg = np.stack([np.where(r <= tt, 0.0, -30000.0), np.where(r >= tt, 0.0, -30000.0)], axis=1).astype(np.float32)
    return {"rope_cos": cos, "rope_sin": sin, "rope_pm": pm, "tri": np.ascontiguousarray(tri), "mneg": np.ascontiguousarray(mneg)}


def ssd_inputs(w_in, conv_w, conv_b, dt_bias, a_log, d_skip, norm_w, half, consts):
    heads = [6 * half + i for i in range(6)]
    zc = list(range(5376 + 384 * half, 5376 + 384 * (half + 1)))
    xsc = list(range(6144 + 384 * half, 6144 + 384 * (half + 1)))
    bc = list(range(6912 + 256 * half, 6912 + 256 * (half + 1)))
    cc = list(range(7424 + 256 * half, 7424 + 256 * (half + 1)))
    dtc = list(range(7936 + 6 * half, 7936 + 6 * half + 6)) + list(range(7948 + 6 * half, 7948 + 6 * half + 6))
    conv_ch = [c_ - 6144 for c_ in xsc + bc + cc]
    cwl = np.ascontiguousarray(conv_w[:, conv_ch].reshape(4, 7, 128).transpose(2, 1, 0))
    cbl = np.ascontiguousarray(conv_b[conv_ch].reshape(7, 128).T)
    dtb = np.concatenate([dt_bias[0][heads], dt_bias[1][heads]])
    alog = np.concatenate([a_log[0][heads], a_log[1][heads]])
    dsk = np.repeat(d_skip[heads], 64)
    nw = norm_w[384 * half:384 * (half + 1)]
    out = {"w_z": np.ascontiguousarray(w_in[:, zc]), "w_xbc": np.ascontiguousarray(w_in[:, xsc + bc + cc]), "w_dt": np.ascontiguousarray(w_in[:, dtc]),
           "conv_w": cwl, "conv_b": cbl, "dt_bias": np.ascontiguousarray(np.broadcast_to(dtb, (128, 12))), "a_log": np.ascontiguousarray(np.broadcast_to(alog, (128, 12))),
           "dskip": np.ascontiguousarray(np.broadcast_to(dsk, (128, 384))), "ssd_nw": np.ascontiguousarray(np.broadcast_to(nw, (128, 384)))}
    out.update(consts)
    return out


def kernel(x, c, ctx, c_ctx, w_mod, b_mod, norm1_w, norm2_w, w_in, hg_lb_logits, hg_norm_w,
           na_q_norm_w, na_k_norm_w, na_rpb, ssd_conv_w, ssd_conv_b, ssd_dt_bias, ssd_a_log, ssd_d,
           ssd_norm_w, w_branch_hg, w_branch_na, w_branch_ssd, w_out, moe_w_router, moe_b_router,
           moe_w_gate, moe_b_gate, moe_w_up, moe_b_up, moe_w_down, moe_b_down):
    f32 = lambda a: np.asarray(a, dtype=np.float32)
    x, c, ctx, c_ctx = f32(x), f32(c), f32(ctx), f32(c_ctx)
    dbg = bool(int(os.environ.get("KDEBUG", "0")))
    ident = np.eye(128, dtype=np.float32)
    cvecs = [c[b] for b in range(4)] + [c_ctx]
    cT5 = np.ascontiguousarray(np.stack([lay16(v) for v in cvecs], axis=-1))
    w_mod = f32(w_mod); b_mod = f32(b_mod)
    ims = [{"cT5": cT5, "w_mod": np.ascontiguousarray(w_mod[:, :, k * MCOLS:(k + 1) * MCOLS]),
            "b_mod": np.ascontiguousarray(b_mod[:, None, k * MCOLS:(k + 1) * MCOLS])} for k in range(8)]
    rM = _run("M", ims)
    mod = np.concatenate([np.asarray(rM[k]["mod"]) for k in range(8)], axis=2)
    if dbg:
        _DBG["mod"] = mod
    consts = ssd_consts()
    maskneg = na_maskneg()
    xl = [x[b] for b in range(4)]; xc = [ctx[b] for b in range(4)]
    for L in range(2):
        W_in = f32(w_in[L])
        xcat = [np.ascontiguousarray(np.concatenate([xc[b], xl[b]], axis=0)) for b in range(4)]
        n1T = lay16(f32(norm1_w[L])); n2T = lay16(f32(norm2_w[L]))
        base = [{"x": xcat[k // 2], "modc": modc_from(mod[L, 4], mod[L, k // 2], [0, 1]), "n1T": n1T, "ident": ident} for k in range(8)]
        ims = []
        for k in range(8):
            half = k % 2
            heads = [2 * half, 2 * half + 1]
            cols = [3840 + w_ * 512 + h * 128 + i for w_ in range(3) for h in heads for i in range(128)]
            m = dict(base[k]); m.update({"w_na": np.ascontiguousarray(W_in[:, cols]), "qkw": np.ascontiguousarray(np.stack([f32(na_q_norm_w[L]), f32(na_k_norm_w[L])], axis=1)),
                                         "bias_g": na_bias_gather(f32(na_rpb[L]), heads), "maskneg": maskneg})
            ims.append(m)
        r_na = _run("na", ims)
        ims = []
        for k in range(8):
            m = dict(base[k]); m.update(hg_inputs(W_in, f32(hg_lb_logits), f32(hg_norm_w[L]), L, k % 2)); ims.append(m)
        r_hg = _run("hg", ims)
        ims = []
        for k in range(8):
            m = dict(base[k]); m.update(ssd_inputs(W_in, f32(ssd_conv_w[L]), f32(ssd_conv_b[L]), f32(ssd_dt_bias[L]), f32(ssd_a_log[L]), f32(ssd_d[L]), f32(ssd_norm_w[L]), k % 2, consts)); ims.append(m)
        r_ssd = _run("ssd", ims)
        yT = [np.concatenate([np.asarray(r_hg[2 * b]["yT"]), np.asarray(r_hg[2 * b + 1]["yT"]), np.asarray(r_na[2 * b]["yT"]), np.asarray(r_na[2 * b + 1]["yT"]),
                              np.asarray(r_ssd[2 * b]["yT"]), np.asarray(r_ssd[2 * b + 1]["yT"])], axis=0) for b in range(4)]
        if dbg:
            _DBG["yT%d" % L] = [np.asarray(y).astype(np.float32) for y in yT]
        wgi = np.ascontiguousarray(W_in[:, NMIX:])
        wb = np.ascontiguousarray(np.concatenate([f32(w_branch_hg[L]), f32(w_branch_na[L]), f32(w_branch_ssd[L])], axis=0))
        ims = []
        for k in range(8):
            b, half = k // 2, k % 2
            sl = slice(1152 * half, 1152 * (half + 1))
            v0 = mod[L, 4] if half == 0 else mod[L, b]
            ims.append({"x": np.ascontiguousarray(xcat[b][sl]), "yT": np.ascontiguousarray(yT[b][:, sl]), "modc4": modc_from(v0, mod[L, b], [0, 1, 3, 4]),
                        "G1": gtile(v0, mod[L, b], 2), "n1T": n1T, "n2T": n2T, "wgi": wgi, "wb": wb, "wo": f32(w_out[L]),
                        "wr": f32(moe_w_router[L]), "br": f32(moe_b_router[L])[None, :], "ident": ident})
        r_b1 = _run("B1", ims)
        if dbg:
            _DBG["xmid%d" % L] = [np.asarray(r_b1[k]["xmid"]) for k in range(8)]
        h2T_all = np.ascontiguousarray(np.concatenate([np.asarray(r_b1[k]["h2T"]) for k in range(8)], axis=1))
        rw_all = np.concatenate([np.asarray(r_b1[k]["rw"]) for k in range(8)], axis=0)
        ims = []
        for k in range(8):
            es = slice(4 * k, 4 * k + 4)
            rwo = np.ascontiguousarray(rw_all[:, es])
            ims.append({"h2T": h2T_all, "rw": rwo, "rwT": np.ascontiguousarray(rwo.T), "wg": f32(moe_w_gate[L][es]), "wu": f32(moe_w_up[L][es]), "wd": f32(moe_w_down[L][es]),
                        "bgT": np.ascontiguousarray(f32(moe_b_gate[L][es]).reshape(4, 16, 128).transpose(2, 0, 1)),
                        "buT": np.ascontiguousarray(f32(moe_b_up[L][es]).reshape(4, 16, 128).transpose(2, 0, 1)), "bd": f32(moe_b_down[L][es])})
        r_b2 = _run("B2x", ims)
        ims = []
        for k in range(8):
            b, half = k // 2, k % 2
            v0 = mod[L, 4] if half == 0 else mod[L, b]
            parts = np.ascontiguousarray(np.stack([np.asarray(r_b2[j]["part"])[k * 1152:(k + 1) * 1152] for j in range(8)], axis=0))
            ims.append({"parts": parts, "xmid": np.asarray(r_b1[k]["xmid"]), "G2": gtile(v0, mod[L, b], 5)})
        r_b3 = _run("B3", ims)
        for b in range(4):
            full = np.concatenate([np.asarray(r_b3[2 * b]["out"]), np.asarray(r_b3[2 * b + 1]["out"])], axis=0)
            xc[b] = full[:256]; xl[b] = full[256:]
        if dbg:
            _DBG["xout%d" % L] = [a.copy() for a in xl]; _DBG["xcout%d" % L] = [a.copy() for a in xc]
    return np.stack(xl, axis=0).astype(np.float32)
```
